# Optimizing a Trainium2 kernel written in Bass

```python
import math
import jax
import jax.numpy as jnp
from jax import lax
import numpy as np

D_MODEL = 1024
BATCH = 16
SEQ = 2048
DEPTH = 4

HEAD_DIM = 64
ROPE_THETA = 500000.0
ROT_DIM = HEAD_DIM // 4
NORM_EPS = 1e-6
BLOCK = 128
NEG_INF = -1e30

A_HEADS = 8
A_WIDTH = A_HEADS * HEAD_DIM
A_PATTERNS = ((128, 1), (512, 4), (2048, 16))

B_HEADS = 4
B_QK_DIM = 64
B_V_DIM = 128
B_CHUNK = 128
RET_THETA = 10000.0

MEM_TOKENS = 256
MEM_HEADS = 4
MEM_WIDTH = MEM_HEADS * HEAD_DIM

C_HEADS = 8
C_Q_RANK = 256
C_KV_RANK = 128
C_NOPE = 64
C_ROPE = 32
C_V = 64

D_HEADS = 8
D_HEAD = 64
D_WIDTH = D_HEADS * D_HEAD
D_DECAY_LORA = 64
D_AAA_LORA = 64
D_MV_LORA = 32
RWKV_LN_EPS = 64e-5

EVEN_MIX = A_WIDTH + B_HEADS * B_V_DIM + MEM_WIDTH
ODD_MIX = C_HEADS * C_V + D_WIDTH + MEM_WIDTH
EVEN_COLS = (A_WIDTH, A_WIDTH, A_WIDTH, B_HEADS * B_QK_DIM, B_HEADS * B_QK_DIM,
             B_HEADS * B_V_DIM, MEM_WIDTH, EVEN_MIX)

kernel_name = 'hybrid_dilated_retention_mla_rwkv7_trunk'


def split_cols(t, sizes):
    return jnp.split(t, np.cumsum(sizes)[:-1].tolist(), axis=-1)


def rms_norm(x, g):
    xf = x.astype(jnp.float32)
    y = xf * lax.rsqrt(jnp.mean(xf * xf, -1, keepdims=True) + NORM_EPS)
    return (y * g.astype(jnp.float32)).astype(x.dtype)


def rope_angles(positions, dim, theta):
    inv = jnp.exp(-math.log(theta) * jnp.arange(0, dim, 2, dtype=jnp.float32) / dim)
    ang = positions.astype(jnp.float32)[..., None] * inv
    return jnp.cos(ang), jnp.sin(ang)


def apply_rotary(x, cos, sin):
    half = x.shape[-1] // 2
    c = cos[:, :, None, :].astype(x.dtype)
    s = sin[:, :, None, :].astype(x.dtype)
    x1, x2 = x[..., :half], x[..., half:]
    return jnp.concatenate([x1 * c - x2 * s, x1 * s + x2 * c], -1)


def partial_rotary(x, cos, sin):
    return jnp.concatenate([apply_rotary(x[..., :ROT_DIM], cos, sin), x[..., ROT_DIM:]], -1)


def banded_window_attention(q, k, v, window):
    n, L, h, hd = q.shape
    blk = min(BLOCK, L)
    nb = -(-L // blk)
    pad = nb * blk - L
    if pad:
        q, k, v = (jnp.pad(t, ((0, 0), (0, pad), (0, 0), (0, 0))) for t in (q, k, v))
    qb, kb, vb = (t.reshape(n, nb, blk, h, hd) for t in (q, k, v))
    kk = jnp.concatenate([jnp.concatenate([jnp.zeros_like(kb[:, :1]), kb[:, :-1]], 1), kb], 2)
    vv = jnp.concatenate([jnp.concatenate([jnp.zeros_like(vb[:, :1]), vb[:, :-1]], 1), vb], 2)
    s = jnp.einsum('nbqhd,nbkhd->nbhqk', qb, kk).astype(jnp.float32) * hd ** -0.5
    qi = jnp.arange(blk)[:, None]
    kj = jnp.arange(2 * blk)[None, :] - blk
    dist = qi - kj
    kpos = (jnp.arange(nb) * blk)[:, None, None] + kj[None]
    mask = (dist >= 0)[None] & (dist <= window)[None] & (kpos >= 0)
    s = jnp.where(mask[None, :, None], s, NEG_INF)
    m = jnp.max(s, -1, keepdims=True)
    e = jnp.exp(s - m)
    den = jnp.sum(e, -1, keepdims=True)
    p = e / den
    lse = (m + jnp.log(den))[..., 0]
    o = jnp.einsum('nbhqk,nbkhd->nbqhd', p.astype(v.dtype), vv).reshape(n, nb * blk, h, hd)[:, :L]
    lse = lse.transpose(0, 1, 3, 2).reshape(n, nb * blk, h)[:, :L]
    return o, lse


def dilated_pattern(q, k, v, window, dilation):
    b, s, h, hd = q.shape
    L = s // dilation
    def to_sub(t):
        return t.reshape(b, L, dilation, h, hd).transpose(0, 2, 1, 3, 4).reshape(b * dilation, L, h, hd)
    o, lse = banded_window_attention(to_sub(q), to_sub(k), to_sub(v), window // dilation)
    o = o.reshape(b, dilation, L, h, hd).transpose(0, 2, 1, 3, 4).reshape(b, s, h, hd)
    lse = lse.reshape(b, dilation, L, h).transpose(0, 2, 1, 3).reshape(b, s, h)
    return o, lse


def dilated_mixture_attention(q, k, v):
    res = [dilated_pattern(q, k, v, w, d) for (w, d) in A_PATTERNS]
    outs = jnp.stack([r[0] for r in res], 0)
    wts = jax.nn.softmax(jnp.stack([r[1] for r in res], 0), axis=0)
    return jnp.einsum('pbsh,pbshd->bshd', wts.astype(q.dtype), outs)


def retention(q, k, v, rot):
    b, s, h, dk = q.shape
    dv = v.shape[-1]
    cos, sin = rot
    qf = apply_rotary(q, cos, sin).astype(jnp.float32)
    kf = apply_rotary(k, cos, sin).astype(jnp.float32) * dk ** -0.5
    vf = v.astype(jnp.float32)
    log_g = jnp.log1p(-jnp.exp2(-5.0 - jnp.arange(h, dtype=jnp.float32)))
    c = B_CHUNK
    n = s // c
    qc = qf.reshape(b, n, c, h, dk)
    kc = kf.reshape(b, n, c, h, dk)
    vc = vf.reshape(b, n, c, h, dv)
    idx = jnp.arange(c, dtype=jnp.float32)
    diff = idx[:, None] - idx[None, :]
    d_in = jnp.where(diff[None] >= 0, jnp.exp(jnp.maximum(diff, 0.0)[None] * log_g[:, None, None]), 0.0)
    scores = jnp.einsum('bnqhd,bnkhd->bnhqk', qc, kc) * d_in
    o_in = jnp.einsum('bnhqk,bnkhe->bnqhe', scores, vc)
    to_end = jnp.exp((c - 1.0 - idx)[:, None] * log_g[None])
    chunk_kv = jnp.einsum('bnkhd,kh,bnkhe->nbhde', kc, to_end, vc)
    g_chunk = jnp.exp(c * log_g)[None, :, None, None]
    def step(state, kv):
        return state * g_chunk + kv, state
    _, prev = lax.scan(step, jnp.zeros((b, h, dk, dv), jnp.float32), chunk_kv)
    from_start = jnp.exp((idx + 1.0)[:, None] * log_g[None])
    o_cross = jnp.einsum('bnqhd,nbhde->bnqhe', qc, prev) * from_start[None, None, :, :, None]
    o = (o_in + o_cross).reshape(b, s, h, dv)
    o = o * lax.rsqrt(jnp.mean(o * o, -1, keepdims=True) + NORM_EPS)
    return o.astype(v.dtype)


def causal_attention(q, k, v, scale):
    b, s, h, dq = q.shape
    nq = s // BLOCK
    qb = q.reshape(b, nq, BLOCK, h, dq).transpose(1, 0, 2, 3, 4)
    kpos = jnp.arange(s)
    def one(args):
        qblk, bi = args
        sc = jnp.einsum('bqhd,bkhd->bhqk', qblk, k).astype(jnp.float32) * scale
        qpos = bi * BLOCK + jnp.arange(BLOCK)
        sc = jnp.where((kpos[None, :] <= qpos[:, None])[None, None], sc, NEG_INF)
        p = jax.nn.softmax(sc, axis=-1)
        return jnp.einsum('bhqk,bkhd->bqhd', p.astype(v.dtype), v)
    o = lax.map(one, (qb, jnp.arange(nq)))
    return o.transpose(1, 0, 2, 3, 4).reshape(b, s, h, v.shape[-1])


def mla_attention(cq, ckv, k_rope, rot, q_norm, w_qb, kv_norm, w_kvb):
    b, s, _ = cq.shape
    cos, sin = rot
    q = (rms_norm(cq, q_norm) @ w_qb).reshape(b, s, C_HEADS, C_NOPE + C_ROPE)
    q = jnp.concatenate([q[..., :C_NOPE], apply_rotary(q[..., C_NOPE:], cos, sin)], -1)
    kv = (rms_norm(ckv, kv_norm) @ w_kvb).reshape(b, s, C_HEADS, C_NOPE + C_V)
    k_pe = apply_rotary(k_rope[:, :, None, :], cos, sin)
    k = jnp.concatenate([kv[..., :C_NOPE], jnp.broadcast_to(k_pe, (b, s, C_HEADS, C_ROPE))], -1)
    o = causal_attention(q, k, kv[..., C_NOPE:], (C_NOPE + C_ROPE) ** -0.5)
    return o.reshape(b, s, C_HEADS * C_V)


def memory_attention(qm, mem_n, w_mem_kv):
    b, s, _ = qm.shape
    kv = mem_n @ w_mem_kv
    km = kv[..., :MEM_WIDTH].reshape(b, MEM_TOKENS, MEM_HEADS, HEAD_DIM)
    vm = kv[..., MEM_WIDTH:].reshape(b, MEM_TOKENS, MEM_HEADS, HEAD_DIM)
    q = qm.reshape(b, s, MEM_HEADS, HEAD_DIM)
    sc = jnp.einsum('bshd,bmhd->bhsm', q, km).astype(jnp.float32) * HEAD_DIM ** -0.5
    p = jax.nn.softmax(sc, axis=-1)
    return jnp.einsum('bhsm,bmhd->bshd', p.astype(vm.dtype), vm).reshape(b, s, MEM_WIDTH)


def token_shift(h, mu):
    prev = jnp.pad(h, ((0, 0), (1, 0), (0, 0)))[:, :-1]
    return h + (prev - h) * mu


def wkv7_scan(r, w, k, v, a, bb):
    b, s, h, n = r.shape
    def step(state, inp):
        rt, wt, kt, vt, at, bt = inp
        sa = jnp.einsum('bhvk,bhk->bhv', state, at)
        state = state * wt[:, :, None, :] + sa[..., None] * bt[:, :, None, :] + vt[..., None] * kt[:, :, None, :]
        return state, jnp.einsum('bhvk,bhk->bhv', state, rt)
    xs = tuple(t.astype(jnp.float32).transpose(1, 0, 2, 3) for t in (r, w, k, v, a, bb))
    _, y = lax.scan(step, jnp.zeros((b, h, n, n), jnp.float32), xs)
    return y.transpose(1, 0, 2, 3)


def rwkv7_time_mix(shifted, v_first, w0, w2, a0, a2, v0, v2, k_k, k_a, r_k, lnx_w, lnx_b):
    b, s, _ = shifted.shape
    sizes = [D_WIDTH] * 3 + [D_DECAY_LORA, D_AAA_LORA] + ([D_MV_LORA] if v_first is not None else [])
    parts = split_cols(shifted, sizes)
    r, k, v, w_dn, a_dn = parts[:5]
    logw = -jax.nn.softplus(-(w0 + jnp.tanh(w_dn) @ w2).astype(jnp.float32)) - 0.5
    decay = jnp.exp(-jnp.exp(logw))
    a = jax.nn.sigmoid(a0 + a_dn @ a2)
    if v_first is None:
        v_first = v
    else:
        v = v + (v_first - v) * jax.nn.sigmoid(v0 + parts[5] @ v2)
    hd = lambda t: t.reshape(b, s, D_HEADS, D_HEAD)
    kk = hd(k * k_k).astype(jnp.float32)
    kk = kk / jnp.maximum(jnp.sqrt(jnp.sum(kk * kk, -1, keepdims=True)), 1e-12)
    k = k * (1.0 + (a - 1.0) * k_a)
    ah = hd(a).astype(jnp.float32)
    y = wkv7_scan(hd(r), hd(decay), hd(k), hd(v), -kk, kk * ah)
    mu = jnp.mean(y, -1, keepdims=True)
    var = jnp.mean((y - mu) ** 2, -1, keepdims=True)
    y = ((y - mu) * lax.rsqrt(var + RWKV_LN_EPS)).reshape(b, s, D_WIDTH) * lnx_w + lnx_b
    bonus = jnp.sum(hd(r) * hd(k) * r_k, -1, keepdims=True) * hd(v)
    return (y + bonus.reshape(b, s, D_WIDTH)).astype(shifted.dtype), v_first


def even_layer(x, mem_n, rot, ret_rot, norm, w_in, w_mem_kv, w_out):
    b, s, _ = x.shape
    h = rms_norm(x, norm)
    qa, ka, va, qr, kr, vr, qm, gate = split_cols(h @ w_in, EVEN_COLS)
    cos, sin = rot
    heads = lambda t, nh: t.reshape(b, s, nh, -1)
    a_out = dilated_mixture_attention(partial_rotary(heads(qa, A_HEADS), cos, sin),
                                      partial_rotary(heads(ka, A_HEADS), cos, sin), heads(va, A_HEADS))
    r_out = retention(heads(qr, B_HEADS), heads(kr, B_HEADS), heads(vr, B_HEADS), ret_rot)
    m_out = memory_attention(qm, mem_n, w_mem_kv)
    y = jnp.concatenate([a_out.reshape(b, s, A_WIDTH), r_out.reshape(b, s, B_HEADS * B_V_DIM), m_out], -1)
    return x + (y * jax.nn.silu(gate)) @ w_out


def odd_layer(x, mem_n, mla_rot, v_first, norm, w_in, q_norm, w_qb, kv_norm, w_kvb, mu_shift,
              w0, w2, a0, a2, v0, v2, k_k, k_a, r_k, lnx_w, lnx_b, w_mem_kv, w_out):
    h = rms_norm(x, norm)
    d_cols = mu_shift.shape[0]
    cq, ckv, krope, dproj, qm, gate = split_cols(
        h @ w_in, [C_Q_RANK, C_KV_RANK, C_ROPE, d_cols, MEM_WIDTH, ODD_MIX])
    c_out = mla_attention(cq, ckv, krope, mla_rot, q_norm, w_qb, kv_norm, w_kvb)
    d_out, v_first = rwkv7_time_mix(token_shift(dproj, mu_shift), v_first, w0, w2, a0, a2, v0, v2,
                                    k_k, k_a, r_k, lnx_w, lnx_b)
    m_out = memory_attention(qm, mem_n, w_mem_kv)
    y = jnp.concatenate([c_out, d_out, m_out], -1)
    return x + (y * jax.nn.silu(gate)) @ w_out, v_first


def setup_inputs(seed: int = 0):
    key = jax.random.key(seed)
    keys = iter(jax.random.split(key, 64 + 32 * DEPTH))
    f32 = jnp.float32
    def normal(shape, scale):
        return jax.random.normal(next(keys), shape, f32) * scale
    def gain(n):
        return 1.0 + 0.02 * jax.random.normal(next(keys), (n,), f32)
    def uniform(shape, lo, hi):
        return jax.random.uniform(next(keys), shape, f32, lo, hi)
    out_scale = (2.0 * DEPTH) ** -0.5
    p = {}
    p['x'] = normal((BATCH, SEQ, D_MODEL), 1.0)
    p['mem'] = normal((BATCH, MEM_TOKENS, D_MODEL), 1.0)
    offsets = jax.random.randint(next(keys), (BATCH, 1), 0, 4096, dtype=jnp.int32)
    p['positions'] = jnp.arange(SEQ, dtype=jnp.int32)[None, :] + offsets
    p['mem_norm'] = gain(D_MODEL)
    p['final_norm'] = gain(D_MODEL)
    for layer in range(DEPTH):
        pre = 'l%d_' % layer
        p[pre + 'norm'] = gain(D_MODEL)
        if layer % 2 == 0:
            p[pre + 'w_in'] = normal((D_MODEL, sum(EVEN_COLS)), D_MODEL ** -0.5)
            p[pre + 'w_mem_kv'] = normal((D_MODEL, 2 * MEM_WIDTH), D_MODEL ** -0.5)
            p[pre + 'w_out'] = normal((EVEN_MIX, D_MODEL), EVEN_MIX ** -0.5 * out_scale)
        else:
            vres = layer > 1
            d_cols = 3 * D_WIDTH + D_DECAY_LORA + D_AAA_LORA + (D_MV_LORA if vres else 0)
            odd_cols = C_Q_RANK + C_KV_RANK + C_ROPE + d_cols + MEM_WIDTH + ODD_MIX
            p[pre + 'w_in'] = normal((D_MODEL, odd_cols), D_MODEL ** -0.5)
            p[pre + 'q_norm'] = gain(C_Q_RANK)
            p[pre + 'w_qb'] = normal((C_Q_RANK, C_HEADS * (C_NOPE + C_ROPE)), C_Q_RANK ** -0.5)
            p[pre + 'kv_norm'] = gain(C_KV_RANK)
            p[pre + 'w_kvb'] = normal((C_KV_RANK, C_HEADS * (C_NOPE + C_V)), C_KV_RANK ** -0.5)
            p[pre + 'mu_shift'] = uniform((d_cols,), 0.0, 1.0)
            p[pre + 'w0'] = uniform((D_WIDTH,), -3.0, 1.0)
            p[pre + 'w2'] = normal((D_DECAY_LORA, D_WIDTH), 0.1 * D_DECAY_LORA ** -0.5)
            p[pre + 'a0'] = normal((D_WIDTH,), 0.1)
            p[pre + 'a2'] = normal((D_AAA_LORA, D_WIDTH), 0.1 * D_AAA_LORA ** -0.5)
            if vres:
                p[pre + 'v0'] = 1.0 + normal((D_WIDTH,), 0.1)
                p[pre + 'v2'] = normal((D_MV_LORA, D_WIDTH), 0.1 * D_MV_LORA ** -0.5)
            p[pre + 'k_k'] = 0.85 + normal((D_WIDTH,), 0.02)
            p[pre + 'k_a'] = 1.0 + normal((D_WIDTH,), 0.02)
            p[pre + 'r_k'] = normal((D_HEADS, D_HEAD), 0.1)
            p[pre + 'lnx_w'] = gain(D_WIDTH)
            p[pre + 'lnx_b'] = normal((D_WIDTH,), 0.02)
            p[pre + 'w_mem_kv'] = normal((D_MODEL, 2 * MEM_WIDTH), D_MODEL ** -0.5)
            p[pre + 'w_out'] = normal((ODD_MIX, D_MODEL), ODD_MIX ** -0.5 * out_scale)
    return p


def reference(x, mem, positions, mem_norm, final_norm,
              l0_norm, l0_w_in, l0_w_mem_kv, l0_w_out,
              l1_norm, l1_w_in, l1_q_norm, l1_w_qb, l1_kv_norm, l1_w_kvb, l1_mu_shift,
              l1_w0, l1_w2, l1_a0, l1_a2, l1_k_k, l1_k_a, l1_r_k, l1_lnx_w, l1_lnx_b,
              l1_w_mem_kv, l1_w_out,
              l2_norm, l2_w_in, l2_w_mem_kv, l2_w_out,
              l3_norm, l3_w_in, l3_q_norm, l3_w_qb, l3_kv_norm, l3_w_kvb, l3_mu_shift,
              l3_w0, l3_w2, l3_a0, l3_a2, l3_v0, l3_v2, l3_k_k, l3_k_a, l3_r_k, l3_lnx_w, l3_lnx_b,
              l3_w_mem_kv, l3_w_out):
    rot = rope_angles(positions, ROT_DIM, ROPE_THETA)
    ret_rot = rope_angles(positions, B_QK_DIM, RET_THETA)
    mla_rot = rope_angles(positions, C_ROPE, ROPE_THETA)
    mem_n = rms_norm(mem, mem_norm)
    layers = (
        dict(norm=l0_norm, w_in=l0_w_in, w_mem_kv=l0_w_mem_kv, w_out=l0_w_out),
        dict(norm=l1_norm, w_in=l1_w_in, q_norm=l1_q_norm, w_qb=l1_w_qb, kv_norm=l1_kv_norm,
             w_kvb=l1_w_kvb, mu_shift=l1_mu_shift, w0=l1_w0, w2=l1_w2, a0=l1_a0, a2=l1_a2,
             v0=None, v2=None, k_k=l1_k_k, k_a=l1_k_a, r_k=l1_r_k, lnx_w=l1_lnx_w, lnx_b=l1_lnx_b,
             w_mem_kv=l1_w_mem_kv, w_out=l1_w_out),
        dict(norm=l2_norm, w_in=l2_w_in, w_mem_kv=l2_w_mem_kv, w_out=l2_w_out),
        dict(norm=l3_norm, w_in=l3_w_in, q_norm=l3_q_norm, w_qb=l3_w_qb, kv_norm=l3_kv_norm,
             w_kvb=l3_w_kvb, mu_shift=l3_mu_shift, w0=l3_w0, w2=l3_w2, a0=l3_a0, a2=l3_a2,
             v0=l3_v0, v2=l3_v2, k_k=l3_k_k, k_a=l3_k_a, r_k=l3_r_k, lnx_w=l3_lnx_w, lnx_b=l3_lnx_b,
             w_mem_kv=l3_w_mem_kv, w_out=l3_w_out),
    )
    v_first = None
    for i in range(DEPTH):
        if i % 2 == 0:
            x = even_layer(x, mem_n, rot, ret_rot, **layers[i])
        else:
            x, v_first = odd_layer(x, mem_n, mla_rot, v_first, **layers[i])
    return rms_norm(x, final_norm)
```

```python
import contextlib
import math
import numpy as np
import ml_dtypes
import concourse.bass as bass
import concourse.mybir as mybir
from concourse.bass_utils import run_bass_kernel_spmd

F32 = mybir.dt.float32
BF16 = mybir.dt.bfloat16
I32 = mybir.dt.int32
ALU = mybir.AluOpType
AF = mybir.ActivationFunctionType
AX = mybir.AxisListType
NPBF = ml_dtypes.bfloat16

PE, ACT, DVE, POOL, SP = 0, 1, 2, 3, 4
SEM_ROT = 30000
NCORES = 8
SEQ = 2048
NT = 16
DM = 1024
EPS = 1e-6


class Buf:
    __slots__ = ("w", "r")

    def __init__(self):
        self.w = None
        self.r = []


class T:
    def __init__(self, t, b=None, excl=False):
        self.t = t
        self.b = b if b is not None else Buf()
        self.excl = excl

    def __getitem__(self, idx):
        return self.t[idx]


def _b(x):
    return x.b if isinstance(x, T) else x


class KB:
    def __init__(self, n_dma_slots=14):
        self.nc = bass.Bass("TRN2", target_bir_lowering=False)
        nc = self.nc
        self.es = contextlib.ExitStack()
        self.eng = [nc.tensor, nc.scalar, nc.vector, nc.gpsimd, nc.sync]
        self.esem = []
        self.ecnt = [0] * 5
        self.eepoch = [0] * 5
        for e in range(5):
            self.esem.append([self.es.enter_context(nc.semaphore("e%d_0" % e))])
        self.waited = [dict() for _ in range(5)]
        self.dsem = [self.es.enter_context(nc.semaphore("d%d" % i)) for i in range(n_dma_slots)]
        self.dcnt = [0] * n_dma_slots
        self.dnext = 0
        self.ninst = 0
        self.nwait = 0

    def sb(self, name, shape, dt, es=None):
        self.nsb = getattr(self, "nsb", 0) + 1
        return T((es or self.es).enter_context(self.nc.sbuf_tensor("%s_%d" % (name, self.nsb), list(shape), dt)))

    def ps(self, name, shape, dt=F32):
        return T(self.es.enter_context(self.nc.psum_tensor(name, list(shape), dt)), excl=True)

    def dram(self, name, shape, dt, kind="Internal"):
        return self.nc.dram_tensor(name, list(shape), dt, kind=kind).ap()

    def _wait(self, e, dep):
        kind, idx, val = dep
        if kind == "e" and idx[0] == e and e == PE:
            return
        key = (kind, idx)
        if self.waited[e].get(key, 0) >= val:
            return
        self.waited[e][key] = val
        sem = self.esem[idx[0]][idx[1]] if kind == "e" else self.dsem[idx]
        self.eng[e].wait_ge(sem, val)
        self.nwait += 1

    def _deps(self, e, reads, writes):
        for b in reads:
            if b.w is not None:
                self._wait(e, b.w)
        for b in writes:
            if b.w is not None:
                self._wait(e, b.w)
            for d in b.r:
                self._wait(e, d)

    def _mark(self, tag, reads, writes):
        for b in writes:
            b.w = tag
            b.r = []
        for b in reads:
            if b.w is tag:
                continue
            b.r = [d for d in b.r if not (d[0] == tag[0] and d[1] == tag[1])]
            b.r.append(tag)

    def op(self, e, fn, reads=(), writes=()):
        writes = [_b(x) for x in writes] + [x.b for x in reads if isinstance(x, T) and x.excl]
        reads = [_b(x) for x in reads]
        self._deps(e, reads, writes)
        inst = fn()
        if self.ecnt[e] >= SEM_ROT:
            self.eepoch[e] += 1
            self.ecnt[e] = 0
            self.esem[e].append(self.es.enter_context(
                self.nc.semaphore("e%d_%d" % (e, self.eepoch[e]))))
        self.ecnt[e] += 1
        inst.then_inc(self.esem[e][self.eepoch[e]], 1)
        tag = ("e", (e, self.eepoch[e]), self.ecnt[e])
        self._mark(tag, reads, writes)
        self.ninst += 1
        return inst

    def dma(self, out, in_, reads=(), writes=(), q=SP, **kw):
        reads = [_b(x) for x in reads]
        writes = [_b(x) for x in writes]
        e = q
        self._deps(e, reads, writes)
        s = self.dnext
        self.dnext = (self.dnext + 1) % len(self.dsem)
        if self.dcnt[s] > 0:
            self._wait(e, ("d", s, self.dcnt[s]))
        inst = self.eng[e].dma_start(out=out, in_=in_, **kw)
        self.dcnt[s] += 16
        inst.then_inc(self.dsem[s], 16)
        tag = ("d", s, self.dcnt[s])
        self._mark(tag, reads, writes)
        self.ninst += 1
        return inst

    def barrier(self):
        for e in range(5):
            for o in range(5):
                if o != e and (self.ecnt[o] > 0 or self.eepoch[o] > 0):
                    if self.ecnt[o] > 0:
                        self._wait(e, ("e", (o, self.eepoch[o]), self.ecnt[o]))
                    else:
                        self._wait(e, ("e", (o, self.eepoch[o] - 1), SEM_ROT))
            for s in range(len(self.dsem)):
                if self.dcnt[s] > 0:
                    self._wait(e, ("d", s, self.dcnt[s]))

    def finish(self):
        self.barrier()
        self.es.close()


def _consts():
    c = {}
    c["ident"] = np.eye(128, dtype=np.float32).astype(NPBF)
    kk = np.arange(128)[:, None, None]
    dl = np.arange(16)[None, :, None]
    qq = np.arange(128)[None, None, :]
    d = dl * 128 + qq - kk
    m = ((d >= 0) & (d <= 128)).astype(np.float32)
    m += ((d >= 0) & (d % 4 == 0) & (d <= 512)).astype(np.float32)
    m += ((d >= 0) & (d % 16 == 0) & (d <= 2047 * 16)).astype(np.float32)
    c["maskA"] = m.astype(NPBF)
    c["causal"] = (np.arange(128)[None, :] >= np.arange(128)[:, None]).astype(np.float32).astype(NPBF)
    g = 1.0 - np.exp2(-5.0 - np.arange(4, dtype=np.float64))
    lg = np.log(g)
    kq = (np.arange(128)[None, :] - np.arange(128)[:, None]).astype(np.float64)
    DT = np.where(kq[:, None, :] >= 0, np.exp(np.maximum(kq, 0)[:, None, :] * lg[None, :, None]), 0.0) / 8.0
    c["retDT"] = DT.astype(np.float32)
    FS = np.zeros((128, 2, 128), np.float64)
    G128 = np.zeros((128, 2), np.float64)
    for p in range(2):
        for s in range(2):
            h = 2 * p + s
            FS[64 * s:64 * s + 64, p, :] = np.exp((np.arange(128) + 1.0) * lg[h])[None, :] / 8.0
            G128[64 * s:64 * s + 64, p] = np.exp(128.0 * lg[h])
    c["retFS"] = FS.astype(np.float32)
    c["retG"] = G128.astype(np.float32)
    c["retTE"] = np.exp((127.0 - np.arange(128))[:, None] * lg[None, :]).astype(np.float32)
    def inv(dim, theta):
        return np.exp(-math.log(theta) * np.arange(0, dim, 2, dtype=np.float32) / dim).astype(np.float32)
    iv = np.concatenate([inv(16, 500000.0), inv(64, 10000.0), inv(32, 500000.0)])
    c["ropeinv"] = np.tile(iv[None, :], (128, 1)).astype(np.float32)
    j = np.arange(64)
    t = np.arange(64)
    strict = (t[None, :] > j[:, None]).astype(np.float32)
    incl = (t[None, :] >= j[:, None]).astype(np.float32)
    mS = np.zeros((128, 128), np.float32)
    for seg in range(2):
        mS[seg * 64:(seg + 1) * 64, 0:64] = strict
        mS[seg * 64:(seg + 1) * 64, 64:128] = incl
    mN = np.zeros((128, 128), np.float32)
    mM = np.zeros((128, 128), np.float32)
    for cc in range(2):
        mN[cc * 64:(cc + 1) * 64, cc * 64:(cc + 1) * 64] = strict.T
        mM[cc * 64:(cc + 1) * 64, cc * 64:(cc + 1) * 64] = strict
    c["maskRW"] = np.concatenate([mS, mS, mN, mM], 1).astype(NPBF)
    ob = np.zeros((128, 128), np.float32)
    ob[:64, :64] = 1.0
    ob[64:, 64:] = 1.0
    c["onesblk"] = ob
    rst = np.ones((128, 128), np.float32)
    rst[:, 0] = 0.0
    rst[:, 64] = 0.0
    c["scanrst"] = rst
    return c


CONST_SHAPES = {
    "ident": ([128, 128], BF16), "maskA": ([128, 16, 128], BF16), "causal": ([128, 128], BF16),
    "retDT": ([128, 4, 128], F32), "retFS": ([128, 2, 128], F32), "retG": ([128, 2], F32),
    "retTE": ([128, 4], F32), "ropeinv": ([128, 56], F32),
    "maskRW": ([128, 512], BF16), "onesblk": ([128, 128], F32), "scanrst": ([128, 128], F32),
}

ODD_W = 7184
O_QT, O_QMT, O_SG, O_AR, O_BK, O_BKP, O_VT, O_BON, O_PC, O_BT = 0, 1024, 1280, 2560, 3584, 4608, 5632, 6144, 6656, 6672
NCOLS = 38
C_MU, C_W0, C_A0, C_V0, C_KK, C_KA, C_RK = 0, 14, 18, 22, 26, 30, 34
LN_EPS = 64e-5

EVEN_W = 3328
E_QAT, E_QRT, E_KRT, E_KD, E_VR, E_QMT, E_SG = 0, 512, 768, 1024, 1280, 1792, 2048


class Net:
    def __init__(self, n_layers=4, final_norm=True, stop=None):
        self.stop = stop
        self.n_layers = n_layers
        self.final_norm = final_norm
        self.k = KB()
        k = self.k
        nc = k.nc
        self.nc = nc
        D = lambda name, shape, dt=F32: k.dram(name, shape, dt, "ExternalInput")
        self.x_in = D("x", [2, SEQ, DM])
        self.mem_in = D("mem", [2, 256, DM])
        self.pos_in = D("pos_col", [128, 32], I32)
        self.out = k.dram("out", [2, SEQ, DM], F32, "ExternalOutput")
        self.cst = {n: D("c_" + n, s, dt) for n, (s, dt) in CONST_SHAPES.items()}
        self.w = {}
        self.w["mem_norm_col"] = D("mem_norm_col", [128, 8])
        self.w["final_norm"] = D("final_norm", [1, DM])
        for l in range(4):
            p = "l%d_" % l
            self.w[p + "norm_col"] = D(p + "norm_col", [128, 8])
            self.w[p + "w_mem_kv"] = D(p + "w_mem_kv", [DM, 512])
            self.w[p + "w_out"] = D(p + "w_out", [1280, DM])
            if l % 2 == 0:
                self.w[p + "w_in"] = D(p + "w_in", [DM, 4096])
            else:
                dc = 1664 if l == 1 else 1696
                self.w[p + "w_in"] = D(p + "w_in", [DM, 416 + dc + 256 + 1280])
                self.w[p + "qn_col"] = D(p + "qn_col", [128, 2])
                self.w[p + "kvn_col"] = D(p + "kvn_col", [128, 1])
                self.w[p + "w_qb"] = D(p + "w_qb", [256, 768])
                self.w[p + "w_kvb"] = D(p + "w_kvb", [128, 1024])
                self.w[p + "cols"] = D(p + "cols", [128, NCOLS])
                self.w[p + "w2"] = D(p + "w2", [64, 512])
                self.w[p + "a2"] = D(p + "a2", [64, 512])
                if l == 3:
                    self.w[p + "v2"] = D(p + "v2", [32, 512])
                self.w[p + "lnx_w"] = D(p + "lnx_w", [1, 512])
                self.w[p + "lnx_b"] = D(p + "lnx_b", [1, 512])
        self.s_kTM = k.dram("s_kTM", [2, 96, 8, SEQ], BF16)
        self.s_vf = k.dram("s_vf", [2, NT, 128, 512], F32)
        self.s_pc = k.dram("s_pc", [2, NT, 128, 8], F32)
        self.s_pk = k.dram("s_pk", [2, NT, 128, ODD_W], BF16)
        self.s_kT = k.dram("s_kT", [2, 128, 4, SEQ], BF16)
        self.s_vA = k.dram("s_vA", [2, NT, 128, 8 * 65], BF16)
        self.s_memT = k.dram("s_memT", [2, 128, 8, 256], BF16)
        self.xbuf = [[Buf() for _ in range(NT)] for _ in range(2)]
        self.P = [k.ps("bank%d" % i, [128, 512], F32) for i in range(8)]
        self.ident = k.sb("ident", [128, 128], BF16)
        self.rot = k.sb("rot", [128, 2 * NT, 2, 56], F32)
        k.dma(self.ident[:], self.cst["ident"][:, :], writes=[self.ident])
        self._rope_tables()
        if stop == "rope":
            k.finish(); return
        self._mem_prep()
        if stop == "mem":
            k.finish(); return
        for l in range(n_layers):
            if l % 2 == 0:
                self._even_layer(l)
            else:
                self._odd_layer(l)
        k.finish()

    def _rope_tables(self):
        k, nc = self.k, self.nc
        with contextlib.ExitStack() as es:
            pi = k.sb("pos_i", [128, 32], I32, es)
            pf = k.sb("pos_f", [128, 32], F32, es)
            inv = k.sb("ropeinv", [128, 56], F32, es)
            ang = k.sb("ang", [128, 32, 56], F32, es)
            nf = k.sb("nf", [128, 32, 56], F32, es)
            ni = k.sb("ni", [128, 32, 56], I32, es)
            msk = k.sb("msk", [128, 32, 56], F32, es)
            k.dma(pi[:], self.pos_in[:, :], writes=[pi])
            k.dma(inv[:], self.cst["ropeinv"][:, :], writes=[inv])
            k.op(DVE, lambda: nc.vector.tensor_copy(out=pf[:], in_=pi[:]), [pi], [pf])
            k.op(DVE, lambda: nc.vector.tensor_tensor(
                out=ang[:], in0=pf[:].unsqueeze(2).to_broadcast([128, 32, 56]),
                in1=inv[:].unsqueeze(1).to_broadcast([128, 32, 56]), op=ALU.mult), [pf, inv], [ang])
            TWO_PI = 2.0 * math.pi
            C1 = 6.28125
            C2 = TWO_PI - C1

            def reduce_and_sin(shift, dst):
                k.op(DVE, lambda: nc.vector.tensor_scalar(out=nf[:], in0=ang[:], scalar1=1.0 / TWO_PI,
                                                          scalar2=shift / TWO_PI, op0=ALU.mult, op1=ALU.add),
                     [ang], [nf])
                k.op(DVE, lambda: nc.vector.tensor_copy(out=ni[:], in_=nf[:]), [nf], [ni])
                k.op(DVE, lambda: nc.vector.tensor_copy(out=nf[:], in_=ni[:]), [ni], [nf])
                k.op(DVE, lambda: nc.vector.scalar_tensor_tensor(out=msk[:], in0=nf[:], scalar=-C1, in1=ang[:],
                                                                 op0=ALU.mult, op1=ALU.add), [nf, ang], [msk])
                k.op(DVE, lambda: nc.vector.scalar_tensor_tensor(out=msk[:], in0=nf[:], scalar=-C2, in1=msk[:],
                                                                 op0=ALU.mult, op1=ALU.add), [nf, msk], [msk])
                if shift != 0.0:
                    k.op(DVE, lambda: nc.vector.tensor_scalar(out=msk[:], in0=msk[:], scalar1=shift, scalar2=None,
                                                              op0=ALU.add), [msk], [msk])
                k.op(DVE, lambda: nc.vector.tensor_scalar(out=nf[:], in0=msk[:], scalar1=math.pi, scalar2=-TWO_PI,
                                                          op0=ALU.is_gt, op1=ALU.mult), [msk], [nf])
                k.op(DVE, lambda: nc.vector.tensor_tensor(out=msk[:], in0=msk[:], in1=nf[:], op=ALU.add),
                     [msk, nf], [msk])
                k.op(DVE, lambda: nc.vector.tensor_scalar(out=nf[:], in0=msk[:], scalar1=-math.pi, scalar2=TWO_PI,
                                                          op0=ALU.is_lt, op1=ALU.mult), [msk], [nf])
                k.op(DVE, lambda: nc.vector.tensor_tensor(out=msk[:], in0=msk[:], in1=nf[:], op=ALU.add),
                     [msk, nf], [msk])
                k.op(DVE, lambda: nc.vector.tensor_scalar(out=msk[:], in0=msk[:], scalar1=3.1415925, scalar2=-3.1415925,
                                                          op0=ALU.min, op1=ALU.max), [msk], [msk])
                k.op(ACT, lambda: nc.scalar.activation(out=dst, in_=msk[:], func=AF.Sin), [msk], [self.rot])

            reduce_and_sin(math.pi / 2.0, self.rot[:, :, 0, :])
            reduce_and_sin(0.0, self.rot[:, :, 1, :])
            k.barrier()

    def _mem_prep(self):
        k, nc = self.k, self.nc
        P = self.P
        with contextlib.ExitStack() as es:
            gcol = k.sb("memg", [128, 8], F32, es)
            k.dma(gcol[:], self.w["mem_norm_col"][:, :], writes=[gcol])
            mt = k.sb("mem_t", [128, DM], F32, es)
            junk = k.sb("mem_junk", [128, DM], BF16, es)
            st = k.sb("mem_st", [128, 4], F32, es)
            hb = k.sb("mem_h", [128, DM], BF16, es)
            mT = k.sb("mem_T", [128, 8, 256], BF16, es)
            pb = P[2][:].bitcast(BF16)
            for b in range(2):
                for mb in range(2):
                    k.dma(mt[:], self.mem_in[b, mb * 128:(mb + 1) * 128, :], writes=[mt])
                    self._rms_to_bf16(mt, junk, st, hb, DM)
                    for c in range(8):
                        k.op(PE, lambda: nc.tensor.transpose(out=pb[:, c * 128:(c + 1) * 128],
                                                             in_=hb[:, c * 128:(c + 1) * 128], identity=self.ident[:]),
                             [hb, self.ident], [P[2]])
                    for c in range(8):
                        k.op(DVE, lambda: nc.vector.tensor_scalar(out=mT[:, c, mb * 128:(mb + 1) * 128],
                                                                  in0=pb[:, c * 128:(c + 1) * 128],
                                                                  scalar1=gcol[:, c:c + 1], scalar2=None, op0=ALU.mult),
                             [P[2], gcol], [mT])
                k.dma(self.s_memT[b], mT[:], reads=[mT], writes=[])
            k.barrier()

    def _rms_to_bf16(self, xt, junk, st, hb, n, eps=EPS):
        k, nc = self.k, self.nc
        k.op(ACT, lambda: nc.scalar.activation(out=junk[:, 0:n], in_=xt[:, 0:n], func=AF.Square, accum_out=st[:, 0:1]),
             [xt], [junk, st])
        k.op(ACT, lambda: nc.scalar.activation(out=st[:, 1:2], in_=st[:, 0:1], func=AF.Sqrt, scale=1.0 / n, bias=eps),
             [st], [st])
        k.op(DVE, lambda: nc.vector.reciprocal(out=st[:, 2:3], in_=st[:, 1:2]), [st], [st])
        k.op(DVE, lambda: nc.vector.tensor_scalar(out=hb[:, 0:n], in0=xt[:, 0:n], scalar1=st[:, 2:3], scalar2=None,
                                                  op0=ALU.mult), [xt, st], [hb])

    def _load_w(self, es, name, dram, rows, cols, gcol=None, wb=None):
        k, nc = self.k, self.nc
        nch = rows // 128
        if wb is None:
            wb = k.sb(name, [128, nch, cols], BF16, es)
        CW = 2048
        i = 0
        for c in range(nch):
            for c0 in range(0, cols, CW):
                cw = min(CW, cols - c0)
                stg = self.stage[i % 2]
                k.dma(stg[:, 0:cw], dram[c * 128:(c + 1) * 128, c0:c0 + cw], writes=[stg])
                eng = POOL if i % 2 == 0 else ACT
                if gcol is None:
                    if eng == POOL:
                        k.op(POOL, lambda: nc.gpsimd.tensor_copy(out=wb[:, c, c0:c0 + cw], in_=stg[:, 0:cw]), [stg], [wb])
                    else:
                        k.op(ACT, lambda: nc.scalar.copy(out=wb[:, c, c0:c0 + cw], in_=stg[:, 0:cw]), [stg], [wb])
                else:
                    if eng == POOL:
                        k.op(POOL, lambda: nc.gpsimd.tensor_scalar(out=wb[:, c, c0:c0 + cw], in0=stg[:, 0:cw],
                                                                   scalar1=gcol[:, c:c + 1], scalar2=1.0,
                                                                   op0=ALU.mult, op1=ALU.mult), [stg, gcol], [wb])
                    else:
                        k.op(ACT, lambda: nc.scalar.activation(out=wb[:, c, c0:c0 + cw], in_=stg[:, 0:cw],
                                                               func=AF.Copy, scale=gcol[:, c:c + 1]), [stg, gcol], [wb])
                i += 1
        return wb

    def _rotary(self, xv, tile_idx, f0, nf, tA, tB, out1, out2, reads, writes, nh):
        k, nc = self.k, self.nc
        cs = self.rot[:, tile_idx, 0, f0:f0 + nf].unsqueeze(1).unsqueeze(1).to_broadcast([128, nh, 2, nf])
        sn = self.rot[:, tile_idx, 1, f0:f0 + nf].unsqueeze(1).unsqueeze(1).to_broadcast([128, nh, 2, nf])
        a = tA[:, 0:nh * 2 * nf].rearrange("p (h t f) -> p h t f", h=nh, t=2)
        bq = tB[:, 0:nh * 2 * nf].rearrange("p (h t f) -> p h t f", h=nh, t=2)
        k.op(DVE, lambda: nc.vector.tensor_tensor(out=a, in0=xv, in1=cs, op=ALU.mult), reads + [self.rot], [tA])
        k.op(DVE, lambda: nc.vector.tensor_tensor(out=bq, in0=xv, in1=sn, op=ALU.mult), reads + [self.rot], [tB])
        k.op(POOL, lambda: nc.gpsimd.tensor_tensor(out=out1, in0=a[:, :, 0, :], in1=bq[:, :, 1, :], op=ALU.subtract),
             [tA, tB], writes)
        k.op(POOL, lambda: nc.gpsimd.tensor_tensor(out=out2, in0=a[:, :, 1, :], in1=bq[:, :, 0, :], op=ALU.add),
             [tA, tB], writes)

    def _mem_kv(self, es, wmem, tagp, pre=None):
        k, nc = self.k, self.nc
        P = self.P
        if pre is None:
            kmT = k.sb(tagp + "kmT", [128, 2, 2, 256], BF16, es)
            vm = k.sb(tagp + "vm", [128, 2, 2, 4, 65], BF16, es)
        else:
            kmT, vm = pre
        k.op(POOL, lambda: nc.gpsimd.memset(vm[:], 1.0), [], [vm])
        with contextlib.ExitStack() as es2:
            mT = k.sb(tagp + "memT", [128, 8, 256], BF16, es2)
            for b in range(2):
                k.dma(mT[:], self.s_memT[b], writes=[mT])
                for p in range(2):
                    for c in range(8):
                        k.op(PE, lambda: nc.tensor.matmul(P[0][:, p * 256:(p + 1) * 256],
                                                          lhsT=wmem[:, c, p * 128:(p + 1) * 128], rhs=mT[:, c, :],
                                                          start=(c == 0), stop=(c == 7)), [wmem, mT], [P[0]])
                k.op(ACT, lambda: nc.scalar.copy(out=kmT[:, b, :, :].rearrange("p a m -> p (a m)"), in_=P[0][:, 0:512]),
                     [P[0]], [kmT])
                for mb in range(2):
                    for c in range(8):
                        k.op(PE, lambda: nc.tensor.matmul(P[1][:, mb * 256:(mb + 1) * 256],
                                                          lhsT=mT[:, c, mb * 128:(mb + 1) * 128], rhs=wmem[:, c, 256:512],
                                                          start=(c == 0), stop=(c == 7)), [wmem, mT], [P[1]])
                for mb in range(2):
                    k.op(DVE, lambda: nc.vector.tensor_copy(
                        out=vm[:, b, mb, :, 0:64],
                        in_=P[1][:, mb * 256:(mb + 1) * 256].rearrange("p (h d) -> p h d", h=4)), [P[1]], [vm])
            k.barrier()
        return kmT, vm

    def _even_layer(self, l):
        k, nc = self.k, self.nc
        P = self.P
        pre = "l%d_" % l
        last = (l == self.n_layers - 1)
        src = self.x_in if l == 0 else self.out
        pb2 = P[2][:].bitcast(BF16)
        with contextlib.ExitStack() as es:
            self.stage = [k.sb("stgA", [128, 2048], F32, es), k.sb("stgB", [128, 2048], F32, es)]
            gcol = k.sb("gcol", [128, 8], F32, es)
            k.dma(gcol[:], self.w[pre + "norm_col"][:, :], writes=[gcol])
            wb = self._load_w(es, "wb", self.w[pre + "w_in"], DM, 4096, gcol)
            retTE = k.sb("retTE", [128, 4], F32, es)
            k.dma(retTE[:], self.cst["retTE"][:, :], writes=[retTE])
            xt = [k.sb("xt%d" % i, [128, DM], F32, es) for i in range(2)]
            junk = k.sb("junk", [128, DM], BF16, es)
            st = k.sb("st", [128, 4], F32, es)
            hb = k.sb("hb", [128, DM], BF16, es)
            hT = k.sb("hT", [128, 8, 128], BF16, es)
            pk = [k.sb("pk%d" % i, [128, EVEN_W], BF16, es) for i in range(2)]
            qk_b = k.sb("qk_b", [128, 2, 8, 64], BF16, es)
            qkr_b = k.sb("qkr_b", [128, 8, 64], BF16, es)
            vA_t = [k.sb("vA_t%d" % i, [128, 8, 65], BF16, es) for i in range(2)]
            kT_t = [k.sb("kT_t%d" % i, [128, 4, 128], BF16, es) for i in range(2)]
            tA = k.sb("rotA", [128, 512], F32, es)
            tB = k.sb("rotB", [128, 512], F32, es)
            for i in range(2):
                k.op(POOL, lambda: nc.gpsimd.memset(vA_t[i][:], 1.0), [], [vA_t[i]])
            tiles = [(b, i) for b in range(2) for i in range(NT)]
            k.dma(xt[0][:], src[0, 0:128, :], reads=[self.xbuf[0][0]], writes=[xt[0]])
            for ti, (b, i) in enumerate(tiles):
                X = xt[ti % 2]
                PK = pk[ti % 2]
                VA = vA_t[ti % 2]
                KT = kT_t[ti % 2]
                if ti + 1 < len(tiles):
                    nb, ni_ = tiles[ti + 1]
                    k.dma(xt[(ti + 1) % 2][:], src[nb, ni_ * 128:(ni_ + 1) * 128, :],
                          reads=[self.xbuf[nb][ni_]], writes=[xt[(ti + 1) % 2]])
                tix = b * NT + i
                self._rms_to_bf16(X, junk, st, hb, DM)
                for c in range(8):
                    k.op(PE, lambda: nc.tensor.transpose(out=pb2[:, c * 128:(c + 1) * 128],
                                                         in_=hb[:, c * 128:(c + 1) * 128], identity=self.ident[:]),
                         [hb, self.ident], [P[2]])
                k.op(ACT, lambda: nc.scalar.copy(out=hT[:].rearrange("p c t -> p (c t)"), in_=pb2[:, 0:1024]),
                     [P[2]], [hT])
                for n in range(8):
                    bk = P[n % 2]
                    for c in range(8):
                        k.op(PE, lambda: nc.tensor.matmul(bk[:, :], lhsT=hT[:, c, :], rhs=wb[:, c, n * 512:(n + 1) * 512],
                                                          start=(c == 0), stop=(c == 7)), [hT, wb], [bk])
                    if n in (0, 1):
                        dst = qk_b[:, n, :, :]
                        k.op(ACT, lambda: nc.scalar.copy(out=dst, in_=bk[:, :].rearrange("p (h d) -> p h d", h=8)),
                             [bk], [qk_b])
                        xv = bk[:, :].rearrange("p (h d) -> p h d", h=8)[:, :, 0:16].rearrange(
                            "p h (t f) -> p h t f", t=2)
                        self._rotary(xv, tix, 0, 8, tA, tB, qk_b[:, n, :, 0:8], qk_b[:, n, :, 8:16], [bk], [qk_b], 8)
                    elif n == 2:
                        k.op(ACT, lambda: nc.scalar.copy(out=VA[:, :, 0:64],
                                                         in_=bk[:, :].rearrange("p (h d) -> p h d", h=8)), [bk], [VA])
                    elif n == 3:
                        xv = bk[:, :].rearrange("p (h t f) -> p h t f", h=8, t=2)
                        self._rotary(xv, tix, 8, 32, tA, tB, qkr_b[:, :, 0:32], qkr_b[:, :, 32:64], [bk], [qkr_b], 8)
                    elif n == 4:
                        k.op(ACT, lambda: nc.scalar.copy(out=PK[:, E_VR:E_VR + 512], in_=bk[:, :]), [bk], [PK])
                    elif n == 5:
                        k.op(DVE, lambda: nc.vector.tensor_copy(out=junk[:, 0:256], in_=bk[:, 0:256]), [bk], [junk])
                        k.op(ACT, lambda: nc.scalar.activation(out=PK[:, E_SG:E_SG + 256], in_=bk[:, 256:512],
                                                               func=AF.Silu), [bk], [PK])
                    else:
                        o = E_SG + 256 + (n - 6) * 512
                        k.op(ACT, lambda: nc.scalar.activation(out=PK[:, o:o + 512], in_=bk[:, :], func=AF.Silu),
                             [bk], [PK])
                qkf = qk_b[:].rearrange("p a h d -> p (a h d)")
                for c in range(8):
                    k.op(PE, lambda: nc.tensor.transpose(out=pb2[:, c * 128:(c + 1) * 128],
                                                         in_=qkf[:, c * 128:(c + 1) * 128], identity=self.ident[:]),
                         [qk_b, self.ident], [P[2]])
                k.op(ACT, lambda: nc.scalar.copy(out=PK[:, E_QAT:E_QAT + 512], in_=pb2[:, 0:512]), [P[2]], [PK])
                k.op(DVE, lambda: nc.vector.tensor_copy(out=KT[:].rearrange("p a t -> p (a t)"), in_=pb2[:, 512:1024]),
                     [P[2]], [KT])
                k.op(DVE, lambda: nc.vector.tensor_tensor(
                    out=PK[:, E_KD:E_KD + 256].rearrange("p (h d) -> p h d", h=4), in0=qkr_b[:, 4:8, :],
                    in1=retTE[:].unsqueeze(2).to_broadcast([128, 4, 64]), op=ALU.mult), [qkr_b, retTE], [PK])
                qrf = qkr_b[:].rearrange("p h d -> p (h d)")
                for c in range(4):
                    k.op(PE, lambda: nc.tensor.transpose(out=pb2[:, c * 128:(c + 1) * 128],
                                                         in_=qrf[:, c * 128:(c + 1) * 128], identity=self.ident[:]),
                         [qkr_b, self.ident], [P[2]])
                for c in range(2):
                    k.op(PE, lambda: nc.tensor.transpose(out=pb2[:, (4 + c) * 128:(5 + c) * 128],
                                                         in_=junk[:, c * 128:(c + 1) * 128], identity=self.ident[:]),
                         [junk, self.ident], [P[2]])
                k.op(ACT, lambda: nc.scalar.copy(out=PK[:, E_QRT:E_QRT + 512], in_=pb2[:, 0:512]), [P[2]], [PK])
                k.op(DVE, lambda: nc.vector.tensor_copy(out=PK[:, E_QMT:E_QMT + 256], in_=pb2[:, 512:768]), [P[2]], [PK])
                k.dma(self.s_pk[b, i, :, 0:EVEN_W], PK[:], reads=[PK])
                k.dma(self.s_kT[b, :, :, i * 128:(i + 1) * 128], KT[:], reads=[KT])
                k.dma(self.s_vA[b, i], VA[:].rearrange("p h d -> p (h d)"), reads=[VA])
            k.barrier()
        if self.stop == "p1":
            return
        with contextlib.ExitStack() as es:
            self.stage = [k.sb("stgA2", [128, 2048], F32, es), k.sb("stgB2", [128, 2048], F32, es)]
            wout = self._load_w(es, "wout", self.w[pre + "w_out"], 1280, DM)
            wmem = self._load_w(es, "wmem", self.w[pre + "w_mem_kv"], DM, 512)
            kmT, vm = self._mem_kv(es, wmem, "e")
            maskA = k.sb("maskA", [128, 16, 128], BF16, es)
            retDT = k.sb("retDT", [128, 4, 128], F32, es)
            retFS = k.sb("retFS", [128, 2, 128], F32, es)
            retG = k.sb("retG", [128, 2], F32, es)
            k.dma(maskA[:], self.cst["maskA"], writes=[maskA])
            k.dma(retDT[:], self.cst["retDT"], writes=[retDT])
            k.dma(retFS[:], self.cst["retFS"], writes=[retFS])
            k.dma(retG[:], self.cst["retG"], writes=[retG])
            if last and self.final_norm:
                gfin = k.sb("gfin", [128, DM], F32, es)
                k.dma(gfin[:], self.w["final_norm"][0:1, :].broadcast_to([128, DM]), writes=[gfin])
            kT = k.sb("kTc", [128, 4, SEQ], BF16, es)
            vA = k.sb("vAc", [128, NT, 8, 65], BF16, es)
            xt = [k.sb("xt2_%d" % i, [128, DM], F32, es) for i in range(2)]
            pk = [k.sb("pk2_%d" % i, [128, EVEN_W], BF16, es) for i in range(2)]
            pT = [[k.sb("pT%d_%d" % (i, j), [128, 4, 128], BF16, es) for j in range(2)] for i in range(2)]
            SBANKS = [(P[3], P[4]), (P[0], P[1])]
            sTb = k.sb("sTb", [128, 4, 128], BF16, es)
            qs = k.sb("qs", [128, 2, 128], BF16, es)
            st_f = k.sb("st_f", [128, 2, 128], F32, es)
            st_b = k.sb("st_b", [128, 2, 128], BF16, es)
            rec = k.sb("rec", [128, 16], F32, es)
            y = k.sb("y", [128, 1280], F32, es)
            ob = k.sb("ob", [128, 512], F32, es)
            sq = k.sb("sq", [128, 512], F32, es)
            yg = k.sb("yg", [128, 1280], BF16, es)
            ygT = k.sb("ygT", [128, 10, 128], BF16, es)
            xo = k.sb("xo", [128, DM], F32, es)
            junk = k.sb("junk2", [128, DM], BF16, es)
            st = k.sb("st2", [128, 8], F32, es)
            tiles = [(b, i) for b in range(2) for i in range(NT)]

            def prefetch(ti):
                b_, i_ = tiles[ti]
                k.dma(xt[ti % 2][:], src[b_, i_ * 128:(i_ + 1) * 128, :], reads=[self.xbuf[b_][i_]], writes=[xt[ti % 2]])
                k.dma(pk[ti % 2][:], self.s_pk[b_, i_, :, 0:EVEN_W], writes=[pk[ti % 2]])

            prefetch(0)
            sc = 0
            for ti, (b, i) in enumerate(tiles):
                X = xt[ti % 2]
                PK = pk[ti % 2]
                if i == 0:
                    k.dma(kT[:], self.s_kT[b], writes=[kT])
                    k.dma(vA[:].rearrange("p i h d -> p i (h d)"), self.s_vA[b].rearrange("i p w -> p i w"), writes=[vA])
                if ti + 1 < len(tiles):
                    prefetch(ti + 1)
                qaT = PK[:, E_QAT:E_QAT + 512].rearrange("p (a t) -> p a t", a=4)
                qrT = PK[:, E_QRT:E_QRT + 256].rearrange("p (a t) -> p a t", a=2)
                krT = PK[:, E_KRT:E_KRT + 256].rearrange("p (a t) -> p a t", a=2)
                kd = PK[:, E_KD:E_KD + 256].rearrange("p (h d) -> p h d", h=4)
                vr = PK[:, E_VR:E_VR + 512].rearrange("p (h e) -> p h e", h=4)
                qmT = PK[:, E_QMT:E_QMT + 256].rearrange("p (a t) -> p a t", a=2)
                sg = PK[:, E_SG:E_SG + 1280]
                first = [True, True]

                def qk_a(j, sc_):
                    Sb_ = SBANKS[sc_ % 2]
                    for p_ in range(4):
                        for s_ in range(2):
                            k.op(PE, lambda: nc.tensor.matmul(Sb_[s_][:, p_ * 128:(p_ + 1) * 128],
                                                              lhsT=kT[64 * s_:64 * s_ + 64, p_, j * 128:(j + 1) * 128],
                                                              rhs=qaT[64 * s_:64 * s_ + 64, p_, :], start=True, stop=True),
                                 [kT, PK], [Sb_[s_]])

                qk_a(0, sc)
                for j in range(i + 1):
                    Sb = SBANKS[sc % 2]
                    ptp = pT[sc % 2]
                    sc += 1
                    if j + 1 <= i:
                        qk_a(j + 1, sc)
                    for s_ in range(2):
                        pt = ptp[s_]
                        k.op(ACT, lambda: nc.scalar.activation(out=pt[:].rearrange("p h q -> p (h q)"), in_=Sb[s_][:, :],
                                                               func=AF.Exp, scale=0.125), [Sb[s_]], [pt])
                        k.op(DVE if s_ == 0 else POOL, lambda: (nc.vector if s_ == 0 else nc.gpsimd).tensor_tensor(
                            out=pt[:], in0=pt[:], in1=maskA[:, i - j, :].unsqueeze(1).to_broadcast([128, 4, 128]),
                            op=ALU.mult), [pt, maskA], [pt])
                        O = P[5 + s_]
                        for p_ in range(4):
                            k.op(PE, lambda: nc.tensor.matmul(O[:, p_ * 65:(p_ + 1) * 65], lhsT=pt[:, p_, :],
                                                              rhs=vA[:, j, 2 * p_ + s_, :], start=first[s_], stop=(j == i),
                                                              skip_group_check=True), [pt, vA], [O])
                            first[s_] = False
                for s_ in range(2):
                    O = P[5 + s_]
                    ov = O[:, 0:260].rearrange("p (h d) -> p h d", h=4)
                    k.op(DVE, lambda: nc.vector.reciprocal(out=rec[:, s_ * 4:s_ * 4 + 4], in_=ov[:, :, 64]), [O], [rec])
                    k.op(DVE, lambda: nc.vector.tensor_tensor(
                        out=y[:, 0:512].rearrange("p (a s d) -> p a s d", a=4, s=2)[:, :, s_, :], in0=ov[:, :, 0:64],
                        in1=rec[:, s_ * 4:s_ * 4 + 4].unsqueeze(2).to_broadcast([128, 4, 64]), op=ALU.mult),
                        [O, rec], [y])
                Sb = SBANKS[sc % 2]
                sc += 1
                for h in range(4):
                    s_, p_ = h % 2, h // 2
                    k.op(PE, lambda: nc.tensor.matmul(Sb[s_][:, p_ * 128:(p_ + 1) * 128], lhsT=krT[64 * s_:64 * s_ + 64, p_, :],
                                                      rhs=qrT[64 * s_:64 * s_ + 64, p_, :], start=True, stop=True),
                         [PK], [Sb[s_]])
                for s_ in range(2):
                    k.op(DVE, lambda: nc.vector.tensor_tensor(
                        out=sTb[:].rearrange("p (a s) q -> p a s q", s=2)[:, :, s_, :],
                        in0=Sb[s_][:, 0:256].rearrange("p (a q) -> p a q", a=2),
                        in1=retDT[:].rearrange("p (a s) q -> p a s q", s=2)[:, :, s_, :], op=ALU.mult),
                        [Sb[s_], retDT], [sTb])
                if i > 0:
                    k.op(POOL, lambda: nc.gpsimd.tensor_tensor(out=qs[:], in0=qrT, in1=retFS[:], op=ALU.mult),
                         [PK, retFS], [qs])
                OB = P[0]
                for h in range(4):
                    s_, p_ = h % 2, h // 2
                    k.op(PE, lambda: nc.tensor.matmul(OB[:, h * 128:(h + 1) * 128], lhsT=sTb[:, h, :], rhs=vr[:, h, :],
                                                      start=True, stop=(i == 0)), [sTb, PK], [OB])
                    if i > 0:
                        k.op(PE, lambda: nc.tensor.matmul(OB[:, h * 128:(h + 1) * 128], lhsT=qs[64 * s_:64 * s_ + 64, p_, :],
                                                          rhs=st_b[64 * s_:64 * s_ + 64, p_, :], start=False, stop=True),
                             [qs, st_b], [OB])
                SU = P[1]
                for h in range(4):
                    s_, p_ = h % 2, h // 2
                    k.op(PE, lambda: nc.tensor.matmul(SU[64 * s_:64 * s_ + 64, p_ * 128:(p_ + 1) * 128], lhsT=kd[:, h, :],
                                                      rhs=vr[:, h, :], start=True, stop=True), [PK], [SU])
                if i == 0:
                    k.op(DVE, lambda: nc.vector.tensor_copy(out=st_f[:].rearrange("p a e -> p (a e)"), in_=SU[:, 0:256]),
                         [SU], [st_f])
                else:
                    for p_ in range(2):
                        k.op(DVE, lambda: nc.vector.scalar_tensor_tensor(
                            out=st_f[:, p_, :], in0=st_f[:, p_, :], scalar=retG[:, p_:p_ + 1],
                            in1=SU[:, p_ * 128:(p_ + 1) * 128], op0=ALU.mult, op1=ALU.add), [st_f, retG, SU], [st_f])
                k.op(ACT, lambda: nc.scalar.copy(out=ob[:], in_=OB[:, :]), [OB], [ob])
                k.op(POOL, lambda: nc.gpsimd.tensor_copy(out=st_b[:], in_=st_f[:]), [st_f], [st_b])
                k.op(POOL, lambda: nc.gpsimd.tensor_tensor(out=sq[:], in0=ob[:], in1=ob[:], op=ALU.mult), [ob], [sq])
                k.op(DVE, lambda: nc.vector.tensor_reduce(out=st[:, 0:4], in_=sq[:].rearrange("p (h e) -> p h e", h=4),
                                                          axis=AX.X, op=ALU.add), [sq], [st])
                k.op(ACT, lambda: nc.scalar.activation(out=st[:, 4:8], in_=st[:, 0:4], func=AF.Sqrt, scale=1.0 / 128,
                                                       bias=EPS), [st], [st])
                k.op(DVE, lambda: nc.vector.reciprocal(out=st[:, 0:4], in_=st[:, 4:8]), [st], [st])
                k.op(DVE, lambda: nc.vector.tensor_tensor(
                    out=y[:, 512:1024].rearrange("p (h e) -> p h e", h=4), in0=ob[:].rearrange("p (h e) -> p h e", h=4),
                    in1=st[:, 0:4].unsqueeze(2).to_broadcast([128, 4, 128]), op=ALU.mult), [ob, st], [y])
                self._mem_attn(b, qmT, PK, kmT, vm, pT, rec, y, sc, SBANKS)
                sc += 2
                self._tail(PK, sg, y, yg, ygT, wout, X, xo, junk, st, b, i, last, gfin if (last and self.final_norm) else None)
            k.barrier()

    def _odd_layer(self, l):
        k, nc = self.k, self.nc
        P = self.P
        pre = "l%d_" % l
        last = (l == self.n_layers - 1)
        src = self.out
        dc = 1664 if l == 1 else 1696
        ntd = (dc + 127) // 128
        c_qm = 416 + dc
        ncols = c_qm + 1536
        vres = (l == 3)
        pb2 = P[2][:].bitcast(BF16)
        tiles = [(b, i) for b in range(2) for i in range(NT)]
        NEG_E = -math.exp(-0.5)
        with contextlib.ExitStack() as es:
            self.stage = [k.sb("stgA", [128, 2048], F32, es), k.sb("stgB", [128, 2048], F32, es)]
            gcol = k.sb("gcol", [128, 8], F32, es)
            qn = k.sb("qn", [128, 2], F32, es)
            kvn = k.sb("kvn", [128, 1], F32, es)
            cols = k.sb("cols", [128, NCOLS], F32, es)
            k.dma(gcol[:], self.w[pre + "norm_col"][:, :], writes=[gcol])
            k.dma(qn[:], self.w[pre + "qn_col"][:, :], writes=[qn])
            k.dma(kvn[:], self.w[pre + "kvn_col"][:, :], writes=[kvn])
            k.dma(cols[:], self.w[pre + "cols"][:, :], writes=[cols])
            wb = self._load_w(es, "wbo", self.w[pre + "w_in"], DM, ncols, gcol)
            wqb = self._load_w(es, "wqb", self.w[pre + "w_qb"], 256, 768, qn)
            wkvb = self._load_w(es, "wkvb", self.w[pre + "w_kvb"], 128, 1024, kvn)
            w2sb = k.sb("w2sb", [128, 512], F32, es)
            a2sb = k.sb("a2sb", [128, 512], F32, es)
            k.dma(w2sb[0:64, :], self.w[pre + "w2"][:, :], writes=[w2sb])
            k.dma(a2sb[64:128, :], self.w[pre + "a2"][:, :], writes=[a2sb])
            if vres:
                v2sb = k.sb("v2sb", [128, 512], F32, es)
                k.dma(v2sb[0:32, :], self.w[pre + "v2"][:, :], writes=[v2sb])
                vft = [k.sb("vft%d" % i, [128, 4, 128], F32, es) for i in range(2)]
            onesblk = k.sb("onesblk", [128, 128], F32, es)
            scanrst = k.sb("scanrst", [128, 128], F32, es)
            k.dma(onesblk[:], self.cst["onesblk"], writes=[onesblk])
            k.dma(scanrst[:], self.cst["scanrst"], writes=[scanrst])
            xt = [k.sb("xt%d" % i, [128, DM], F32, es) for i in range(2)]
            junk = k.sb("junk", [128, DM], BF16, es)
            st = k.sb("st", [128, 8], F32, es)
            hb = k.sb("hb", [128, DM], BF16, es)
            hT = k.sb("hT", [128, 8, 128], BF16, es)
            pk = [k.sb("pk%d" % i, [128, ODD_W], BF16, es) for i in range(2)]
            mq_b = k.sb("mq_b", [128, 512], BF16, es)
            qm_b = k.sb("qm_b", [128, 256], BF16, es)
            cqnT = k.sb("cqnT", [128, 2, 128], BF16, es)
            ckvnT = k.sb("ckvnT", [128, 128], BF16, es)
            kpeT = k.sb("kpeT", [128, 128], BF16, es)
            q_b = k.sb("q_b", [128, 8, 96], BF16, es)
            KTt = [k.sb("KTt%d" % i, [128, 8, 128], BF16, es) for i in range(2)]
            VMt = [k.sb("VMt%d" % i, [128, 8, 65], BF16, es) for i in range(2)]
            tA = k.sb("rotA", [128, 512], F32, es)
            tB = k.sb("rotB", [128, 512], F32, es)
            d_fs = [k.sb("d_f%d" % i, [128, 14, 128], F32, es) for i in range(2)]
            diff = k.sb("diff", [128, 14, 128], F32, es)
            carry = k.sb("carry", [128, 14], F32, es)
            tw = k.sb("tw", [128, 128], F32, es)
            G = [k.sb("g%d" % i, [128, 4, 128], F32, es) for i in range(12)]
            pcs = [k.sb("pcs%d" % i, [128, 8], F32, es) for i in range(2)]
            for i in range(2):
                k.op(POOL, lambda: nc.gpsimd.memset(VMt[i][:], 1.0), [], [VMt[i]])
                k.op(POOL, lambda: nc.gpsimd.memset(pk[i][:], 0.0), [], [pk[i]])
                k.op(POOL, lambda: nc.gpsimd.memset(KTt[i][:], 0.0), [], [KTt[i]])
            for i in range(2):
                k.op(POOL, lambda: nc.gpsimd.memset(d_fs[i][:], 0.0), [], [d_fs[i]])
            k.op(POOL, lambda: nc.gpsimd.memset(mq_b[:], 0.0), [], [mq_b])

            def cb(c0, n=4):
                return cols[:, c0:c0 + n].unsqueeze(2).to_broadcast([128, n, 128])

            def v4(t):
                return t[:].rearrange("p n t -> p (n t)")

            def load(ti):
                b_, i_ = tiles[ti]
                k.dma(xt[ti % 2][:], src[b_, i_ * 128:(i_ + 1) * 128, :], reads=[self.xbuf[b_][i_]], writes=[xt[ti % 2]])

            def load_vf(ti):
                b_, i_ = tiles[ti]
                k.dma(vft[ti % 2][:].rearrange("p n t -> p (n t)"), self.s_vf[b_, i_], writes=[vft[ti % 2]])

            load(0)
            if vres:
                load_vf(0)
            def F_gen(ti, b, i):
                d_f = d_fs[ti % 2]
                X = xt[ti % 2]
                PK = pk[ti % 2]
                KT = KTt[ti % 2]
                VM = VMt[ti % 2]
                if ti + 1 < len(tiles):
                    load(ti + 1)
                tix = b * NT + i
                self._rms_to_bf16(X, junk, st, hb, DM)
                for c in range(8):
                    k.op(PE, lambda: nc.tensor.transpose(out=pb2[:, c * 128:(c + 1) * 128],
                                                         in_=hb[:, c * 128:(c + 1) * 128], identity=self.ident[:]),
                         [hb, self.ident], [P[2]])
                k.op(ACT, lambda: nc.scalar.copy(out=hT[:].rearrange("p c t -> p (c t)"), in_=pb2[:, 0:1024]),
                     [P[2]], [hT])
                yield
                for c in range(8):
                    k.op(PE, lambda: nc.tensor.matmul(P[0][:, 0:416], lhsT=hT[:, c, :], rhs=wb[:, c, 0:416],
                                                      start=(c == 0), stop=(c == 7)), [hT, wb], [P[0]])
                for (c0, c1, so) in ((0, 256, 0), (256, 384, 3)):
                    n_ = c1 - c0
                    k.op(ACT, lambda: nc.scalar.activation(out=junk[:, c0:c1], in_=P[0][:, c0:c1], func=AF.Square,
                                                           accum_out=st[:, so:so + 1]), [P[0]], [junk, st])
                    k.op(ACT, lambda: nc.scalar.activation(out=st[:, so + 1:so + 2], in_=st[:, so:so + 1], func=AF.Sqrt,
                                                           scale=1.0 / n_, bias=EPS), [st], [st])
                    k.op(DVE, lambda: nc.vector.reciprocal(out=st[:, so + 2:so + 3], in_=st[:, so + 1:so + 2]), [st], [st])
                    k.op(DVE, lambda: nc.vector.tensor_scalar(out=mq_b[:, c0:c1], in0=P[0][:, c0:c1],
                                                              scalar1=st[:, so + 2:so + 3], scalar2=None, op0=ALU.mult),
                         [P[0], st], [mq_b])
                xv = P[0][:, 384:416].rearrange("p (h t f) -> p h t f", h=1, t=2)
                self._rotary(xv, tix, 40, 16, tA, tB, mq_b[:, 384:400].unsqueeze(1), mq_b[:, 400:416].unsqueeze(1),
                             [P[0]], [mq_b], 1)
                yield
                for g in range(3):
                    bk = P[(g + 1) % 2]
                    c0 = c_qm + g * 512
                    for c in range(8):
                        k.op(PE, lambda: nc.tensor.matmul(bk[:, :], lhsT=hT[:, c, :], rhs=wb[:, c, c0:c0 + 512],
                                                          start=(c == 0), stop=(c == 7)), [hT, wb], [bk])
                    if g == 0:
                        k.op(DVE, lambda: nc.vector.tensor_copy(out=qm_b[:], in_=bk[:, 0:256]), [bk], [qm_b])
                        k.op(ACT, lambda: nc.scalar.activation(out=PK[:, O_SG:O_SG + 256], in_=bk[:, 256:512],
                                                               func=AF.Silu), [bk], [PK])
                    else:
                        o = O_SG + 256 + (g - 1) * 512
                        k.op(ACT, lambda: nc.scalar.activation(out=PK[:, o:o + 512], in_=bk[:, :], func=AF.Silu),
                             [bk], [PK])
                yield
                for c in range(3):
                    k.op(PE, lambda: nc.tensor.transpose(out=pb2[:, c * 128:(c + 1) * 128],
                                                         in_=mq_b[:, c * 128:(c + 1) * 128], identity=self.ident[:]),
                         [mq_b, self.ident], [P[2]])
                k.op(PE, lambda: nc.tensor.transpose(out=pb2[64:96, 384:512], in_=mq_b[:, 384:416], identity=self.ident[:]),
                     [mq_b, self.ident], [P[2]])
                for c in range(2):
                    k.op(PE, lambda: nc.tensor.transpose(out=pb2[:, (4 + c) * 128:(5 + c) * 128],
                                                         in_=qm_b[:, c * 128:(c + 1) * 128], identity=self.ident[:]),
                         [qm_b, self.ident], [P[2]])
                k.op(ACT, lambda: nc.scalar.copy(out=cqnT[:].rearrange("p c t -> p (c t)"), in_=pb2[:, 0:256]), [P[2]], [cqnT])
                k.op(DVE, lambda: nc.vector.tensor_copy(out=ckvnT[:], in_=pb2[:, 256:384]), [P[2]], [ckvnT])
                k.op(DVE, lambda: nc.vector.tensor_copy(out=kpeT[64:96, :], in_=pb2[64:96, 384:512]), [P[2]], [kpeT])
                k.op(ACT, lambda: nc.scalar.copy(out=PK[:, O_QMT:O_QMT + 256], in_=pb2[:, 512:768]), [P[2]], [PK])
                yield
                for n2 in range(2):
                    bk = P[n2]
                    for c in range(2):
                        k.op(PE, lambda: nc.tensor.matmul(bk[:, 0:384], lhsT=cqnT[:, c, :],
                                                          rhs=wqb[:, c, n2 * 384:(n2 + 1) * 384],
                                                          start=(c == 0), stop=(c == 1)), [cqnT, wqb], [bk])
                    qv = bk[:, 0:384].rearrange("p (h d) -> p h d", h=4)
                    k.op(ACT, lambda: nc.scalar.copy(out=q_b[:, n2 * 4:(n2 + 1) * 4, 0:64], in_=qv[:, :, 0:64]), [bk], [q_b])
                    xv = qv[:, :, 64:96].rearrange("p h (t f) -> p h t f", t=2)
                    self._rotary(xv, tix, 40, 16, tA, tB, q_b[:, n2 * 4:(n2 + 1) * 4, 64:80],
                                 q_b[:, n2 * 4:(n2 + 1) * 4, 80:96], [bk], [q_b], 4)
                for h in range(8):
                    k.op(PE, lambda: nc.tensor.transpose(out=pb2[0:96, h * 128:(h + 1) * 128], in_=q_b[:, h, :],
                                                         identity=self.ident[:]), [q_b, self.ident], [P[2]])
                k.op(ACT, lambda: nc.scalar.copy(out=PK[0:96, O_QT:O_QT + 1024], in_=pb2[0:96, 0:1024]), [P[2]], [PK])
                yield
                for h in range(8):
                    bk = P[h // 4]
                    k.op(PE, lambda: nc.tensor.matmul(bk[0:64, (h % 4) * 128:(h % 4 + 1) * 128],
                                                      lhsT=wkvb[:, 0, h * 128:h * 128 + 64], rhs=ckvnT[:, :],
                                                      start=True, stop=True), [wkvb, ckvnT], [bk])
                k.op(ACT, lambda: nc.scalar.copy(out=KT[0:64, 0:4, :].rearrange("p h t -> p (h t)"), in_=P[0][0:64, :]),
                     [P[0]], [KT])
                k.op(DVE, lambda: nc.vector.tensor_copy(out=KT[0:64, 4:8, :].rearrange("p h t -> p (h t)"), in_=P[1][0:64, :]),
                     [P[1]], [KT])
                k.op(POOL, lambda: nc.gpsimd.tensor_copy(out=KT[64:96, :, :],
                                                         in_=kpeT[64:96, :].unsqueeze(1).to_broadcast([32, 8, 128])),
                     [kpeT], [KT])
                k.dma(self.s_kTM[b, :, :, i * 128:(i + 1) * 128], KT[0:96, :, :], reads=[KT])
                k.op(PE, lambda: nc.tensor.matmul(P[0][:, :], lhsT=ckvnT[:, :],
                                                  rhs=wkvb[:, 0, :].rearrange("p (h x) -> p h x", h=8)[:, :, 64:128],
                                                  start=True, stop=True), [wkvb, ckvnT], [P[0]])
                k.op(ACT, lambda: nc.scalar.copy(out=VM[:, :, 0:64], in_=P[0][:, :].rearrange("p (h d) -> p h d", h=8)),
                     [P[0]], [VM])
                k.dma(self.s_vA[b, i], VM[:].rearrange("p h d -> p (h d)"), reads=[VM])
                yield
                for g0 in range(0, ntd, 4):
                    bk = P[(g0 // 4) % 2]
                    cnt = min(4, ntd - g0)
                    for sl in range(cnt):
                        nt = g0 + sl
                        rows = min(128, dc - nt * 128)
                        for c in range(8):
                            k.op(PE, lambda: nc.tensor.matmul(bk[0:rows, sl * 128:(sl + 1) * 128],
                                                              lhsT=wb[:, c, 416 + nt * 128:416 + nt * 128 + rows],
                                                              rhs=hT[:, c, :], start=(c == 0), stop=(c == 7)), [hT, wb], [bk])
                    full = cnt if (dc - (g0 + cnt - 1) * 128) >= 128 else cnt - 1
                    if full > 0:
                        k.op(ACT, lambda: nc.scalar.copy(out=d_f[:, g0:g0 + full, :].rearrange("p n t -> p (n t)"),
                                                         in_=bk[:, 0:full * 128]), [bk], [d_f])
                    if full < cnt:
                        rows = dc - (g0 + cnt - 1) * 128
                        k.op(ACT, lambda: nc.scalar.copy(out=d_f[0:rows, g0 + cnt - 1, :],
                                                         in_=bk[0:rows, (cnt - 1) * 128:cnt * 128]), [bk], [d_f])
            def R_gen(ti, b, i):
                PK = pk[ti % 2]
                d_f = d_fs[ti % 2]
                if vres and ti + 1 < len(tiles):
                    load_vf(ti + 1)
                k.op(POOL, lambda: nc.gpsimd.tensor_tensor(out=diff[:, :, 1:128], in0=d_f[:, :, 0:127], in1=d_f[:, :, 1:128],
                                                           op=ALU.subtract), [d_f], [diff])
                if i == 0:
                    k.op(POOL, lambda: nc.gpsimd.tensor_scalar(out=diff[:, :, 0], in0=d_f[:, :, 0], scalar1=-1.0, scalar2=1.0,
                                                               op0=ALU.mult, op1=ALU.mult), [d_f], [diff])
                else:
                    k.op(POOL, lambda: nc.gpsimd.tensor_tensor(out=diff[:, :, 0], in0=carry[:, :], in1=d_f[:, :, 0],
                                                               op=ALU.subtract), [carry, d_f], [diff])
                k.op(POOL, lambda: nc.gpsimd.tensor_copy(out=carry[:, :], in_=d_f[:, :, 127]), [d_f], [carry])
                k.op(DVE, lambda: nc.vector.tensor_tensor(out=diff[:], in0=diff[:], in1=cb(C_MU, 14), op=ALU.mult),
                     [diff, cols], [diff])
                sh = diff
                k.op(DVE, lambda: nc.vector.tensor_tensor(out=sh[:], in0=diff[:], in1=d_f[:], op=ALU.add), [diff, d_f], [sh])
                R_ = sh[:, 0:4, :]
                K_ = sh[:, 4:8, :]
                V_ = sh[:, 8:12, :]
                sgw, alr, kx, tmp, kmod, bb, lp, eP, eNP, ePp, eD, tmp2 = G
                yield
                k.op(ACT, lambda: nc.scalar.activation(out=tw[0:64, :], in_=sh[0:64, 12, :], func=AF.Tanh), [sh], [tw])
                for nt in range(4):
                    k.op(PE, lambda: nc.tensor.matmul(P[3][:, nt * 128:(nt + 1) * 128], lhsT=w2sb[0:64, nt * 128:(nt + 1) * 128],
                                                      rhs=tw[0:64, :], start=True, stop=True), [w2sb, tw], [P[3]])
                for nt in range(4):
                    k.op(PE, lambda: nc.tensor.matmul(P[4][:, nt * 128:(nt + 1) * 128], lhsT=a2sb[64:128, nt * 128:(nt + 1) * 128],
                                                      rhs=sh[64:128, 12, :], start=True, stop=True), [a2sb, sh], [P[4]])
                for nt in range(4):
                    k.op(ACT, lambda: nc.scalar.activation(out=sgw[:, nt, :], in_=P[3][:, nt * 128:(nt + 1) * 128],
                                                           func=AF.Sigmoid, bias=cols[:, C_W0 + nt:C_W0 + nt + 1]),
                         [P[3], cols], [sgw])
                for nt in range(4):
                    k.op(ACT, lambda: nc.scalar.activation(out=alr[:, nt, :], in_=P[4][:, nt * 128:(nt + 1) * 128],
                                                           func=AF.Sigmoid, bias=cols[:, C_A0 + nt:C_A0 + nt + 1]),
                         [P[4], cols], [alr])
                if vres:
                    VF = vft[ti % 2]
                    for nt in range(4):
                        k.op(PE, lambda: nc.tensor.matmul(P[5][:, nt * 128:(nt + 1) * 128], lhsT=v2sb[0:32, nt * 128:(nt + 1) * 128],
                                                          rhs=sh[0:32, 13, :], start=True, stop=True), [v2sb, sh], [P[5]])
                    for nt in range(4):
                        k.op(ACT, lambda: nc.scalar.activation(out=tmp[:, nt, :], in_=P[5][:, nt * 128:(nt + 1) * 128],
                                                               func=AF.Sigmoid, bias=cols[:, C_V0 + nt:C_V0 + nt + 1]),
                             [P[5], cols], [tmp])
                    k.op(POOL, lambda: nc.gpsimd.tensor_tensor(out=tmp2[:], in0=VF[:], in1=V_, op=ALU.subtract), [VF, sh], [tmp2])
                    k.op(POOL, lambda: nc.gpsimd.tensor_tensor(out=tmp2[:], in0=tmp2[:], in1=tmp[:], op=ALU.mult), [tmp2, tmp], [tmp2])
                    k.op(POOL, lambda: nc.gpsimd.tensor_tensor(out=V_, in0=V_, in1=tmp2[:], op=ALU.add), [sh, tmp2], [sh])
                else:
                    k.dma(self.s_vf[b, i].rearrange("p (n t) -> p n t", n=4), V_, reads=[sh])
                k.op(DVE, lambda: nc.vector.tensor_scalar(out=v4(sgw), in0=v4(sgw), scalar1=NEG_E, scalar2=None, op0=ALU.mult),
                     [sgw], [sgw])
                lw = sgw
                yield
                k.op(POOL, lambda: nc.gpsimd.tensor_tensor(out=kx[:], in0=K_, in1=cb(C_KK), op=ALU.mult), [sh, cols], [kx])
                k.op(POOL, lambda: nc.gpsimd.tensor_tensor(out=tmp[:], in0=kx[:], in1=kx[:], op=ALU.mult), [kx], [tmp])
                for nt in range(4):
                    k.op(PE, lambda: nc.tensor.matmul(P[6][:, nt * 128:(nt + 1) * 128], lhsT=onesblk[:, :], rhs=tmp[:, nt, :],
                                                      start=True, stop=True), [onesblk, tmp], [P[6]])
                k.op(ACT, lambda: nc.scalar.activation(out=v4(tmp2), in_=P[6][:, :], func=AF.Sqrt), [P[6]], [tmp2])
                k.op(DVE, lambda: nc.vector.tensor_scalar(out=v4(tmp2), in0=v4(tmp2), scalar1=1e-12, scalar2=None, op0=ALU.max),
                     [tmp2], [tmp2])
                k.op(DVE, lambda: nc.vector.reciprocal(out=v4(tmp2), in_=v4(tmp2)), [tmp2], [tmp2])
                yield
                kk = kx
                k.op(POOL, lambda: nc.gpsimd.tensor_tensor(out=kk[:], in0=kx[:], in1=tmp2[:], op=ALU.mult), [kx, tmp2], [kk])
                k.op(DVE, lambda: nc.vector.tensor_tensor(out=tmp[:], in0=alr[:], in1=cb(C_KA), op=ALU.mult), [alr, cols], [tmp])
                k.op(DVE, lambda: nc.vector.tensor_tensor(out=tmp[:], in0=tmp[:], in1=cb(C_KA), op=ALU.subtract), [tmp, cols], [tmp])
                k.op(DVE, lambda: nc.vector.scalar_tensor_tensor(out=v4(kmod), in0=v4(tmp), scalar=1.0,
                                                                 in1=K_.rearrange("p n t -> p (n t)"),
                                                                 op0=ALU.add, op1=ALU.mult), [tmp, sh], [kmod])
                k.op(POOL, lambda: nc.gpsimd.tensor_tensor(out=bb[:], in0=kk[:], in1=alr[:], op=ALU.mult), [kk, alr], [bb])
                yield
                k.op(POOL, lambda: nc.gpsimd.tensor_tensor(out=tmp2[:], in0=R_, in1=cb(C_RK), op=ALU.mult), [sh, cols], [tmp2])
                k.op(POOL, lambda: nc.gpsimd.tensor_tensor(out=tmp2[:], in0=tmp2[:], in1=kmod[:], op=ALU.mult), [tmp2, kmod], [tmp2])
                for nt in range(4):
                    k.op(PE, lambda: nc.tensor.matmul(P[7][:, nt * 128:(nt + 1) * 128], lhsT=onesblk[:, :], rhs=tmp2[:, nt, :],
                                                      start=True, stop=True), [onesblk, tmp2], [P[7]])
                k.op(DVE, lambda: nc.vector.tensor_tensor(out=PK[:, O_BON:O_BON + 512], in0=P[7][:, :],
                                                          in1=V_.rearrange("p n t -> p (n t)"), op=ALU.mult), [P[7], sh], [PK])
                yield
                for nt in range(4):
                    k.op(DVE, lambda: nc.vector.tensor_tensor_scan(out=lp[:, nt, :], data0=scanrst[:, :], data1=lw[:, nt, :],
                                                                   initial=0.0, op0=ALU.mult, op1=ALU.add),
                         [scanrst, lw], [lp])
                k.op(ACT, lambda: nc.scalar.activation(out=v4(eP), in_=v4(lp), func=AF.Exp), [lp], [eP])
                k.op(ACT, lambda: nc.scalar.activation(out=v4(eNP), in_=v4(lp), func=AF.Exp, scale=-1.0), [lp], [eNP])
                k.op(POOL, lambda: nc.gpsimd.tensor_tensor(out=ePp[:], in0=lp[:], in1=lw[:], op=ALU.subtract), [lp, lw], [ePp])
                k.op(ACT, lambda: nc.scalar.activation(out=v4(ePp), in_=v4(ePp), func=AF.Exp), [ePp], [ePp])
                yield
                for c in range(2):
                    k.op(POOL, lambda: nc.gpsimd.tensor_tensor(
                        out=eD[:, :, 64 * c:64 * c + 64], in0=lp[:, :, 64 * c + 63:64 * c + 64].to_broadcast([128, 4, 64]),
                        in1=lp[:, :, 64 * c:64 * c + 64], op=ALU.subtract), [lp], [eD])
                k.op(ACT, lambda: nc.scalar.activation(out=v4(eD), in_=v4(eD), func=AF.Exp), [eD], [eD])
                PCS = pcs[ti % 2]
                k.op(DVE, lambda: nc.vector.tensor_copy(out=PCS[:].rearrange("p (n c) -> p n c", n=4),
                                                        in_=eP[:].rearrange("p n (c t) -> p n c t", c=2)[:, :, :, 63]),
                     [eP], [PCS])
                k.dma(self.s_pc[b, i], PCS[:], reads=[PCS])
                yield
                AR = PK[:, O_AR:O_AR + 1024].rearrange("p (n w t) -> p n w t", n=4, w=2)
                BKv = PK[:, O_BK:O_BK + 1024].rearrange("p (n c g t) -> p n c g t", n=4, c=2, g=2)
                BPv = PK[:, O_BKP:O_BKP + 1024].rearrange("p (n c g t) -> p n c g t", n=4, c=2, g=2)
                BT = PK[:, O_BT:O_BT + 512].rearrange("p (n t) -> p n t", n=4)
                k.op(DVE, lambda: nc.vector.scalar_tensor_tensor(out=AR[:, :, 0, :], in0=kk[:], scalar=-1.0, in1=ePp[:],
                                                                 op0=ALU.mult, op1=ALU.mult), [kk, ePp], [PK])
                k.op(POOL, lambda: nc.gpsimd.tensor_tensor(out=AR[:, :, 1, :], in0=R_, in1=eP[:], op=ALU.mult), [sh, eP], [PK])
                k.op(POOL, lambda: nc.gpsimd.tensor_tensor(out=BT, in0=bb[:], in1=eNP[:], op=ALU.mult), [bb, eNP], [PK])
                yield
                n_ = 0
                for (dst, ee) in ((BKv, eNP), (BPv, eD)):
                    for c in range(2):
                        sl = slice(64 * c, 64 * c + 64)
                        for (srcv, g_) in ((bb, c), (kmod, 1 - c)):
                            eng = DVE if n_ % 2 == 0 else POOL
                            mod = nc.vector if eng == DVE else nc.gpsimd
                            k.op(eng, lambda: mod.tensor_tensor(out=dst[:, :, c, g_, :], in0=srcv[:, :, sl], in1=ee[:, :, sl],
                                                                op=ALU.mult), [srcv, ee], [PK])
                            n_ += 1
                k.op(ACT, lambda: nc.scalar.copy(out=PK[:, O_VT:O_VT + 512], in_=V_.rearrange("p n t -> p (n t)")), [sh], [PK])
                k.dma(self.s_pk[b, i], PK[:], reads=[PK])

            def drain(*gens):
                alive = [g for g in gens if g is not None]
                while alive:
                    for g in list(alive):
                        try:
                            next(g)
                        except StopIteration:
                            alive.remove(g)

            drain(F_gen(0, *tiles[0]))
            for ti in range(len(tiles)):
                nf = F_gen(ti + 1, *tiles[ti + 1]) if ti + 1 < len(tiles) else None
                drain(R_gen(ti, *tiles[ti]), nf)
            k.barrier()
        if self.stop == "p1":
            return
        with contextlib.ExitStack() as es:
            wout = k.sb("wout", [128, 10, DM], BF16, es)
            kmT = k.sb("okmT", [128, 2, 2, 256], BF16, es)
            vm = k.sb("ovm", [128, 2, 2, 4, 65], BF16, es)
            with contextlib.ExitStack() as es0:
                wmem = k.sb("wmem", [128, 8, 512], BF16, es0)
                self.stage = [k.sb("stgA2", [128, 2048], F32, es0), k.sb("stgB2", [128, 2048], F32, es0)]
                self._load_w(es, "wout", self.w[pre + "w_out"], 1280, DM, wb=wout)
                self._load_w(es, "wmem", self.w[pre + "w_mem_kv"], DM, 512, wb=wmem)
                self._mem_kv(es0, wmem, "o", pre=(kmT, vm))
                k.barrier()
            causal = k.sb("causal", [128, 128], BF16, es)
            maskRW = k.sb("maskRW", [128, 512], BF16, es)
            lnw = k.sb("lnw", [128, 512], F32, es)
            lnb = k.sb("lnb", [128, 512], F32, es)
            k.dma(causal[:], self.cst["causal"], writes=[causal])
            k.dma(maskRW[:], self.cst["maskRW"], writes=[maskRW])
            k.dma(lnw[:], self.w[pre + "lnx_w"][0:1, :].broadcast_to([128, 512]), writes=[lnw])
            k.dma(lnb[:], self.w[pre + "lnx_b"][0:1, :].broadcast_to([128, 512]), writes=[lnb])
            gfin = None
            if last and self.final_norm:
                gfin = k.sb("gfin", [128, DM], F32, es)
                k.dma(gfin[:], self.w["final_norm"][0:1, :].broadcast_to([128, DM]), writes=[gfin])
            kT = k.sb("kTM", [128, 8, SEQ], BF16, es)
            vM = k.sb("vMc", [128, NT, 8, 65], BF16, es)
            xt = [k.sb("xt2_%d" % i, [128, DM], F32, es) for i in range(3)]
            pk = [k.sb("pk2_%d" % i, [128, ODD_W], BF16, es) for i in range(3)]
            pT = [[k.sb("pT%d_%d" % (i, j), [128, 2, 128], BF16, es) for j in range(2)] for i in range(2)]
            SBANKS = [(P[3], P[4]), (P[0], P[1])]
            rec = k.sb("rec", [128, 16], F32, es)
            y2 = [k.sb("y%d" % i, [128, 1280], F32, es) for i in range(2)]
            yg = k.sb("yg", [128, 1280], BF16, es)
            ygT = k.sb("ygT", [128, 10, 128], BF16, es)
            st = k.sb("st2", [128, 48], F32, es)
            Am = k.sb("Am", [128, 8, 512], BF16, es)
            Xa = k.sb("Xa", [128, 8, 128], BF16, es)
            Xb = k.sb("Xb", [128, 8, 128], BF16, es)
            XT = k.sb("XT", [128, 8, 128], BF16, es)
            NM = [k.sb("NM%d" % i, [128, 8, 256], BF16, es) for i in range(2)]
            W_all = k.sb("W_all", [128, 8, 2, 64], BF16, es)
            BPtm = [k.sb("BPtm%d" % p, [128, 2, 128], BF16, es) for p in range(4)]
            S_f = k.sb("S_f", [128, 4, 64], F32, es)
            S_b = k.sb("S_b", [128, 4, 64], BF16, es)
            ytok = k.sb("ytok", [128, 8, 64], F32, es)
            ysq = k.sb("ysq", [128, 8, 64], F32, es)
            pcl = [k.sb("pcl%d" % i, [128, 8], F32, es) for i in range(3)]
            pTA = [k.sb("pTA%d" % i, [128, 4, 128], BF16, es) for i in range(2)]
            recA = k.sb("recA", [128, 4], F32, es)
            k.op(POOL, lambda: nc.gpsimd.memset(W_all[:], 0.0), [], [W_all])

            def prefetch(ti):
                b_, i_ = tiles[ti]
                k.dma(xt[ti % 3][:], src[b_, i_ * 128:(i_ + 1) * 128, :], reads=[self.xbuf[b_][i_]], writes=[xt[ti % 3]])
                k.dma(pk[ti % 3][:], self.s_pk[b_, i_], writes=[pk[ti % 3]])
                k.dma(pcl[ti % 3][:], self.s_pc[b_, i_], writes=[pcl[ti % 3]])

            NTL = len(tiles)
            prefetch(0)
            if NTL > 1:
                prefetch(1)
            scl = [0]
            SCL = 96.0 ** -0.5

            def A_gen(ti, b, i):
                PK = pk[ti % 3]
                Y = y2[ti % 2]
                if i == 0:
                    k.dma(kT[0:96, :, :], self.s_kTM[b], writes=[kT])
                    k.dma(vM[:].rearrange("p i h d -> p i (h d)"), self.s_vA[b].rearrange("i p w -> p i w"), writes=[vM])
                qT = PK[:, O_QT:O_QT + 1024].rearrange("p (h t) -> p h t", h=8)
                S, O = P[5], P[6]
                for half in range(2):
                    first = True
                    for j in range(i + 1):
                        pt = pTA[j % 2]
                        for hh in range(4):
                            h = half * 4 + hh
                            k.op(PE, lambda: nc.tensor.matmul(S[:, hh * 128:(hh + 1) * 128], lhsT=kT[0:96, h, j * 128:(j + 1) * 128],
                                                              rhs=qT[0:96, h, :], start=True, stop=True), [kT, PK], [S])
                        k.op(ACT, lambda: nc.scalar.activation(out=pt[:].rearrange("p h q -> p (h q)"), in_=S[:, :],
                                                               func=AF.Exp, scale=SCL), [S], [pt])
                        if j == i:
                            k.op(DVE, lambda: nc.vector.tensor_tensor(
                                out=pt[:], in0=pt[:], in1=causal[:, :].unsqueeze(1).to_broadcast([128, 4, 128]), op=ALU.mult),
                                [pt, causal], [pt])
                        for hh in range(4):
                            h = half * 4 + hh
                            k.op(PE, lambda: nc.tensor.matmul(O[:, hh * 65:(hh + 1) * 65], lhsT=pt[:, hh, :], rhs=vM[:, j, h, :],
                                                              start=first, stop=(j == i), skip_group_check=True), [pt, vM], [O])
                            first = False
                        yield
                    ov = O[:, 0:260].rearrange("p (h d) -> p h d", h=4)
                    k.op(DVE, lambda: nc.vector.reciprocal(out=recA[:, 0:4], in_=ov[:, :, 64]), [O], [recA])
                    k.op(DVE, lambda: nc.vector.tensor_tensor(
                        out=Y[:, half * 256:(half + 1) * 256].rearrange("p (h d) -> p h d", h=4), in0=ov[:, :, 0:64],
                        in1=recA[:, 0:4].unsqueeze(2).to_broadcast([128, 4, 64]), op=ALU.mult), [O, recA], [Y])
                    yield

            def B_gen(ti, b, i):
                X = xt[ti % 3]
                PK = pk[ti % 3]
                y = y2[ti % 2]
                sc = scl[0]
                qmT = PK[:, O_QMT:O_QMT + 256].rearrange("p (a t) -> p a t", a=2)
                sg = PK[:, O_SG:O_SG + 1280]
                AR = PK[:, O_AR:O_AR + 1024].rearrange("p (n w t) -> p n w t", n=4, w=2)
                BKv = PK[:, O_BK:O_BK + 1024].rearrange("p (n c x) -> p n c x", n=4, c=2)
                BPv = PK[:, O_BKP:O_BKP + 1024].rearrange("p (n c x) -> p n c x", n=4, c=2)
                BT = PK[:, O_BT:O_BT + 512].rearrange("p (n t) -> p n t", n=4)
                VT = PK[:, O_VT:O_VT + 512].rearrange("p (n t) -> p n t", n=4)
                BON = PK[:, O_BON:O_BON + 512]
                PCL = pcl[ti % 3]
                pcv = PCL[:]
                k.op(POOL, lambda: nc.gpsimd.memset(W_all[0:64, :, 0, :], 0.0), [], [W_all])
                k.op(POOL, lambda: nc.gpsimd.memset(W_all[64:128, :, 1, :], 0.0), [], [W_all])
                for p_ in range(4):
                    k.op(PE, lambda: nc.tensor.transpose(out=pb2[:, 0:128], in_=AR[:, p_, 0, :], identity=self.ident[:]),
                         [PK, self.ident], [P[2]])
                    k.op(PE, lambda: nc.tensor.transpose(out=pb2[64:128, 128:256], in_=VT[:, p_, 0:64], identity=self.ident[:]),
                         [PK, self.ident], [P[2]])
                    k.op(PE, lambda: nc.tensor.transpose(out=pb2[0:64, 128:256], in_=VT[:, p_, 64:128], identity=self.ident[:]),
                         [PK, self.ident], [P[2]])
                    for c in range(2):
                        k.op(PE, lambda: nc.tensor.transpose(out=pb2[:, 256 + c * 128:384 + c * 128], in_=BPv[:, p_, c, :],
                                                             identity=self.ident[:]), [PK, self.ident], [P[2]])
                    for s_ in range(2):
                        k.op(ACT, lambda: nc.scalar.copy(out=Xa[:, 2 * p_ + s_, 64 * s_:64 * s_ + 64],
                                                         in_=pb2[:, 64 * s_:64 * s_ + 64]), [P[2]], [Xa])
                    k.op(DVE, lambda: nc.vector.tensor_copy(out=W_all[64:128, 2 * p_:2 * p_ + 2, 0, :],
                                                            in_=pb2[64:128, 128:256].rearrange("p (s d) -> p s d", s=2)),
                         [P[2]], [W_all])
                    k.op(DVE, lambda: nc.vector.tensor_copy(out=W_all[0:64, 2 * p_:2 * p_ + 2, 1, :],
                                                            in_=pb2[0:64, 128:256].rearrange("p (s d) -> p s d", s=2)),
                         [P[2]], [W_all])
                    k.op(ACT, lambda: nc.scalar.copy(out=BPtm[p_][:].rearrange("p c x -> p (c x)"), in_=pb2[:, 256:512]),
                         [P[2]], [BPtm[p_]])
                yield
                for h in range(8):
                    s_, p_ = h % 2, h // 2
                    rs = slice(64 * s_, 64 * s_ + 64)
                    A = P[3 + s_]
                    for c in range(2):
                        k.op(PE, lambda: nc.tensor.matmul(A[:, c * 128:(c + 1) * 128], lhsT=BKv[rs, p_, c, :],
                                                          rhs=AR[rs, p_, :, 64 * c:64 * c + 64], start=True, stop=True), [PK], [A])
                    k.op(PE, lambda: nc.tensor.matmul(A[:, 256:384], lhsT=AR[rs, p_, 0, :], rhs=BT[rs, p_, :],
                                                      start=True, stop=True), [PK], [A])
                    k.op(PE, lambda: nc.tensor.matmul(A[:, 384:512], lhsT=BT[rs, p_, :], rhs=AR[rs, p_, 0, :],
                                                      start=True, stop=True), [PK], [A])
                    k.op(DVE, lambda: nc.vector.tensor_tensor(out=Am[:, h, :], in0=A[:, :], in1=maskRW[:], op=ALU.mult),
                         [A, maskRW], [Am])
                    yield
                GB = [(P[0], P[3], P[4]), (P[1], P[7], P[2])]
                for g in range(2):
                    XB = GB[g][0]
                    for q in range(4):
                        h = 4 * g + q
                        for c in range(2):
                            k.op(PE, lambda: nc.tensor.matmul(XB[64 * c:64 * c + 64, q * 64:(q + 1) * 64],
                                                              lhsT=Am[:, h, c * 128:c * 128 + 64], rhs=W_all[:, h, c, :],
                                                              start=True, stop=True), [Am, W_all], [XB])
                    for s_ in range(2):
                        uc = slice(64 * (1 - s_), 64 * (1 - s_) + 64)
                        k.op(ACT, lambda: nc.scalar.copy(
                            out=Xa[:, 4 * g + s_:4 * g + 4:2, uc],
                            in_=XB[:, 0:256].rearrange("p (q d) -> p q d", q=4)[:, s_:4:2, :]), [XB], [Xa])
                yield
                Xc, Xn = Xa, Xb
                cur = None
                for lvl in range(6):
                    for g in range(2):
                        XB = GB[g][0]
                        for q in range(4):
                            h = 4 * g + q
                            Mh = Am[:, h, 384:512] if cur is None else cur[:, h, 128:256]
                            k.op(PE, lambda: nc.tensor.matmul(XB[:, q * 128:(q + 1) * 128], lhsT=Mh, rhs=Xc[:, h, :],
                                                              start=True, stop=True), [Am if cur is None else cur, Xc], [XB])
                    for g in range(2):
                        XB = GB[g][0]
                        k.op(DVE, lambda: nc.vector.tensor_tensor(
                            out=Xn[:, 4 * g:4 * g + 4, :].rearrange("p q t -> p (q t)"), in0=XB[:, :],
                            in1=Xc[:, 4 * g:4 * g + 4, :].rearrange("p q t -> p (q t)"), op=ALU.add), [XB, Xc], [Xn])
                    Xc, Xn = Xn, Xc
                    yield
                    if lvl < 5:
                        nxt = NM[lvl % 2]
                        for g in range(2):
                            Qn, Qm = GB[g][1], GB[g][2]
                            for q in range(4):
                                h = 4 * g + q
                                Nh = Am[:, h, 256:384] if cur is None else cur[:, h, 0:128]
                                Mh = Am[:, h, 384:512] if cur is None else cur[:, h, 128:256]
                                srcb = Am if cur is None else cur
                                if lvl < 4:
                                    k.op(PE, lambda: nc.tensor.matmul(Qn[:, q * 128:(q + 1) * 128], lhsT=Mh, rhs=Nh,
                                                                      start=True, stop=True), [srcb], [Qn])
                                k.op(PE, lambda: nc.tensor.matmul(Qm[:, q * 128:(q + 1) * 128], lhsT=Nh, rhs=Mh,
                                                                  start=True, stop=True), [srcb], [Qm])
                        for g in range(2):
                            Qn, Qm = GB[g][1], GB[g][2]
                            if lvl < 4:
                                k.op(ACT, lambda: nc.scalar.copy(out=nxt[:, 4 * g:4 * g + 4, 0:128],
                                                                 in_=Qn[:, :].rearrange("p (q t) -> p q t", q=4)), [Qn], [nxt])
                            k.op(DVE if g == 0 else ACT, lambda: (nc.vector.tensor_copy if g == 0 else nc.scalar.copy)(
                                out=nxt[:, 4 * g:4 * g + 4, 128:256], in_=Qm[:, :].rearrange("p (q t) -> p q t", q=4)),
                                [Qm], [nxt])
                        cur = nxt
                    yield
                yield
                for h in range(8):
                    k.op(PE, lambda: nc.tensor.transpose(out=pb2[:, h * 128:(h + 1) * 128], in_=Xa[:, h, :],
                                                         identity=self.ident[:]), [Xa, self.ident], [P[2]])
                k.op(ACT, lambda: nc.scalar.copy(out=XT[:].rearrange("p h t -> p (h t)"), in_=pb2[:, 0:1024]), [P[2]], [XT])
                yield
                pcv3 = pcv.rearrange("p (n c) -> p n c", n=4)
                for c in range(2):
                    cs = slice(64 * c, 64 * c + 64)
                    fresh = (i == 0 and c == 0)
                    for s_ in range(2):
                        rs = slice(64 * s_, 64 * s_ + 64)
                        uc = slice(64 * (1 - s_), 64 * (1 - s_) + 64)
                        Ba = P[s_]
                        if fresh:
                            k.op(DVE, lambda: nc.vector.tensor_copy(out=W_all[cs, s_:8:2, c, :], in_=Xa[cs, s_:8:2, uc]),
                                 [Xa], [W_all])
                        else:
                            for p_ in range(4):
                                h = 2 * p_ + s_
                                k.op(PE, lambda: nc.tensor.matmul(Ba[cs, p_ * 64:(p_ + 1) * 64], lhsT=XT[rs, h, cs],
                                                                  rhs=S_b[rs, p_, :], start=True, stop=True), [XT, S_b], [Ba])
                            k.op(DVE, lambda: nc.vector.tensor_tensor(
                                out=W_all[cs, s_:8:2, c, :], in0=Ba[cs, 0:256].rearrange("p (q d) -> p q d", q=4),
                                in1=Xa[cs, s_:8:2, uc], op=ALU.add), [Ba, Xa], [W_all])
                    yield
                    for s_ in range(2):
                        rs = slice(64 * s_, 64 * s_ + 64)
                        Ba, Bb = P[s_], P[3 + s_]
                        for p_ in range(4):
                            h = 2 * p_ + s_
                            yo = slice(256 + p_ * 64, 256 + (p_ + 1) * 64)
                            if not fresh:
                                k.op(PE, lambda: nc.tensor.matmul(Ba[cs, yo], lhsT=AR[rs, p_, 1, cs], rhs=S_b[rs, p_, :],
                                                                  start=True, stop=False), [PK, S_b], [Ba])
                            k.op(PE, lambda: nc.tensor.matmul(Ba[cs, yo], lhsT=Am[:, h, c * 128 + 64:(c + 1) * 128],
                                                              rhs=W_all[:, h, c, :], start=fresh, stop=True), [Am, W_all], [Ba])
                        for p_ in range(4):
                            h = 2 * p_ + s_
                            k.op(PE, lambda: nc.tensor.matmul(Bb[rs, p_ * 64:(p_ + 1) * 64], lhsT=BPtm[p_][:, c, rs],
                                                              rhs=W_all[:, h, c, :], start=True, stop=True),
                                 [BPtm[p_], W_all], [Bb])
                    yield
                    for s_ in range(2):
                        rs = slice(64 * s_, 64 * s_ + 64)
                        Ba, Bb = P[s_], P[3 + s_]
                        sfv = S_f[rs, :, :]
                        if fresh:
                            k.op(DVE, lambda: nc.vector.tensor_copy(out=sfv, in_=Bb[rs, 0:256].rearrange("p (q d) -> p q d", q=4)),
                                 [Bb], [S_f])
                        else:
                            k.op(POOL, lambda: nc.gpsimd.tensor_tensor(
                                out=sfv, in0=sfv, in1=pcv3[rs, :, c].unsqueeze(2).to_broadcast([64, 4, 64]), op=ALU.mult),
                                [S_f, PCL], [S_f])
                            k.op(DVE, lambda: nc.vector.tensor_tensor(out=sfv, in0=Bb[rs, 0:256].rearrange("p (q d) -> p q d", q=4),
                                                                      in1=sfv, op=ALU.add), [Bb, S_f], [S_f])
                        k.op(POOL, lambda: nc.gpsimd.tensor_copy(out=S_b[rs, :, :], in_=sfv), [S_f], [S_b])
                        k.op(ACT, lambda: nc.scalar.copy(out=ytok[cs, s_:8:2, :],
                                                         in_=Ba[cs, 256:512].rearrange("p (q d) -> p q d", q=4)), [Ba], [ytok])
                yield
                k.op(DVE, lambda: nc.vector.tensor_reduce(out=st[:, 8:16], in_=ytok[:], axis=AX.X, op=ALU.add), [ytok], [st])
                k.op(POOL, lambda: nc.gpsimd.tensor_tensor(out=ysq[:], in0=ytok[:], in1=ytok[:], op=ALU.mult), [ytok], [ysq])
                k.op(DVE, lambda: nc.vector.tensor_reduce(out=st[:, 16:24], in_=ysq[:], axis=AX.X, op=ALU.add), [ysq], [st])
                k.op(DVE, lambda: nc.vector.tensor_scalar(out=st[:, 8:16], in0=st[:, 8:16], scalar1=1.0 / 64, scalar2=None,
                                                          op0=ALU.mult), [st], [st])
                k.op(DVE, lambda: nc.vector.tensor_tensor(out=st[:, 24:32], in0=st[:, 8:16], in1=st[:, 8:16], op=ALU.mult),
                     [st], [st])
                k.op(DVE, lambda: nc.vector.scalar_tensor_tensor(out=st[:, 16:24], in0=st[:, 16:24], scalar=1.0 / 64,
                                                                 in1=st[:, 24:32], op0=ALU.mult, op1=ALU.subtract),
                     [st], [st])
                k.op(ACT, lambda: nc.scalar.activation(out=st[:, 24:32], in_=st[:, 16:24], func=AF.Sqrt, bias=LN_EPS), [st], [st])
                k.op(DVE, lambda: nc.vector.reciprocal(out=st[:, 16:24], in_=st[:, 24:32]), [st], [st])
                k.op(DVE, lambda: nc.vector.tensor_tensor(out=ysq[:], in0=ytok[:],
                                                          in1=st[:, 8:16].unsqueeze(2).to_broadcast([128, 8, 64]),
                                                          op=ALU.subtract), [ytok, st], [ysq])
                k.op(DVE, lambda: nc.vector.tensor_tensor(out=ysq[:], in0=ysq[:],
                                                          in1=st[:, 16:24].unsqueeze(2).to_broadcast([128, 8, 64]),
                                                          op=ALU.mult), [ysq, st], [ysq])
                ysf = ysq[:].rearrange("p h d -> p (h d)")
                k.op(POOL, lambda: nc.gpsimd.tensor_tensor(out=ysf, in0=ysf, in1=lnw[:], op=ALU.mult), [ysq, lnw], [ysq])
                k.op(POOL, lambda: nc.gpsimd.tensor_tensor(out=ysf, in0=ysf, in1=lnb[:], op=ALU.add), [ysq, lnb], [ysq])
                for c in range(4):
                    k.op(PE, lambda: nc.tensor.transpose(out=pb2[:, c * 128:(c + 1) * 128], in_=BON[:, c * 128:(c + 1) * 128],
                                                         identity=self.ident[:]), [PK, self.ident], [P[2]])
                k.op(DVE, lambda: nc.vector.tensor_tensor(out=y[:, 512:1024], in0=pb2[:, 0:512], in1=ysf, op=ALU.add),
                     [P[2], ysq], [y])
                yield
                self._mem_attn(b, qmT, PK, kmT, vm, pT, rec, y, sc, SBANKS)
                scl[0] = sc + 2
                yield
                self._tail(PK, sg, y, yg, ygT, wout, X, X, yg, st, b, i, last, gfin)

            def drain(*gens):
                alive = [g for g in gens if g is not None]
                while alive:
                    for g in list(alive):
                        try:
                            next(g)
                        except StopIteration:
                            alive.remove(g)

            drain(A_gen(0, *tiles[0]))
            for ti in range(NTL):
                if ti + 2 < NTL:
                    prefetch(ti + 2)
                drain(B_gen(ti, *tiles[ti]), A_gen(ti + 1, *tiles[ti + 1]) if ti + 1 < NTL else None)
            k.barrier()

    def _mem_attn(self, b, qmT, PK, kmT, vm, pT, rec, y, sc, SBANKS):
        k, nc = self.k, self.nc
        P = self.P
        O = P[7]
        first = True
        for mb in range(2):
            Sb = SBANKS[sc % 2]
            ptp = pT[sc % 2]
            sc += 1
            for h in range(4):
                s_, p_ = h % 2, h // 2
                k.op(PE, lambda: nc.tensor.matmul(Sb[s_][:, p_ * 128:(p_ + 1) * 128],
                                                  lhsT=kmT[64 * s_:64 * s_ + 64, b, p_, mb * 128:(mb + 1) * 128],
                                                  rhs=qmT[64 * s_:64 * s_ + 64, p_, :], start=True, stop=True),
                     [kmT, PK], [Sb[s_]])
            for s_ in range(2):
                pt = ptp[s_]
                k.op(ACT, lambda: nc.scalar.activation(out=pt[:, 0:2, :].rearrange("p h q -> p (h q)"), in_=Sb[s_][:, 0:256],
                                                       func=AF.Exp, scale=0.125), [Sb[s_]], [pt])
                for p_ in range(2):
                    h = 2 * p_ + s_
                    k.op(PE, lambda: nc.tensor.matmul(O[:, h * 65:(h + 1) * 65], lhsT=pt[:, p_, :], rhs=vm[:, b, mb, h, :],
                                                      start=first, stop=(mb == 1), skip_group_check=True), [pt, vm], [O])
                    first = False
        ov = O[:, 0:260].rearrange("p (h d) -> p h d", h=4)
        k.op(DVE, lambda: nc.vector.reciprocal(out=rec[:, 8:12], in_=ov[:, :, 64]), [O], [rec])
        k.op(DVE, lambda: nc.vector.tensor_tensor(
            out=y[:, 1024:1280].rearrange("p (h d) -> p h d", h=4), in0=ov[:, :, 0:64],
            in1=rec[:, 8:12].unsqueeze(2).to_broadcast([128, 4, 64]), op=ALU.mult), [O, rec], [y])

    def _tail(self, PK, sg, y, yg, ygT, wout, X, xo, junk, st, b, i, last, gfin):
        k, nc = self.k, self.nc
        P = self.P
        pb2 = P[2][:].bitcast(BF16)
        k.op(POOL, lambda: nc.gpsimd.tensor_tensor(out=yg[:], in0=y[:], in1=sg, op=ALU.mult), [y, PK], [yg])
        for c0, c1 in ((0, 8), (8, 10)):
            for c in range(c0, c1):
                k.op(PE, lambda: nc.tensor.transpose(out=pb2[:, (c - c0) * 128:(c - c0 + 1) * 128],
                                                     in_=yg[:, c * 128:(c + 1) * 128], identity=self.ident[:]),
                     [yg, self.ident], [P[2]])
            k.op(ACT, lambda: nc.scalar.copy(out=ygT[:, c0:c1, :].rearrange("p c t -> p (c t)"),
                                             in_=pb2[:, 0:(c1 - c0) * 128]), [P[2]], [ygT])
        for n in range(2):
            for c in range(10):
                k.op(PE, lambda: nc.tensor.matmul(P[n][:, :], lhsT=ygT[:, c, :], rhs=wout[:, c, n * 512:(n + 1) * 512],
                                                  start=(c == 0), stop=(c == 9)), [ygT, wout], [P[n]])
            k.op(DVE, lambda: nc.vector.tensor_tensor(out=xo[:, n * 512:(n + 1) * 512], in0=P[n][:, :],
                                                      in1=X[:, n * 512:(n + 1) * 512], op=ALU.add), [P[n], X], [xo])
        if gfin is not None:
            k.op(ACT, lambda: nc.scalar.activation(out=junk[:, 0:DM], in_=xo[:], func=AF.Square, accum_out=st[:, 0:1]),
                 [xo], [junk, st])
            k.op(ACT, lambda: nc.scalar.activation(out=st[:, 1:2], in_=st[:, 0:1], func=AF.Sqrt, scale=1.0 / DM, bias=EPS),
                 [st], [st])
            k.op(DVE, lambda: nc.vector.reciprocal(out=st[:, 2:3], in_=st[:, 1:2]), [st], [st])
            k.op(DVE, lambda: nc.vector.scalar_tensor_tensor(out=xo[:], in0=xo[:], scalar=st[:, 2:3], in1=gfin[:],
                                                             op0=ALU.mult, op1=ALU.mult), [xo, st, gfin], [xo])
        k.dma(self.out[b, i * 128:(i + 1) * 128, :], xo[:], reads=[xo], writes=[self.xbuf[b][i]])


def _col(v, n):
    return np.ascontiguousarray(np.asarray(v, np.float32).reshape(n, 128).T)


_NET_CACHE = {}


def _get_net(n_layers=4, final_norm=True):
    key = (n_layers, final_norm)
    if key not in _NET_CACHE:
        _NET_CACHE[key] = Net(n_layers, final_norm)
    return _NET_CACHE[key]


def _in_maps(inputs):
    cst = _consts()
    shared = {"c_" + n: v for n, v in cst.items()}
    shared["mem_norm_col"] = _col(inputs["mem_norm"], 8)
    shared["final_norm"] = np.asarray(inputs["final_norm"], np.float32).reshape(1, DM)
    for l in range(4):
        p = "l%d_" % l
        shared[p + "norm_col"] = _col(inputs[p + "norm"], 8)
        shared[p + "w_mem_kv"] = np.asarray(inputs[p + "w_mem_kv"], np.float32)
        shared[p + "w_out"] = np.asarray(inputs[p + "w_out"], np.float32)
        shared[p + "w_in"] = np.asarray(inputs[p + "w_in"], np.float32)
        if l % 2 == 1:
            dc = 1664 if l == 1 else 1696
            shared[p + "qn_col"] = _col(inputs[p + "q_norm"], 2)
            shared[p + "kvn_col"] = _col(inputs[p + "kv_norm"], 1)
            shared[p + "w_qb"] = np.asarray(inputs[p + "w_qb"], np.float32)
            shared[p + "w_kvb"] = np.asarray(inputs[p + "w_kvb"], np.float32)
            mu = np.zeros(14 * 128, np.float32)
            mu[:dc] = np.asarray(inputs[p + "mu_shift"], np.float32)
            v0 = np.asarray(inputs[p + "v0"], np.float32) if l == 3 else np.zeros(512, np.float32)
            cols = [_col(mu, 14)] + [_col(np.asarray(inputs[p + n], np.float32).reshape(-1), 4) if n else _col(v0, 4)
                                     for n in ("w0", "a0", None, "k_k", "k_a", "r_k")]
            shared[p + "cols"] = np.ascontiguousarray(np.concatenate(cols, 1))
            shared[p + "w2"] = np.asarray(inputs[p + "w2"], np.float32)
            shared[p + "a2"] = np.asarray(inputs[p + "a2"], np.float32)
            if l == 3:
                shared[p + "v2"] = np.asarray(inputs[p + "v2"], np.float32)
            shared[p + "lnx_w"] = np.asarray(inputs[p + "lnx_w"], np.float32).reshape(1, 512)
            shared[p + "lnx_b"] = np.asarray(inputs[p + "lnx_b"], np.float32).reshape(1, 512)
    maps = []
    x = np.asarray(inputs["x"], np.float32)
    mem = np.asarray(inputs["mem"], np.float32)
    pos = np.asarray(inputs["positions"], np.int32)
    for c in range(NCORES):
        m = dict(shared)
        m["x"] = np.ascontiguousarray(x[2 * c:2 * c + 2])
        m["mem"] = np.ascontiguousarray(mem[2 * c:2 * c + 2])
        m["pos_col"] = np.ascontiguousarray(pos[2 * c:2 * c + 2].reshape(2, NT, 128).transpose(2, 0, 1).reshape(128, 2 * NT))
        maps.append(m)
    return maps


ALL_INPUTS = (
    "x", "mem", "positions", "mem_norm", "final_norm",
    "l0_norm", "l0_w_in", "l0_w_mem_kv", "l0_w_out",
    "l1_norm", "l1_w_in", "l1_q_norm", "l1_w_qb", "l1_kv_norm", "l1_w_kvb", "l1_mu_shift", "l1_w0", "l1_w2", "l1_a0",
    "l1_a2", "l1_k_k", "l1_k_a", "l1_r_k", "l1_lnx_w", "l1_lnx_b", "l1_w_mem_kv", "l1_w_out",
    "l2_norm", "l2_w_in", "l2_w_mem_kv", "l2_w_out",
    "l3_norm", "l3_w_in", "l3_q_norm", "l3_w_qb", "l3_kv_norm", "l3_w_kvb", "l3_mu_shift", "l3_w0", "l3_w2", "l3_a0",
    "l3_a2", "l3_v0", "l3_v2", "l3_k_k", "l3_k_a", "l3_r_k", "l3_lnx_w", "l3_lnx_b", "l3_w_mem_kv", "l3_w_out",
)


def kernel(**inputs):
    assert all(n in inputs for n in ALL_INPUTS)
    net = _get_net()
    maps = _in_maps(inputs)
    res = run_bass_kernel_spmd(net.nc, maps, core_ids=list(range(NCORES)))
    return np.concatenate([np.asarray(r["out"], np.float32) for r in res.results], axis=0)
```

```python
import contextlib
import math
import numpy as np
import ml_dtypes
import concourse.bass as bass
import concourse.mybir as mybir
from concourse.bass_utils import run_bass_kernel_spmd

F32 = mybir.dt.float32
BF16 = mybir.dt.bfloat16
I32 = mybir.dt.int32
ALU = mybir.AluOpType
AF = mybir.ActivationFunctionType
AX = mybir.AxisListType
NPBF = ml_dtypes.bfloat16

PE, ACT, DVE, POOL, SP = 0, 1, 2, 3, 4
SEM_ROT = 30000
NCORES = 8
SEQ = 2048
NT = 16
DM = 1024
EPS = 1e-6


class Buf:
    __slots__ = ("w", "r")

    def __init__(self):
        self.w = None
        self.r = []


class T:
    def __init__(self, t, b=None, excl=False):
        self.t = t
        self.b = b if b is not None else Buf()
        self.excl = excl

    def __getitem__(self, idx):
        return self.t[idx]


def _b(x):
    return x.b if isinstance(x, T) else x


class KB:
    def __init__(self, n_dma_slots=14):
        self.nc = bass.Bass("TRN2", target_bir_lowering=False)
        nc = self.nc
        self.es = contextlib.ExitStack()
        self.eng = [nc.tensor, nc.scalar, nc.vector, nc.gpsimd, nc.sync]
        self.esem = []
        self.ecnt = [0] * 5
        self.eepoch = [0] * 5
        for e in range(5):
            self.esem.append([self.es.enter_context(nc.semaphore("e%d_0" % e))])
        self.waited = [dict() for _ in range(5)]
        self.dsem = [self.es.enter_context(nc.semaphore("d%d" % i)) for i in range(n_dma_slots)]
        self.dcnt = [0] * n_dma_slots
        self.dnext = 0
        self.ninst = 0
        self.nwait = 0

    def sb(self, name, shape, dt, es=None):
        self.nsb = getattr(self, "nsb", 0) + 1
        return T((es or self.es).enter_context(self.nc.sbuf_tensor("%s_%d" % (name, self.nsb), list(shape), dt)))

    def ps(self, name, shape, dt=F32):
        return T(self.es.enter_context(self.nc.psum_tensor(name, list(shape), dt)), excl=True)

    def dram(self, name, shape, dt, kind="Internal"):
        return self.nc.dram_tensor(name, list(shape), dt, kind=kind).ap()

    def _wait(self, e, dep):
        kind, idx, val = dep
        if kind == "e" and idx[0] == e and e == PE:
            return
        key = (kind, idx)
        if self.waited[e].get(key, 0) >= val:
            return
        self.waited[e][key] = val
        sem = self.esem[idx[0]][idx[1]] if kind == "e" else self.dsem[idx]
        self.eng[e].wait_ge(sem, val)
        self.nwait += 1

    def _deps(self, e, reads, writes):
        for b in reads:
            if b.w is not None:
                self._wait(e, b.w)
        for b in writes:
            if b.w is not None:
                self._wait(e, b.w)
            for d in b.r:
                self._wait(e, d)

    def _mark(self, tag, reads, writes):
        for b in writes:
            b.w = tag
            b.r = []
        for b in reads:
            if b.w is tag:
                continue
            b.r = [d for d in b.r if not (d[0] == tag[0] and d[1] == tag[1])]
            b.r.append(tag)

    def op(self, e, fn, reads=(), writes=()):
        writes = [_b(x) for x in writes] + [x.b for x in reads if isinstance(x, T) and x.excl]
        reads = [_b(x) for x in reads]
        self._deps(e, reads, writes)
        inst = fn()
        if self.ecnt[e] >= SEM_ROT:
            self.eepoch[e] += 1
            self.ecnt[e] = 0
            self.esem[e].append(self.es.enter_context(
                self.nc.semaphore("e%d_%d" % (e, self.eepoch[e]))))
        self.ecnt[e] += 1
        inst.then_inc(self.esem[e][self.eepoch[e]], 1)
        tag = ("e", (e, self.eepoch[e]), self.ecnt[e])
        self._mark(tag, reads, writes)
        self.ninst += 1
        return inst

    def dma(self, out, in_, reads=(), writes=(), q=SP, **kw):
        reads = [_b(x) for x in reads]
        writes = [_b(x) for x in writes]
        e = q
        self._deps(e, reads, writes)
        s = self.dnext
        self.dnext = (self.dnext + 1) % len(self.dsem)
        if self.dcnt[s] > 0:
            self._wait(e, ("d", s, self.dcnt[s]))
        inst = self.eng[e].dma_start(out=out, in_=in_, **kw)
        self.dcnt[s] += 16
        inst.then_inc(self.dsem[s], 16)
        tag = ("d", s, self.dcnt[s])
        self._mark(tag, reads, writes)
        self.ninst += 1
        return inst

    def barrier(self):
        for e in range(5):
            for o in range(5):
                if o != e and (self.ecnt[o] > 0 or self.eepoch[o] > 0):
                    if self.ecnt[o] > 0:
                        self._wait(e, ("e", (o, self.eepoch[o]), self.ecnt[o]))
                    else:
                        self._wait(e, ("e", (o, self.eepoch[o] - 1), SEM_ROT))
            for s in range(len(self.dsem)):
                if self.dcnt[s] > 0:
                    self._wait(e, ("d", s, self.dcnt[s]))

    def finish(self):
        self.barrier()
        self.es.close()


def _consts():
    c = {}
    c["ident"] = np.eye(128, dtype=np.float32).astype(NPBF)
    kk = np.arange(128)[:, None, None]
    dl = np.arange(16)[None, :, None]
    qq = np.arange(128)[None, None, :]
    d = dl * 128 + qq - kk
    m = ((d >= 0) & (d <= 128)).astype(np.float32)
    m += ((d >= 0) & (d % 4 == 0) & (d <= 512)).astype(np.float32)
    m += ((d >= 0) & (d % 16 == 0) & (d <= 2047 * 16)).astype(np.float32)
    c["maskA"] = m.astype(NPBF)
    c["causal"] = (np.arange(128)[None, :] >= np.arange(128)[:, None]).astype(np.float32).astype(NPBF)
    g = 1.0 - np.exp2(-5.0 - np.arange(4, dtype=np.float64))
    lg = np.log(g)
    kq = (np.arange(128)[None, :] - np.arange(128)[:, None]).astype(np.float64)
    DT = np.where(kq[:, None, :] >= 0, np.exp(np.maximum(kq, 0)[:, None, :] * lg[None, :, None]), 0.0) / 8.0
    c["retDT"] = DT.astype(np.float32)
    FS = np.zeros((128, 2, 128), np.float64)
    G128 = np.zeros((128, 2), np.float64)
    for p in range(2):
        for s in range(2):
            h = 2 * p + s
            FS[64 * s:64 * s + 64, p, :] = np.exp((np.arange(128) + 1.0) * lg[h])[None, :] / 8.0
            G128[64 * s:64 * s + 64, p] = np.exp(128.0 * lg[h])
    c["retFS"] = FS.astype(np.float32)
    c["retG"] = G128.astype(np.float32)
    c["retTE"] = np.exp((127.0 - np.arange(128))[:, None] * lg[None, :]).astype(np.float32)
    def inv(dim, theta):
        return np.exp(-math.log(theta) * np.arange(0, dim, 2, dtype=np.float32) / dim).astype(np.float32)
    iv = np.concatenate([inv(16, 500000.0), inv(64, 10000.0), inv(32, 500000.0)])
    c["ropeinv"] = np.tile(iv[None, :], (128, 1)).astype(np.float32)
    j = np.arange(64)
    t = np.arange(64)
    strict = (t[None, :] > j[:, None]).astype(np.float32)
    incl = (t[None, :] >= j[:, None]).astype(np.float32)
    mS = np.zeros((128, 128), np.float32)
    for seg in range(2):
        mS[seg * 64:(seg + 1) * 64, 0:64] = strict
        mS[seg * 64:(seg + 1) * 64, 64:128] = incl
    mN = np.zeros((128, 128), np.float32)
    mM = np.zeros((128, 128), np.float32)
    for cc in range(2):
        mN[cc * 64:(cc + 1) * 64, cc * 64:(cc + 1) * 64] = strict.T
        mM[cc * 64:(cc + 1) * 64, cc * 64:(cc + 1) * 64] = strict
    c["maskRW"] = np.concatenate([mS, mS, mN, mM], 1).astype(NPBF)
    ob = np.zeros((128, 128), np.float32)
    ob[:64, :64] = 1.0
    ob[64:, 64:] = 1.0
    c["onesblk"] = ob
    rst = np.ones((128, 128), np.float32)
    rst[:, 0] = 0.0
    rst[:, 64] = 0.0
    c["scanrst"] = rst
    return c


CONST_SHAPES = {
    "ident": ([128, 128], BF16), "maskA": ([128, 16, 128], BF16), "causal": ([128, 128], BF16),
    "retDT": ([128, 4, 128], F32), "retFS": ([128, 2, 128], F32), "retG": ([128, 2], F32),
    "retTE": ([128, 4], F32), "ropeinv": ([128, 56], F32),
    "maskRW": ([128, 512], BF16), "onesblk": ([128, 128], F32), "scanrst": ([128, 128], F32),
}

ODD_W = 7184
O_QT, O_QMT, O_SG, O_AR, O_BK, O_BKP, O_VT, O_BON, O_PC, O_BT = 0, 1024, 1280, 2560, 3584, 4608, 5632, 6144, 6656, 6672
NCOLS = 38
C_MU, C_W0, C_A0, C_V0, C_KK, C_KA, C_RK = 0, 14, 18, 22, 26, 30, 34
LN_EPS = 64e-5

EVEN_W = 3328
E_QAT, E_QRT, E_KRT, E_KD, E_VR, E_QMT, E_SG = 0, 512, 768, 1024, 1280, 1792, 2048


class Net:
    def __init__(self, n_layers=4, final_norm=True, stop=None):
        self.stop = stop
        self.n_layers = n_layers
        self.final_norm = final_norm
        self.k = KB()
        k = self.k
        nc = k.nc
        self.nc = nc
        D = lambda name, shape, dt=F32: k.dram(name, shape, dt, "ExternalInput")
        self.x_in = D("x", [2, SEQ, DM])
        self.mem_in = D("mem", [2, 256, DM])
        self.pos_in = D("pos_col", [128, 32], I32)
        self.out = k.dram("out", [2, SEQ, DM], F32, "ExternalOutput")
        self.cst = {n: D("c_" + n, s, dt) for n, (s, dt) in CONST_SHAPES.items()}
        self.w = {}
        self.w["mem_norm_col"] = D("mem_norm_col", [128, 8])
        self.w["final_norm"] = D("final_norm", [1, DM])
        for l in range(4):
            p = "l%d_" % l
            self.w[p + "norm_col"] = D(p + "norm_col", [128, 8])
            self.w[p + "w_mem_kv"] = D(p + "w_mem_kv", [DM, 512])
            self.w[p + "w_out"] = D(p + "w_out", [1280, DM])
            if l % 2 == 0:
                self.w[p + "w_in"] = D(p + "w_in", [DM, 4096])
            else:
                dc = 1664 if l == 1 else 1696
                self.w[p + "w_in"] = D(p + "w_in", [DM, 416 + dc + 256 + 1280])
                self.w[p + "qn_col"] = D(p + "qn_col", [128, 2])
                self.w[p + "kvn_col"] = D(p + "kvn_col", [128, 1])
                self.w[p + "w_qb"] = D(p + "w_qb", [256, 768])
                self.w[p + "w_kvb"] = D(p + "w_kvb", [128, 1024])
                self.w[p + "cols"] = D(p + "cols", [128, NCOLS])
                self.w[p + "w2"] = D(p + "w2", [64, 512])
                self.w[p + "a2"] = D(p + "a2", [64, 512])
                if l == 3:
                    self.w[p + "v2"] = D(p + "v2", [32, 512])
                self.w[p + "lnx_w"] = D(p + "lnx_w", [1, 512])
                self.w[p + "lnx_b"] = D(p + "lnx_b", [1, 512])
        self.s_kTM = k.dram("s_kTM", [2, 96, 8, SEQ], BF16)
        self.s_vf = k.dram("s_vf", [2, NT, 128, 512], F32)
        self.s_pc = k.dram("s_pc", [2, NT, 128, 8], F32)
        self.s_pk = k.dram("s_pk", [2, NT, 128, ODD_W], BF16)
        self.s_kT = k.dram("s_kT", [2, 128, 4, SEQ], BF16)
        self.s_vA = k.dram("s_vA", [2, NT, 128, 8 * 65], BF16)
        self.s_memT = k.dram("s_memT", [2, 128, 8, 256], BF16)
        self.xbuf = [[Buf() for _ in range(NT)] for _ in range(2)]
        self.P = [k.ps("bank%d" % i, [128, 512], F32) for i in range(8)]
        self.ident = k.sb("ident", [128, 128], BF16)
        self.rot = k.sb("rot", [128, 2 * NT, 2, 56], F32)
        k.dma(self.ident[:], self.cst["ident"][:, :], writes=[self.ident])
        self._rope_tables()
        if stop == "rope":
            k.finish(); return
        self._mem_prep()
        if stop == "mem":
            k.finish(); return
        for l in range(n_layers):
            if l % 2 == 0:
                self._even_layer(l)
            else:
                self._odd_layer(l)
        k.finish()

    def _rope_tables(self):
        k, nc = self.k, self.nc
        with contextlib.ExitStack() as es:
            pi = k.sb("pos_i", [128, 32], I32, es)
            pf = k.sb("pos_f", [128, 32], F32, es)
            inv = k.sb("ropeinv", [128, 56], F32, es)
            ang = k.sb("ang", [128, 32, 56], F32, es)
            nf = k.sb("nf", [128, 32, 56], F32, es)
            ni = k.sb("ni", [128, 32, 56], I32, es)
            msk = k.sb("msk", [128, 32, 56], F32, es)
            k.dma(pi[:], self.pos_in[:, :], writes=[pi])
            k.dma(inv[:], self.cst["ropeinv"][:, :], writes=[inv])
            k.op(DVE, lambda: nc.vector.tensor_copy(out=pf[:], in_=pi[:]), [pi], [pf])
            k.op(DVE, lambda: nc.vector.tensor_tensor(
                out=ang[:], in0=pf[:].unsqueeze(2).to_broadcast([128, 32, 56]),
                in1=inv[:].unsqueeze(1).to_broadcast([128, 32, 56]), op=ALU.mult), [pf, inv], [ang])
            TWO_PI = 2.0 * math.pi
            C1 = 6.28125
            C2 = TWO_PI - C1

            def reduce_and_sin(shift, dst):
                k.op(DVE, lambda: nc.vector.tensor_scalar(out=nf[:], in0=ang[:], scalar1=1.0 / TWO_PI,
                                                          scalar2=shift / TWO_PI, op0=ALU.mult, op1=ALU.add),
                     [ang], [nf])
                k.op(DVE, lambda: nc.vector.tensor_copy(out=ni[:], in_=nf[:]), [nf], [ni])
                k.op(DVE, lambda: nc.vector.tensor_copy(out=nf[:], in_=ni[:]), [ni], [nf])
                k.op(DVE, lambda: nc.vector.scalar_tensor_tensor(out=msk[:], in0=nf[:], scalar=-C1, in1=ang[:],
                                                                 op0=ALU.mult, op1=ALU.add), [nf, ang], [msk])
                k.op(DVE, lambda: nc.vector.scalar_tensor_tensor(out=msk[:], in0=nf[:], scalar=-C2, in1=msk[:],
                                                                 op0=ALU.mult, op1=ALU.add), [nf, msk], [msk])
                if shift != 0.0:
                    k.op(DVE, lambda: nc.vector.tensor_scalar(out=msk[:], in0=msk[:], scalar1=shift, scalar2=None,
                                                              op0=ALU.add), [msk], [msk])
                k.op(DVE, lambda: nc.vector.tensor_scalar(out=nf[:], in0=msk[:], scalar1=math.pi, scalar2=-TWO_PI,
                                                          op0=ALU.is_gt, op1=ALU.mult), [msk], [nf])
                k.op(DVE, lambda: nc.vector.tensor_tensor(out=msk[:], in0=msk[:], in1=nf[:], op=ALU.add),
                     [msk, nf], [msk])
                k.op(DVE, lambda: nc.vector.tensor_scalar(out=nf[:], in0=msk[:], scalar1=-math.pi, scalar2=TWO_PI,
                                                          op0=ALU.is_lt, op1=ALU.mult), [msk], [nf])
                k.op(DVE, lambda: nc.vector.tensor_tensor(out=msk[:], in0=msk[:], in1=nf[:], op=ALU.add),
                     [msk, nf], [msk])
                k.op(DVE, lambda: nc.vector.tensor_scalar(out=msk[:], in0=msk[:], scalar1=3.1415925, scalar2=-3.1415925,
                                                          op0=ALU.min, op1=ALU.max), [msk], [msk])
                k.op(ACT, lambda: nc.scalar.activation(out=dst, in_=msk[:], func=AF.Sin), [msk], [self.rot])

            reduce_and_sin(math.pi / 2.0, self.rot[:, :, 0, :])
            reduce_and_sin(0.0, self.rot[:, :, 1, :])
            k.barrier()

    def _mem_prep(self):
        k, nc = self.k, self.nc
        P = self.P
        with contextlib.ExitStack() as es:
            gcol = k.sb("memg", [128, 8], F32, es)
            k.dma(gcol[:], self.w["mem_norm_col"][:, :], writes=[gcol])
            mt = k.sb("mem_t", [128, DM], F32, es)
            junk = k.sb("mem_junk", [128, DM], BF16, es)
            st = k.sb("mem_st", [128, 4], F32, es)
            hb = k.sb("mem_h", [128, DM], BF16, es)
            mT = k.sb("mem_T", [128, 8, 256], BF16, es)
            pb = P[2][:].bitcast(BF16)
            for b in range(2):
                for mb in range(2):
                    k.dma(mt[:], self.mem_in[b, mb * 128:(mb + 1) * 128, :], writes=[mt])
                    self._rms_to_bf16(mt, junk, st, hb, DM)
                    for c in range(8):
                        k.op(PE, lambda: nc.tensor.transpose(out=pb[:, c * 128:(c + 1) * 128],
                                                             in_=hb[:, c * 128:(c + 1) * 128], identity=self.ident[:]),
                             [hb, self.ident], [P[2]])
                    for c in range(8):
                        k.op(DVE, lambda: nc.vector.tensor_scalar(out=mT[:, c, mb * 128:(mb + 1) * 128],
                                                                  in0=pb[:, c * 128:(c + 1) * 128],
                                                                  scalar1=gcol[:, c:c + 1], scalar2=None, op0=ALU.mult),
                             [P[2], gcol], [mT])
                k.dma(self.s_memT[b], mT[:], reads=[mT], writes=[])
            k.barrier()

    def _rms_to_bf16(self, xt, junk, st, hb, n, eps=EPS):
        k, nc = self.k, self.nc
        k.op(ACT, lambda: nc.scalar.activation(out=junk[:, 0:n], in_=xt[:, 0:n], func=AF.Square, accum_out=st[:, 0:1]),
             [xt], [junk, st])
        k.op(ACT, lambda: nc.scalar.activation(out=st[:, 1:2], in_=st[:, 0:1], func=AF.Sqrt, scale=1.0 / n, bias=eps),
             [st], [st])
        k.op(DVE, lambda: nc.vector.reciprocal(out=st[:, 2:3], in_=st[:, 1:2]), [st], [st])
        k.op(DVE, lambda: nc.vector.tensor_scalar(out=hb[:, 0:n], in0=xt[:, 0:n], scalar1=st[:, 2:3], scalar2=None,
                                                  op0=ALU.mult), [xt, st], [hb])

    def _load_w(self, es, name, dram, rows, cols, gcol=None, wb=None):
        k, nc = self.k, self.nc
        nch = rows // 128
        if wb is None:
            wb = k.sb(name, [128, nch, cols], BF16, es)
        CW = 2048
        i = 0
        for c in range(nch):
            for c0 in range(0, cols, CW):
                cw = min(CW, cols - c0)
                stg = self.stage[i % 2]
                k.dma(stg[:, 0:cw], dram[c * 128:(c + 1) * 128, c0:c0 + cw], writes=[stg])
                eng = POOL if i % 2 == 0 else ACT
                if gcol is None:
                    if eng == POOL:
                        k.op(POOL, lambda: nc.gpsimd.tensor_copy(out=wb[:, c, c0:c0 + cw], in_=stg[:, 0:cw]), [stg], [wb])
                    else:
                        k.op(ACT, lambda: nc.scalar.copy(out=wb[:, c, c0:c0 + cw], in_=stg[:, 0:cw]), [stg], [wb])
                else:
                    if eng == POOL:
                        k.op(POOL, lambda: nc.gpsimd.tensor_scalar(out=wb[:, c, c0:c0 + cw], in0=stg[:, 0:cw],
                                                                   scalar1=gcol[:, c:c + 1], scalar2=1.0,
                                                                   op0=ALU.mult, op1=ALU.mult), [stg, gcol], [wb])
                    else:
                        k.op(ACT, lambda: nc.scalar.activation(out=wb[:, c, c0:c0 + cw], in_=stg[:, 0:cw],
                                                               func=AF.Copy, scale=gcol[:, c:c + 1]), [stg, gcol], [wb])
                i += 1
        return wb

    def _rotary(self, xv, tile_idx, f0, nf, tA, tB, out1, out2, reads, writes, nh):
        k, nc = self.k, self.nc
        cs = self.rot[:, tile_idx, 0, f0:f0 + nf].unsqueeze(1).unsqueeze(1).to_broadcast([128, nh, 2, nf])
        sn = self.rot[:, tile_idx, 1, f0:f0 + nf].unsqueeze(1).unsqueeze(1).to_broadcast([128, nh, 2, nf])
        a = tA[:, 0:nh * 2 * nf].rearrange("p (h t f) -> p h t f", h=nh, t=2)
        bq = tB[:, 0:nh * 2 * nf].rearrange("p (h t f) -> p h t f", h=nh, t=2)
        k.op(DVE, lambda: nc.vector.tensor_tensor(out=a, in0=xv, in1=cs, op=ALU.mult), reads + [self.rot], [tA])
        k.op(DVE, lambda: nc.vector.tensor_tensor(out=bq, in0=xv, in1=sn, op=ALU.mult), reads + [self.rot], [tB])
        k.op(POOL, lambda: nc.gpsimd.tensor_tensor(out=out1, in0=a[:, :, 0, :], in1=bq[:, :, 1, :], op=ALU.subtract),
             [tA, tB], writes)
        k.op(POOL, lambda: nc.gpsimd.tensor_tensor(out=out2, in0=a[:, :, 1, :], in1=bq[:, :, 0, :], op=ALU.add),
             [tA, tB], writes)

    def _mem_kv(self, es, wmem, tagp):
        k, nc = self.k, self.nc
        P = self.P
        kmT = k.sb(tagp + "kmT", [128, 2, 2, 256], BF16, es)
        vm = k.sb(tagp + "vm", [128, 2, 2, 4, 65], BF16, es)
        k.op(POOL, lambda: nc.gpsimd.memset(vm[:], 1.0), [], [vm])
        with contextlib.ExitStack() as es2:
            mT = k.sb(tagp + "memT", [128, 8, 256], BF16, es2)
            for b in range(2):
                k.dma(mT[:], self.s_memT[b], writes=[mT])
                for p in range(2):
                    for c in range(8):
                        k.op(PE, lambda: nc.tensor.matmul(P[0][:, p * 256:(p + 1) * 256],
                                                          lhsT=wmem[:, c, p * 128:(p + 1) * 128], rhs=mT[:, c, :],
                                                          start=(c == 0), stop=(c == 7)), [wmem, mT], [P[0]])
                k.op(ACT, lambda: nc.scalar.copy(out=kmT[:, b, :, :].rearrange("p a m -> p (a m)"), in_=P[0][:, 0:512]),
                     [P[0]], [kmT])
                for mb in range(2):
                    for c in range(8):
                        k.op(PE, lambda: nc.tensor.matmul(P[1][:, mb * 256:(mb + 1) * 256],
                                                          lhsT=mT[:, c, mb * 128:(mb + 1) * 128], rhs=wmem[:, c, 256:512],
                                                          start=(c == 0), stop=(c == 7)), [wmem, mT], [P[1]])
                for mb in range(2):
                    k.op(DVE, lambda: nc.vector.tensor_copy(
                        out=vm[:, b, mb, :, 0:64],
                        in_=P[1][:, mb * 256:(mb + 1) * 256].rearrange("p (h d) -> p h d", h=4)), [P[1]], [vm])
            k.barrier()
        return kmT, vm

    def _even_layer(self, l):
        k, nc = self.k, self.nc
        P = self.P
        pre = "l%d_" % l
        last = (l == self.n_layers - 1)
        src = self.x_in if l == 0 else self.out
        pb2 = P[2][:].bitcast(BF16)
        with contextlib.ExitStack() as es:
            self.stage = [k.sb("stgA", [128, 2048], F32, es), k.sb("stgB", [128, 2048], F32, es)]
            gcol = k.sb("gcol", [128, 8], F32, es)
            k.dma(gcol[:], self.w[pre + "norm_col"][:, :], writes=[gcol])
            wb = self._load_w(es, "wb", self.w[pre + "w_in"], DM, 4096, gcol)
            retTE = k.sb("retTE", [128, 4], F32, es)
            k.dma(retTE[:], self.cst["retTE"][:, :], writes=[retTE])
            xt = [k.sb("xt%d" % i, [128, DM], F32, es) for i in range(2)]
            junk = k.sb("junk", [128, DM], BF16, es)
            st = k.sb("st", [128, 4], F32, es)
            hb = k.sb("hb", [128, DM], BF16, es)
            hT = k.sb("hT", [128, 8, 128], BF16, es)
            pk = [k.sb("pk%d" % i, [128, EVEN_W], BF16, es) for i in range(2)]
            qk_b = k.sb("qk_b", [128, 2, 8, 64], BF16, es)
            qkr_b = k.sb("qkr_b", [128, 8, 64], BF16, es)
            vA_t = [k.sb("vA_t%d" % i, [128, 8, 65], BF16, es) for i in range(2)]
            kT_t = [k.sb("kT_t%d" % i, [128, 4, 128], BF16, es) for i in range(2)]
            tA = k.sb("rotA", [128, 512], F32, es)
            tB = k.sb("rotB", [128, 512], F32, es)
            for i in range(2):
                k.op(POOL, lambda: nc.gpsimd.memset(vA_t[i][:], 1.0), [], [vA_t[i]])
            tiles = [(b, i) for b in range(2) for i in range(NT)]
            k.dma(xt[0][:], src[0, 0:128, :], reads=[self.xbuf[0][0]], writes=[xt[0]])
            for ti, (b, i) in enumerate(tiles):
                X = xt[ti % 2]
                PK = pk[ti % 2]
                VA = vA_t[ti % 2]
                KT = kT_t[ti % 2]
                if ti + 1 < len(tiles):
                    nb, ni_ = tiles[ti + 1]
                    k.dma(xt[(ti + 1) % 2][:], src[nb, ni_ * 128:(ni_ + 1) * 128, :],
                          reads=[self.xbuf[nb][ni_]], writes=[xt[(ti + 1) % 2]])
                tix = b * NT + i
                self._rms_to_bf16(X, junk, st, hb, DM)
                for c in range(8):
                    k.op(PE, lambda: nc.tensor.transpose(out=pb2[:, c * 128:(c + 1) * 128],
                                                         in_=hb[:, c * 128:(c + 1) * 128], identity=self.ident[:]),
                         [hb, self.ident], [P[2]])
                k.op(ACT, lambda: nc.scalar.copy(out=hT[:].rearrange("p c t -> p (c t)"), in_=pb2[:, 0:1024]),
                     [P[2]], [hT])
                for n in range(8):
                    bk = P[n % 2]
                    for c in range(8):
                        k.op(PE, lambda: nc.tensor.matmul(bk[:, :], lhsT=hT[:, c, :], rhs=wb[:, c, n * 512:(n + 1) * 512],
                                                          start=(c == 0), stop=(c == 7)), [hT, wb], [bk])
                    if n in (0, 1):
                        dst = qk_b[:, n, :, :]
                        k.op(ACT, lambda: nc.scalar.copy(out=dst, in_=bk[:, :].rearrange("p (h d) -> p h d", h=8)),
                             [bk], [qk_b])
                        xv = bk[:, :].rearrange("p (h d) -> p h d", h=8)[:, :, 0:16].rearrange(
                            "p h (t f) -> p h t f", t=2)
                        self._rotary(xv, tix, 0, 8, tA, tB, qk_b[:, n, :, 0:8], qk_b[:, n, :, 8:16], [bk], [qk_b], 8)
                    elif n == 2:
                        k.op(ACT, lambda: nc.scalar.copy(out=VA[:, :, 0:64],
                                                         in_=bk[:, :].rearrange("p (h d) -> p h d", h=8)), [bk], [VA])
                    elif n == 3:
                        xv = bk[:, :].rearrange("p (h t f) -> p h t f", h=8, t=2)
                        self._rotary(xv, tix, 8, 32, tA, tB, qkr_b[:, :, 0:32], qkr_b[:, :, 32:64], [bk], [qkr_b], 8)
                    elif n == 4:
                        k.op(ACT, lambda: nc.scalar.copy(out=PK[:, E_VR:E_VR + 512], in_=bk[:, :]), [bk], [PK])
                    elif n == 5:
                        k.op(DVE, lambda: nc.vector.tensor_copy(out=junk[:, 0:256], in_=bk[:, 0:256]), [bk], [junk])
                        k.op(ACT, lambda: nc.scalar.activation(out=PK[:, E_SG:E_SG + 256], in_=bk[:, 256:512],
                                                               func=AF.Silu), [bk], [PK])
                    else:
                        o = E_SG + 256 + (n - 6) * 512
                        k.op(ACT, lambda: nc.scalar.activation(out=PK[:, o:o + 512], in_=bk[:, :], func=AF.Silu),
                             [bk], [PK])
                qkf = qk_b[:].rearrange("p a h d -> p (a h d)")
                for c in range(8):
                    k.op(PE, lambda: nc.tensor.transpose(out=pb2[:, c * 128:(c + 1) * 128],
                                                         in_=qkf[:, c * 128:(c + 1) * 128], identity=self.ident[:]),
                         [qk_b, self.ident], [P[2]])
                k.op(ACT, lambda: nc.scalar.copy(out=PK[:, E_QAT:E_QAT + 512], in_=pb2[:, 0:512]), [P[2]], [PK])
                k.op(DVE, lambda: nc.vector.tensor_copy(out=KT[:].rearrange("p a t -> p (a t)"), in_=pb2[:, 512:1024]),
                     [P[2]], [KT])
                k.op(DVE, lambda: nc.vector.tensor_tensor(
                    out=PK[:, E_KD:E_KD + 256].rearrange("p (h d) -> p h d", h=4), in0=qkr_b[:, 4:8, :],
                    in1=retTE[:].unsqueeze(2).to_broadcast([128, 4, 64]), op=ALU.mult), [qkr_b, retTE], [PK])
                qrf = qkr_b[:].rearrange("p h d -> p (h d)")
                for c in range(4):
                    k.op(PE, lambda: nc.tensor.transpose(out=pb2[:, c * 128:(c + 1) * 128],
                                                         in_=qrf[:, c * 128:(c + 1) * 128], identity=self.ident[:]),
                         [qkr_b, self.ident], [P[2]])
                for c in range(2):
                    k.op(PE, lambda: nc.tensor.transpose(out=pb2[:, (4 + c) * 128:(5 + c) * 128],
                                                         in_=junk[:, c * 128:(c + 1) * 128], identity=self.ident[:]),
                         [junk, self.ident], [P[2]])
                k.op(ACT, lambda: nc.scalar.copy(out=PK[:, E_QRT:E_QRT + 512], in_=pb2[:, 0:512]), [P[2]], [PK])
                k.op(DVE, lambda: nc.vector.tensor_copy(out=PK[:, E_QMT:E_QMT + 256], in_=pb2[:, 512:768]), [P[2]], [PK])
                k.dma(self.s_pk[b, i, :, 0:EVEN_W], PK[:], reads=[PK])
                k.dma(self.s_kT[b, :, :, i * 128:(i + 1) * 128], KT[:], reads=[KT])
                k.dma(self.s_vA[b, i], VA[:].rearrange("p h d -> p (h d)"), reads=[VA])
            k.barrier()
        if self.stop == "p1":
            return
        with contextlib.ExitStack() as es:
            self.stage = [k.sb("stgA2", [128, 2048], F32, es), k.sb("stgB2", [128, 2048], F32, es)]
            wout = self._load_w(es, "wout", self.w[pre + "w_out"], 1280, DM)
            wmem = self._load_w(es, "wmem", self.w[pre + "w_mem_kv"], DM, 512)
            kmT, vm = self._mem_kv(es, wmem, "e")
            maskA = k.sb("maskA", [128, 16, 128], BF16, es)
            retDT = k.sb("retDT", [128, 4, 128], F32, es)
            retFS = k.sb("retFS", [128, 2, 128], F32, es)
            retG = k.sb("retG", [128, 2], F32, es)
            k.dma(maskA[:], self.cst["maskA"], writes=[maskA])
            k.dma(retDT[:], self.cst["retDT"], writes=[retDT])
            k.dma(retFS[:], self.cst["retFS"], writes=[retFS])
            k.dma(retG[:], self.cst["retG"], writes=[retG])
            if last and self.final_norm:
                gfin = k.sb("gfin", [128, DM], F32, es)
                k.dma(gfin[:], self.w["final_norm"][0:1, :].broadcast_to([128, DM]), writes=[gfin])
            kT = k.sb("kTc", [128, 4, SEQ], BF16, es)
            vA = k.sb("vAc", [128, NT, 8, 65], BF16, es)
            xt = [k.sb("xt2_%d" % i, [128, DM], F32, es) for i in range(3)]
            pk = [k.sb("pk2_%d" % i, [128, EVEN_W], BF16, es) for i in range(3)]
            pT = [[k.sb("pT%d_%d" % (i, j), [128, 4, 128], BF16, es) for j in range(2)] for i in range(2)]
            SBANKS = [(P[3], P[4]), (P[0], P[1])]
            sTb = k.sb("sTb", [128, 4, 128], BF16, es)
            qs = k.sb("qs", [128, 2, 128], BF16, es)
            st_f = k.sb("st_f", [128, 2, 128], F32, es)
            st_b = k.sb("st_b", [128, 2, 128], BF16, es)
            rec = k.sb("rec", [128, 16], F32, es)
            y2 = [k.sb("y%d" % i, [128, 1280], F32, es) for i in range(2)]
            ob = k.sb("ob", [128, 512], F32, es)
            sq = k.sb("sq", [128, 512], F32, es)
            yg = k.sb("yg", [128, 1280], BF16, es)
            ygT = k.sb("ygT", [128, 10, 128], BF16, es)
            xo = k.sb("xo", [128, DM], F32, es)
            junk = k.sb("junk2", [128, DM], BF16, es)
            st = k.sb("st2", [128, 8], F32, es)
            tiles = [(b, i) for b in range(2) for i in range(NT)]

            def prefetch(ti):
                b_, i_ = tiles[ti]
                k.dma(xt[ti % 3][:], src[b_, i_ * 128:(i_ + 1) * 128, :], reads=[self.xbuf[b_][i_]], writes=[xt[ti % 3]])
                k.dma(pk[ti % 3][:], self.s_pk[b_, i_, :, 0:EVEN_W], writes=[pk[ti % 3]])

            NTL = len(tiles)
            prefetch(0)
            if NTL > 1:
                prefetch(1)
            scl = [0]

            def T1(ti, b, i):
                PK = pk[ti % 3]
                y = y2[ti % 2]
                sc = scl[0]
                if i == 0:
                    k.dma(kT[:], self.s_kT[b], writes=[kT])
                    k.dma(vA[:].rearrange("p i h d -> p i (h d)"), self.s_vA[b].rearrange("i p w -> p i w"), writes=[vA])
                qaT = PK[:, E_QAT:E_QAT + 512].rearrange("p (a t) -> p a t", a=4)
                qrT = PK[:, E_QRT:E_QRT + 256].rearrange("p (a t) -> p a t", a=2)
                krT = PK[:, E_KRT:E_KRT + 256].rearrange("p (a t) -> p a t", a=2)
                kd = PK[:, E_KD:E_KD + 256].rearrange("p (h d) -> p h d", h=4)
                vr = PK[:, E_VR:E_VR + 512].rearrange("p (h e) -> p h e", h=4)
                qmT = PK[:, E_QMT:E_QMT + 256].rearrange("p (a t) -> p a t", a=2)
                sg = PK[:, E_SG:E_SG + 1280]
                first = [True, True]

                def qk_a(j, sc_):
                    Sb_ = SBANKS[sc_ % 2]
                    for p_ in range(4):
                        for s_ in range(2):
                            k.op(PE, lambda: nc.tensor.matmul(Sb_[s_][:, p_ * 128:(p_ + 1) * 128],
                                                              lhsT=kT[64 * s_:64 * s_ + 64, p_, j * 128:(j + 1) * 128],
                                                              rhs=qaT[64 * s_:64 * s_ + 64, p_, :], start=True, stop=True),
                                 [kT, PK], [Sb_[s_]])

                qk_a(0, sc)
                for j in range(i + 1):
                    Sb = SBANKS[sc % 2]
                    ptp = pT[sc % 2]
                    sc += 1
                    scl[0] = sc
                    if j + 1 <= i:
                        qk_a(j + 1, sc)
                    for s_ in range(2):
                        pt = ptp[s_]
                        k.op(ACT, lambda: nc.scalar.activation(out=pt[:].rearrange("p h q -> p (h q)"), in_=Sb[s_][:, :],
                                                               func=AF.Exp, scale=0.125), [Sb[s_]], [pt])
                        k.op(DVE if s_ == 0 else POOL, lambda: (nc.vector if s_ == 0 else nc.gpsimd).tensor_tensor(
                            out=pt[:], in0=pt[:], in1=maskA[:, i - j, :].unsqueeze(1).to_broadcast([128, 4, 128]),
                            op=ALU.mult), [pt, maskA], [pt])
                        O = P[5 + s_]
                        for p_ in range(4):
                            k.op(PE, lambda: nc.tensor.matmul(O[:, p_ * 65:(p_ + 1) * 65], lhsT=pt[:, p_, :],
                                                              rhs=vA[:, j, 2 * p_ + s_, :], start=first[s_], stop=(j == i),
                                                              skip_group_check=True), [pt, vA], [O])
                            first[s_] = False
                    yield
                for s_ in range(2):
                    O = P[5 + s_]
                    ov = O[:, 0:260].rearrange("p (h d) -> p h d", h=4)
                    k.op(DVE, lambda: nc.vector.reciprocal(out=rec[:, s_ * 4:s_ * 4 + 4], in_=ov[:, :, 64]), [O], [rec])
                    k.op(DVE, lambda: nc.vector.tensor_tensor(
                        out=y[:, 0:512].rearrange("p (a s d) -> p a s d", a=4, s=2)[:, :, s_, :], in0=ov[:, :, 0:64],
                        in1=rec[:, s_ * 4:s_ * 4 + 4].unsqueeze(2).to_broadcast([128, 4, 64]), op=ALU.mult),
                        [O, rec], [y])
                scl[0] = sc
                yield

            def T2(ti, b, i):
                PK = pk[ti % 3]
                y = y2[ti % 2]
                sc = scl[0]
                qaT = PK[:, E_QAT:E_QAT + 512].rearrange("p (a t) -> p a t", a=4)
                qrT = PK[:, E_QRT:E_QRT + 256].rearrange("p (a t) -> p a t", a=2)
                krT = PK[:, E_KRT:E_KRT + 256].rearrange("p (a t) -> p a t", a=2)
                kd = PK[:, E_KD:E_KD + 256].rearrange("p (h d) -> p h d", h=4)
                vr = PK[:, E_VR:E_VR + 512].rearrange("p (h e) -> p h e", h=4)
                qmT = PK[:, E_QMT:E_QMT + 256].rearrange("p (a t) -> p a t", a=2)
                sg = PK[:, E_SG:E_SG + 1280]
                Sb = SBANKS[sc % 2]
                sc += 1
                for h in range(4):
                    s_, p_ = h % 2, h // 2
                    k.op(PE, lambda: nc.tensor.matmul(Sb[s_][:, p_ * 128:(p_ + 1) * 128], lhsT=krT[64 * s_:64 * s_ + 64, p_, :],
                                                      rhs=qrT[64 * s_:64 * s_ + 64, p_, :], start=True, stop=True),
                         [PK], [Sb[s_]])
                for s_ in range(2):
                    k.op(DVE, lambda: nc.vector.tensor_tensor(
                        out=sTb[:].rearrange("p (a s) q -> p a s q", s=2)[:, :, s_, :],
                        in0=Sb[s_][:, 0:256].rearrange("p (a q) -> p a q", a=2),
                        in1=retDT[:].rearrange("p (a s) q -> p a s q", s=2)[:, :, s_, :], op=ALU.mult),
                        [Sb[s_], retDT], [sTb])
                if i > 0:
                    k.op(POOL, lambda: nc.gpsimd.tensor_tensor(out=qs[:], in0=qrT, in1=retFS[:], op=ALU.mult),
                         [PK, retFS], [qs])
                OB = P[0]
                for h in range(4):
                    s_, p_ = h % 2, h // 2
                    k.op(PE, lambda: nc.tensor.matmul(OB[:, h * 128:(h + 1) * 128], lhsT=sTb[:, h, :], rhs=vr[:, h, :],
                                                      start=True, stop=(i == 0)), [sTb, PK], [OB])
                    if i > 0:
                        k.op(PE, lambda: nc.tensor.matmul(OB[:, h * 128:(h + 1) * 128], lhsT=qs[64 * s_:64 * s_ + 64, p_, :],
                                                          rhs=st_b[64 * s_:64 * s_ + 64, p_, :], start=False, stop=True),
                             [qs, st_b], [OB])
                SU = P[1]
                for h in range(4):
                    s_, p_ = h % 2, h // 2
                    k.op(PE, lambda: nc.tensor.matmul(SU[64 * s_:64 * s_ + 64, p_ * 128:(p_ + 1) * 128], lhsT=kd[:, h, :],
                                                      rhs=vr[:, h, :], start=True, stop=True), [PK], [SU])
                if i == 0:
                    k.op(DVE, lambda: nc.vector.tensor_copy(out=st_f[:].rearrange("p a e -> p (a e)"), in_=SU[:, 0:256]),
                         [SU], [st_f])
                else:
                    for p_ in range(2):
                        k.op(DVE, lambda: nc.vector.scalar_tensor_tensor(
                            out=st_f[:, p_, :], in0=st_f[:, p_, :], scalar=retG[:, p_:p_ + 1],
                            in1=SU[:, p_ * 128:(p_ + 1) * 128], op0=ALU.mult, op1=ALU.add), [st_f, retG, SU], [st_f])
                k.op(ACT, lambda: nc.scalar.copy(out=ob[:], in_=OB[:, :]), [OB], [ob])
                k.op(POOL, lambda: nc.gpsimd.tensor_copy(out=st_b[:], in_=st_f[:]), [st_f], [st_b])
                k.op(POOL, lambda: nc.gpsimd.tensor_tensor(out=sq[:], in0=ob[:], in1=ob[:], op=ALU.mult), [ob], [sq])
                k.op(DVE, lambda: nc.vector.tensor_reduce(out=st[:, 0:4], in_=sq[:].rearrange("p (h e) -> p h e", h=4),
                                                          axis=AX.X, op=ALU.add), [sq], [st])
                k.op(ACT, lambda: nc.scalar.activation(out=st[:, 4:8], in_=st[:, 0:4], func=AF.Sqrt, scale=1.0 / 128,
                                                       bias=EPS), [st], [st])
                k.op(DVE, lambda: nc.vector.reciprocal(out=st[:, 0:4], in_=st[:, 4:8]), [st], [st])
                k.op(DVE, lambda: nc.vector.tensor_tensor(
                    out=y[:, 512:1024].rearrange("p (h e) -> p h e", h=4), in0=ob[:].rearrange("p (h e) -> p h e", h=4),
                    in1=st[:, 0:4].unsqueeze(2).to_broadcast([128, 4, 128]), op=ALU.mult), [ob, st], [y])
                self._mem_attn(b, qmT, PK, kmT, vm, pT, rec, y, sc, SBANKS)
                sc += 2
                scl[0] = sc

            def T3(ti, b, i):
                PK = pk[ti % 3]
                sg = PK[:, E_SG:E_SG + 1280]
                return self._tail_gen(PK, sg, y2[ti % 2], yg, ygT, wout, xt[ti % 3], xo, junk, st, b, i, last,
                                      gfin if (last and self.final_norm) else None, ob=7)

            def drain(*gens):
                alive = [g for g in gens if g is not None]
                while alive:
                    for g in list(alive):
                        try:
                            next(g)
                        except StopIteration:
                            alive.remove(g)

            drain(T1(0, *tiles[0]))
            T2(0, *tiles[0])
            for ti in range(NTL):
                if ti + 2 < NTL:
                    prefetch(ti + 2)
                drain(T3(ti, *tiles[ti]), T1(ti + 1, *tiles[ti + 1]) if ti + 1 < NTL else None)
                if ti + 1 < NTL:
                    T2(ti + 1, *tiles[ti + 1])
            k.barrier()

    def _odd_layer(self, l):
        k, nc = self.k, self.nc
        P = self.P
        pre = "l%d_" % l
        last = (l == self.n_layers - 1)
        src = self.out
        dc = 1664 if l == 1 else 1696
        ntd = (dc + 127) // 128
        c_qm = 416 + dc
        ncols = c_qm + 1536
        vres = (l == 3)
        pb2 = P[2][:].bitcast(BF16)
        tiles = [(b, i) for b in range(2) for i in range(NT)]
        NEG_E = -math.exp(-0.5)
        with contextlib.ExitStack() as es:
            self.stage = [k.sb("stgA", [128, 2048], F32, es), k.sb("stgB", [128, 2048], F32, es)]
            gcol = k.sb("gcol", [128, 8], F32, es)
            qn = k.sb("qn", [128, 2], F32, es)
            kvn = k.sb("kvn", [128, 1], F32, es)
            cols = k.sb("cols", [128, NCOLS], F32, es)
            k.dma(gcol[:], self.w[pre + "norm_col"][:, :], writes=[gcol])
            k.dma(qn[:], self.w[pre + "qn_col"][:, :], writes=[qn])
            k.dma(kvn[:], self.w[pre + "kvn_col"][:, :], writes=[kvn])
            k.dma(cols[:], self.w[pre + "cols"][:, :], writes=[cols])
            wb = self._load_w(es, "wbo", self.w[pre + "w_in"], DM, ncols, gcol)
            wqb = self._load_w(es, "wqb", self.w[pre + "w_qb"], 256, 768, qn)
            wkvb = self._load_w(es, "wkvb", self.w[pre + "w_kvb"], 128, 1024, kvn)
            w2sb = k.sb("w2sb", [128, 512], F32, es)
            a2sb = k.sb("a2sb", [128, 512], F32, es)
            k.dma(w2sb[0:64, :], self.w[pre + "w2"][:, :], writes=[w2sb])
            k.dma(a2sb[64:128, :], self.w[pre + "a2"][:, :], writes=[a2sb])
            if vres:
                v2sb = k.sb("v2sb", [128, 512], F32, es)
                k.dma(v2sb[0:32, :], self.w[pre + "v2"][:, :], writes=[v2sb])
                vft = [k.sb("vft%d" % i, [128, 4, 128], F32, es) for i in range(2)]
            onesblk = k.sb("onesblk", [128, 128], F32, es)
            scanrst = k.sb("scanrst", [128, 128], F32, es)
            k.dma(onesblk[:], self.cst["onesblk"], writes=[onesblk])
            k.dma(scanrst[:], self.cst["scanrst"], writes=[scanrst])
            xt = [k.sb("xt%d" % i, [128, DM], F32, es) for i in range(2)]
            junk = k.sb("junk", [128, DM], BF16, es)
            st = k.sb("st", [128, 8], F32, es)
            hb = k.sb("hb", [128, DM], BF16, es)
            hT = k.sb("hT", [128, 8, 128], BF16, es)
            pk = [k.sb("pk%d" % i, [128, ODD_W], BF16, es) for i in range(2)]
            mq_b = k.sb("mq_b", [128, 512], BF16, es)
            qm_b = k.sb("qm_b", [128, 256], BF16, es)
            cqnT = k.sb("cqnT", [128, 2, 128], BF16, es)
            ckvnT = k.sb("ckvnT", [128, 128], BF16, es)
            kpeT = k.sb("kpeT", [128, 128], BF16, es)
            q_b = k.sb("q_b", [128, 8, 96], BF16, es)
            KTt = [k.sb("KTt%d" % i, [128, 8, 128], BF16, es) for i in range(2)]
            VMt = [k.sb("VMt%d" % i, [128, 8, 65], BF16, es) for i in range(2)]
            tA = k.sb("rotA", [128, 512], F32, es)
            tB = k.sb("rotB", [128, 512], F32, es)
            d_fs = [k.sb("d_f%d" % i, [128, 14, 128], F32, es) for i in range(2)]
            diff = k.sb("diff", [128, 14, 128], F32, es)
            carry = k.sb("carry", [128, 14], F32, es)
            tw = k.sb("tw", [128, 128], F32, es)
            G = [k.sb("g%d" % i, [128, 4, 128], F32, es) for i in range(12)]
            pcs = [k.sb("pcs%d" % i, [128, 8], F32, es) for i in range(2)]
            for i in range(2):
                k.op(POOL, lambda: nc.gpsimd.memset(VMt[i][:], 1.0), [], [VMt[i]])
                k.op(POOL, lambda: nc.gpsimd.memset(pk[i][:], 0.0), [], [pk[i]])
                k.op(POOL, lambda: nc.gpsimd.memset(KTt[i][:], 0.0), [], [KTt[i]])
            for i in range(2):
                k.op(POOL, lambda: nc.gpsimd.memset(d_fs[i][:], 0.0), [], [d_fs[i]])
            k.op(POOL, lambda: nc.gpsimd.memset(mq_b[:], 0.0), [], [mq_b])

            def cb(c0, n=4):
                return cols[:, c0:c0 + n].unsqueeze(2).to_broadcast([128, n, 128])

            def v4(t):
                return t[:].rearrange("p n t -> p (n t)")

            def load(ti):
                b_, i_ = tiles[ti]
                k.dma(xt[ti % 2][:], src[b_, i_ * 128:(i_ + 1) * 128, :], reads=[self.xbuf[b_][i_]], writes=[xt[ti % 2]])

            def load_vf(ti):
                b_, i_ = tiles[ti]
                k.dma(vft[ti % 2][:].rearrange("p n t -> p (n t)"), self.s_vf[b_, i_], writes=[vft[ti % 2]])

            load(0)
            if vres:
                load_vf(0)
            def F_gen(ti, b, i):
                d_f = d_fs[ti % 2]
                X = xt[ti % 2]
                PK = pk[ti % 2]
                KT = KTt[ti % 2]
                VM = VMt[ti % 2]
                if ti + 1 < len(tiles):
                    load(ti + 1)
                tix = b * NT + i
                self._rms_to_bf16(X, junk, st, hb, DM)
                for c in range(8):
                    k.op(PE, lambda: nc.tensor.transpose(out=pb2[:, c * 128:(c + 1) * 128],
                                                         in_=hb[:, c * 128:(c + 1) * 128], identity=self.ident[:]),
                         [hb, self.ident], [P[2]])
                k.op(ACT, lambda: nc.scalar.copy(out=hT[:].rearrange("p c t -> p (c t)"), in_=pb2[:, 0:1024]),
                     [P[2]], [hT])
                yield
                for c in range(8):
                    k.op(PE, lambda: nc.tensor.matmul(P[0][:, 0:416], lhsT=hT[:, c, :], rhs=wb[:, c, 0:416],
                                                      start=(c == 0), stop=(c == 7)), [hT, wb], [P[0]])
                for (c0, c1, so) in ((0, 256, 0), (256, 384, 3)):
                    n_ = c1 - c0
                    k.op(ACT, lambda: nc.scalar.activation(out=junk[:, c0:c1], in_=P[0][:, c0:c1], func=AF.Square,
                                                           accum_out=st[:, so:so + 1]), [P[0]], [junk, st])
                    k.op(ACT, lambda: nc.scalar.activation(out=st[:, so + 1:so + 2], in_=st[:, so:so + 1], func=AF.Sqrt,
                                                           scale=1.0 / n_, bias=EPS), [st], [st])
                    k.op(DVE, lambda: nc.vector.reciprocal(out=st[:, so + 2:so + 3], in_=st[:, so + 1:so + 2]), [st], [st])
                    k.op(DVE, lambda: nc.vector.tensor_scalar(out=mq_b[:, c0:c1], in0=P[0][:, c0:c1],
                                                              scalar1=st[:, so + 2:so + 3], scalar2=None, op0=ALU.mult),
                         [P[0], st], [mq_b])
                xv = P[0][:, 384:416].rearrange("p (h t f) -> p h t f", h=1, t=2)
                self._rotary(xv, tix, 40, 16, tA, tB, mq_b[:, 384:400].unsqueeze(1), mq_b[:, 400:416].unsqueeze(1),
                             [P[0]], [mq_b], 1)
                yield
                for g in range(3):
                    bk = P[(g + 1) % 2]
                    c0 = c_qm + g * 512
                    for c in range(8):
                        k.op(PE, lambda: nc.tensor.matmul(bk[:, :], lhsT=hT[:, c, :], rhs=wb[:, c, c0:c0 + 512],
                                                          start=(c == 0), stop=(c == 7)), [hT, wb], [bk])
                    if g == 0:
                        k.op(DVE, lambda: nc.vector.tensor_copy(out=qm_b[:], in_=bk[:, 0:256]), [bk], [qm_b])
                        k.op(ACT, lambda: nc.scalar.activation(out=PK[:, O_SG:O_SG + 256], in_=bk[:, 256:512],
                                                               func=AF.Silu), [bk], [PK])
                    else:
                        o = O_SG + 256 + (g - 1) * 512
                        k.op(ACT, lambda: nc.scalar.activation(out=PK[:, o:o + 512], in_=bk[:, :], func=AF.Silu),
                             [bk], [PK])
                yield
                for c in range(3):
                    k.op(PE, lambda: nc.tensor.transpose(out=pb2[:, c * 128:(c + 1) * 128],
                                                         in_=mq_b[:, c * 128:(c + 1) * 128], identity=self.ident[:]),
                         [mq_b, self.ident], [P[2]])
                k.op(PE, lambda: nc.tensor.transpose(out=pb2[64:96, 384:512], in_=mq_b[:, 384:416], identity=self.ident[:]),
                     [mq_b, self.ident], [P[2]])
                for c in range(2):
                    k.op(PE, lambda: nc.tensor.transpose(out=pb2[:, (4 + c) * 128:(5 + c) * 128],
                                                         in_=qm_b[:, c * 128:(c + 1) * 128], identity=self.ident[:]),
                         [qm_b, self.ident], [P[2]])
                k.op(ACT, lambda: nc.scalar.copy(out=cqnT[:].rearrange("p c t -> p (c t)"), in_=pb2[:, 0:256]), [P[2]], [cqnT])
                k.op(DVE, lambda: nc.vector.tensor_copy(out=ckvnT[:], in_=pb2[:, 256:384]), [P[2]], [ckvnT])
                k.op(DVE, lambda: nc.vector.tensor_copy(out=kpeT[64:96, :], in_=pb2[64:96, 384:512]), [P[2]], [kpeT])
                k.op(ACT, lambda: nc.scalar.copy(out=PK[:, O_QMT:O_QMT + 256], in_=pb2[:, 512:768]), [P[2]], [PK])
                yield
                for n2 in range(2):
                    bk = P[n2]
                    for c in range(2):
                        k.op(PE, lambda: nc.tensor.matmul(bk[:, 0:384], lhsT=cqnT[:, c, :],
                                                          rhs=wqb[:, c, n2 * 384:(n2 + 1) * 384],
                                                          start=(c == 0), stop=(c == 1)), [cqnT, wqb], [bk])
                    qv = bk[:, 0:384].rearrange("p (h d) -> p h d", h=4)
                    k.op(ACT, lambda: nc.scalar.copy(out=q_b[:, n2 * 4:(n2 + 1) * 4, 0:64], in_=qv[:, :, 0:64]), [bk], [q_b])
                    xv = qv[:, :, 64:96].rearrange("p h (t f) -> p h t f", t=2)
                    self._rotary(xv, tix, 40, 16, tA, tB, q_b[:, n2 * 4:(n2 + 1) * 4, 64:80],
                                 q_b[:, n2 * 4:(n2 + 1) * 4, 80:96], [bk], [q_b], 4)
                for h in range(8):
                    k.op(PE, lambda: nc.tensor.transpose(out=pb2[0:96, h * 128:(h + 1) * 128], in_=q_b[:, h, :],
                                                         identity=self.ident[:]), [q_b, self.ident], [P[2]])
                k.op(ACT, lambda: nc.scalar.copy(out=PK[0:96, O_QT:O_QT + 1024], in_=pb2[0:96, 0:1024]), [P[2]], [PK])
                yield
                for h in range(8):
                    bk = P[h // 4]
                    k.op(PE, lambda: nc.tensor.matmul(bk[0:64, (h % 4) * 128:(h % 4 + 1) * 128],
                                                      lhsT=wkvb[:, 0, h * 128:h * 128 + 64], rhs=ckvnT[:, :],
                                                      start=True, stop=True), [wkvb, ckvnT], [bk])
                k.op(ACT, lambda: nc.scalar.copy(out=KT[0:64, 0:4, :].rearrange("p h t -> p (h t)"), in_=P[0][0:64, :]),
                     [P[0]], [KT])
                k.op(DVE, lambda: nc.vector.tensor_copy(out=KT[0:64, 4:8, :].rearrange("p h t -> p (h t)"), in_=P[1][0:64, :]),
                     [P[1]], [KT])
                k.op(POOL, lambda: nc.gpsimd.tensor_copy(out=KT[64:96, :, :],
                                                         in_=kpeT[64:96, :].unsqueeze(1).to_broadcast([32, 8, 128])),
                     [kpeT], [KT])
                k.dma(self.s_kTM[b, :, :, i * 128:(i + 1) * 128], KT[0:96, :, :], reads=[KT])
                k.op(PE, lambda: nc.tensor.matmul(P[0][:, :], lhsT=ckvnT[:, :],
                                                  rhs=wkvb[:, 0, :].rearrange("p (h x) -> p h x", h=8)[:, :, 64:128],
                                                  start=True, stop=True), [wkvb, ckvnT], [P[0]])
                k.op(ACT, lambda: nc.scalar.copy(out=VM[:, :, 0:64], in_=P[0][:, :].rearrange("p (h d) -> p h d", h=8)),
                     [P[0]], [VM])
                k.dma(self.s_vA[b, i], VM[:].rearrange("p h d -> p (h d)"), reads=[VM])
                yield
                for g0 in range(0, ntd, 4):
                    bk = P[(g0 // 4) % 2]
                    cnt = min(4, ntd - g0)
                    for sl in range(cnt):
                        nt = g0 + sl
                        rows = min(128, dc - nt * 128)
                        for c in range(8):
                            k.op(PE, lambda: nc.tensor.matmul(bk[0:rows, sl * 128:(sl + 1) * 128],
                                                              lhsT=wb[:, c, 416 + nt * 128:416 + nt * 128 + rows],
                                                              rhs=hT[:, c, :], start=(c == 0), stop=(c == 7)), [hT, wb], [bk])
                    full = cnt if (dc - (g0 + cnt - 1) * 128) >= 128 else cnt - 1
                    if full > 0:
                        k.op(ACT, lambda: nc.scalar.copy(out=d_f[:, g0:g0 + full, :].rearrange("p n t -> p (n t)"),
                                                         in_=bk[:, 0:full * 128]), [bk], [d_f])
                    if full < cnt:
                        rows = dc - (g0 + cnt - 1) * 128
                        k.op(ACT, lambda: nc.scalar.copy(out=d_f[0:rows, g0 + cnt - 1, :],
                                                         in_=bk[0:rows, (cnt - 1) * 128:cnt * 128]), [bk], [d_f])
            def R_gen(ti, b, i):
                PK = pk[ti % 2]
                d_f = d_fs[ti % 2]
                if vres and ti + 1 < len(tiles):
                    load_vf(ti + 1)
                k.op(POOL, lambda: nc.gpsimd.tensor_tensor(out=diff[:, :, 1:128], in0=d_f[:, :, 0:127], in1=d_f[:, :, 1:128],
                                                           op=ALU.subtract), [d_f], [diff])
                if i == 0:
                    k.op(POOL, lambda: nc.gpsimd.tensor_scalar(out=diff[:, :, 0], in0=d_f[:, :, 0], scalar1=-1.0, scalar2=1.0,
                                                               op0=ALU.mult, op1=ALU.mult), [d_f], [diff])
                else:
                    k.op(POOL, lambda: nc.gpsimd.tensor_tensor(out=diff[:, :, 0], in0=carry[:, :], in1=d_f[:, :, 0],
                                                               op=ALU.subtract), [carry, d_f], [diff])
                k.op(POOL, lambda: nc.gpsimd.tensor_copy(out=carry[:, :], in_=d_f[:, :, 127]), [d_f], [carry])
                k.op(DVE, lambda: nc.vector.tensor_tensor(out=diff[:], in0=diff[:], in1=cb(C_MU, 14), op=ALU.mult),
                     [diff, cols], [diff])
                sh = diff
                k.op(DVE, lambda: nc.vector.tensor_tensor(out=sh[:], in0=diff[:], in1=d_f[:], op=ALU.add), [diff, d_f], [sh])
                R_ = sh[:, 0:4, :]
                K_ = sh[:, 4:8, :]
                V_ = sh[:, 8:12, :]
                sgw, alr, kx, tmp, kmod, bb, lp, eP, eNP, ePp, eD, tmp2 = G
                yield
                k.op(ACT, lambda: nc.scalar.activation(out=tw[0:64, :], in_=sh[0:64, 12, :], func=AF.Tanh), [sh], [tw])
                for nt in range(4):
                    k.op(PE, lambda: nc.tensor.matmul(P[3][:, nt * 128:(nt + 1) * 128], lhsT=w2sb[0:64, nt * 128:(nt + 1) * 128],
                                                      rhs=tw[0:64, :], start=True, stop=True), [w2sb, tw], [P[3]])
                for nt in range(4):
                    k.op(PE, lambda: nc.tensor.matmul(P[4][:, nt * 128:(nt + 1) * 128], lhsT=a2sb[64:128, nt * 128:(nt + 1) * 128],
                                                      rhs=sh[64:128, 12, :], start=True, stop=True), [a2sb, sh], [P[4]])
                for nt in range(4):
                    k.op(ACT, lambda: nc.scalar.activation(out=sgw[:, nt, :], in_=P[3][:, nt * 128:(nt + 1) * 128],
                                                           func=AF.Sigmoid, bias=cols[:, C_W0 + nt:C_W0 + nt + 1]),
                         [P[3], cols], [sgw])
                for nt in range(4):
                    k.op(ACT, lambda: nc.scalar.activation(out=alr[:, nt, :], in_=P[4][:, nt * 128:(nt + 1) * 128],
                                                           func=AF.Sigmoid, bias=cols[:, C_A0 + nt:C_A0 + nt + 1]),
                         [P[4], cols], [alr])
                if vres:
                    VF = vft[ti % 2]
                    for nt in range(4):
                        k.op(PE, lambda: nc.tensor.matmul(P[5][:, nt * 128:(nt + 1) * 128], lhsT=v2sb[0:32, nt * 128:(nt + 1) * 128],
                                                          rhs=sh[0:32, 13, :], start=True, stop=True), [v2sb, sh], [P[5]])
                    for nt in range(4):
                        k.op(ACT, lambda: nc.scalar.activation(out=tmp[:, nt, :], in_=P[5][:, nt * 128:(nt + 1) * 128],
                                                               func=AF.Sigmoid, bias=cols[:, C_V0 + nt:C_V0 + nt + 1]),
                             [P[5], cols], [tmp])
                    k.op(POOL, lambda: nc.gpsimd.tensor_tensor(out=tmp2[:], in0=VF[:], in1=V_, op=ALU.subtract), [VF, sh], [tmp2])
                    k.op(POOL, lambda: nc.gpsimd.tensor_tensor(out=tmp2[:], in0=tmp2[:], in1=tmp[:], op=ALU.mult), [tmp2, tmp], [tmp2])
                    k.op(POOL, lambda: nc.gpsimd.tensor_tensor(out=V_, in0=V_, in1=tmp2[:], op=ALU.add), [sh, tmp2], [sh])
                else:
                    k.dma(self.s_vf[b, i].rearrange("p (n t) -> p n t", n=4), V_, reads=[sh])
                k.op(DVE, lambda: nc.vector.tensor_scalar(out=v4(sgw), in0=v4(sgw), scalar1=NEG_E, scalar2=None, op0=ALU.mult),
                     [sgw], [sgw])
                lw = sgw
                yield
                k.op(POOL, lambda: nc.gpsimd.tensor_tensor(out=kx[:], in0=K_, in1=cb(C_KK), op=ALU.mult), [sh, cols], [kx])
                k.op(POOL, lambda: nc.gpsimd.tensor_tensor(out=tmp[:], in0=kx[:], in1=kx[:], op=ALU.mult), [kx], [tmp])
                for nt in range(4):
                    k.op(PE, lambda: nc.tensor.matmul(P[6][:, nt * 128:(nt + 1) * 128], lhsT=onesblk[:, :], rhs=tmp[:, nt, :],
                                                      start=True, stop=True), [onesblk, tmp], [P[6]])
                k.op(ACT, lambda: nc.scalar.activation(out=v4(tmp2), in_=P[6][:, :], func=AF.Sqrt), [P[6]], [tmp2])
                k.op(DVE, lambda: nc.vector.tensor_scalar(out=v4(tmp2), in0=v4(tmp2), scalar1=1e-12, scalar2=None, op0=ALU.max),
                     [tmp2], [tmp2])
                k.op(DVE, lambda: nc.vector.reciprocal(out=v4(tmp2), in_=v4(tmp2)), [tmp2], [tmp2])
                yield
                kk = kx
                k.op(POOL, lambda: nc.gpsimd.tensor_tensor(out=kk[:], in0=kx[:], in1=tmp2[:], op=ALU.mult), [kx, tmp2], [kk])
                k.op(DVE, lambda: nc.vector.tensor_tensor(out=tmp[:], in0=alr[:], in1=cb(C_KA), op=ALU.mult), [alr, cols], [tmp])
                k.op(DVE, lambda: nc.vector.tensor_tensor(out=tmp[:], in0=tmp[:], in1=cb(C_KA), op=ALU.subtract), [tmp, cols], [tmp])
                k.op(DVE, lambda: nc.vector.scalar_tensor_tensor(out=v4(kmod), in0=v4(tmp), scalar=1.0,
                                                                 in1=K_.rearrange("p n t -> p (n t)"),
                                                                 op0=ALU.add, op1=ALU.mult), [tmp, sh], [kmod])
                k.op(POOL, lambda: nc.gpsimd.tensor_tensor(out=bb[:], in0=kk[:], in1=alr[:], op=ALU.mult), [kk, alr], [bb])
                yield
                k.op(POOL, lambda: nc.gpsimd.tensor_tensor(out=tmp2[:], in0=R_, in1=cb(C_RK), op=ALU.mult), [sh, cols], [tmp2])
                k.op(POOL, lambda: nc.gpsimd.tensor_tensor(out=tmp2[:], in0=tmp2[:], in1=kmod[:], op=ALU.mult), [tmp2, kmod], [tmp2])
                for nt in range(4):
                    k.op(PE, lambda: nc.tensor.matmul(P[7][:, nt * 128:(nt + 1) * 128], lhsT=onesblk[:, :], rhs=tmp2[:, nt, :],
                                                      start=True, stop=True), [onesblk, tmp2], [P[7]])
                k.op(DVE, lambda: nc.vector.tensor_tensor(out=PK[:, O_BON:O_BON + 512], in0=P[7][:, :],
                                                          in1=V_.rearrange("p n t -> p (n t)"), op=ALU.mult), [P[7], sh], [PK])
                yield
                for nt in range(4):
                    k.op(DVE, lambda: nc.vector.tensor_tensor_scan(out=lp[:, nt, :], data0=scanrst[:, :], data1=lw[:, nt, :],
                                                                   initial=0.0, op0=ALU.mult, op1=ALU.add),
                         [scanrst, lw], [lp])
                k.op(ACT, lambda: nc.scalar.activation(out=v4(eP), in_=v4(lp), func=AF.Exp), [lp], [eP])
                k.op(ACT, lambda: nc.scalar.activation(out=v4(eNP), in_=v4(lp), func=AF.Exp, scale=-1.0), [lp], [eNP])
                k.op(POOL, lambda: nc.gpsimd.tensor_tensor(out=ePp[:], in0=lp[:], in1=lw[:], op=ALU.subtract), [lp, lw], [ePp])
                k.op(ACT, lambda: nc.scalar.activation(out=v4(ePp), in_=v4(ePp), func=AF.Exp), [ePp], [ePp])
                yield
                for c in range(2):
                    k.op(POOL, lambda: nc.gpsimd.tensor_tensor(
                        out=eD[:, :, 64 * c:64 * c + 64], in0=lp[:, :, 64 * c + 63:64 * c + 64].to_broadcast([128, 4, 64]),
                        in1=lp[:, :, 64 * c:64 * c + 64], op=ALU.subtract), [lp], [eD])
                k.op(ACT, lambda: nc.scalar.activation(out=v4(eD), in_=v4(eD), func=AF.Exp), [eD], [eD])
                PCS = pcs[ti % 2]
                k.op(DVE, lambda: nc.vector.tensor_copy(out=PCS[:].rearrange("p (n c) -> p n c", n=4),
                                                        in_=eP[:].rearrange("p n (c t) -> p n c t", c=2)[:, :, :, 63]),
                     [eP], [PCS])
                k.dma(self.s_pc[b, i], PCS[:], reads=[PCS])
                yield
                AR = PK[:, O_AR:O_AR + 1024].rearrange("p (n w t) -> p n w t", n=4, w=2)
                BKv = PK[:, O_BK:O_BK + 1024].rearrange("p (n c g t) -> p n c g t", n=4, c=2, g=2)
                BPv = PK[:, O_BKP:O_BKP + 1024].rearrange("p (n c g t) -> p n c g t", n=4, c=2, g=2)
                BT = PK[:, O_BT:O_BT + 512].rearrange("p (n t) -> p n t", n=4)
                k.op(DVE, lambda: nc.vector.scalar_tensor_tensor(out=AR[:, :, 0, :], in0=kk[:], scalar=-1.0, in1=ePp[:],
                                                                 op0=ALU.mult, op1=ALU.mult), [kk, ePp], [PK])
                k.op(POOL, lambda: nc.gpsimd.tensor_tensor(out=AR[:, :, 1, :], in0=R_, in1=eP[:], op=ALU.mult), [sh, eP], [PK])
                k.op(POOL, lambda: nc.gpsimd.tensor_tensor(out=BT, in0=bb[:], in1=eNP[:], op=ALU.mult), [bb, eNP], [PK])
                yield
                n_ = 0
                for (dst, ee) in ((BKv, eNP), (BPv, eD)):
                    for c in range(2):
                        sl = slice(64 * c, 64 * c + 64)
                        for (srcv, g_) in ((bb, c), (kmod, 1 - c)):
                            eng = DVE if n_ % 2 == 0 else POOL
                            mod = nc.vector if eng == DVE else nc.gpsimd
                            k.op(eng, lambda: mod.tensor_tensor(out=dst[:, :, c, g_, :], in0=srcv[:, :, sl], in1=ee[:, :, sl],
                                                                op=ALU.mult), [srcv, ee], [PK])
                            n_ += 1
                k.op(ACT, lambda: nc.scalar.copy(out=PK[:, O_VT:O_VT + 512], in_=V_.rearrange("p n t -> p (n t)")), [sh], [PK])
                k.dma(self.s_pk[b, i], PK[:], reads=[PK])

            def drain(*gens):
                alive = [g for g in gens if g is not None]
                while alive:
                    for g in list(alive):
                        try:
                            next(g)
                        except StopIteration:
                            alive.remove(g)

            drain(F_gen(0, *tiles[0]))
            for ti in range(len(tiles)):
                nf = F_gen(ti + 1, *tiles[ti + 1]) if ti + 1 < len(tiles) else None
                drain(R_gen(ti, *tiles[ti]), nf)
            k.barrier()
        if self.stop == "p1":
            return
        with contextlib.ExitStack() as es:
            wout = k.sb("wout", [128, 10, DM], BF16, es)
            wmem = k.sb("wmem", [128, 8, 512], BF16, es)
            with contextlib.ExitStack() as es0:
                self.stage = [k.sb("stgA2", [128, 2048], F32, es0), k.sb("stgB2", [128, 2048], F32, es0)]
                self._load_w(es, "wout", self.w[pre + "w_out"], 1280, DM, wb=wout)
                self._load_w(es, "wmem", self.w[pre + "w_mem_kv"], DM, 512, wb=wmem)
                k.barrier()
            kmT, vm = self._mem_kv(es, wmem, "o")
            causal = k.sb("causal", [128, 128], BF16, es)
            maskRW = k.sb("maskRW", [128, 512], BF16, es)
            lnw = k.sb("lnw", [128, 512], F32, es)
            lnb = k.sb("lnb", [128, 512], F32, es)
            k.dma(causal[:], self.cst["causal"], writes=[causal])
            k.dma(maskRW[:], self.cst["maskRW"], writes=[maskRW])
            k.dma(lnw[:], self.w[pre + "lnx_w"][0:1, :].broadcast_to([128, 512]), writes=[lnw])
            k.dma(lnb[:], self.w[pre + "lnx_b"][0:1, :].broadcast_to([128, 512]), writes=[lnb])
            gfin = None
            if last and self.final_norm:
                gfin = k.sb("gfin", [128, DM], F32, es)
                k.dma(gfin[:], self.w["final_norm"][0:1, :].broadcast_to([128, DM]), writes=[gfin])
            kT = k.sb("kTM", [128, 8, SEQ], BF16, es)
            vM = k.sb("vMc", [128, NT, 8, 65], BF16, es)
            xt = [k.sb("xt2_%d" % i, [128, DM], F32, es) for i in range(2)]
            pk = [k.sb("pk2_%d" % i, [128, ODD_W], BF16, es) for i in range(2)]
            pT = [[k.sb("pT%d_%d" % (i, j), [128, 4, 128], BF16, es) for j in range(2)] for i in range(2)]
            SBANKS = [(P[3], P[4]), (P[0], P[1])]
            rec = k.sb("rec", [128, 16], F32, es)
            y = k.sb("y", [128, 1280], F32, es)
            yg = k.sb("yg", [128, 1280], BF16, es)
            ygT = k.sb("ygT", [128, 10, 128], BF16, es)
            xo = k.sb("xo", [128, DM], F32, es)
            junk = k.sb("junk2", [128, DM], BF16, es)
            st = k.sb("st2", [128, 48], F32, es)
            Am = k.sb("Am", [128, 8, 512], BF16, es)
            Xa = k.sb("Xa", [128, 8, 128], BF16, es)
            Xb = k.sb("Xb", [128, 8, 128], BF16, es)
            XT = k.sb("XT", [128, 8, 128], BF16, es)
            NM = [k.sb("NM%d" % i, [128, 8, 256], BF16, es) for i in range(2)]
            W_all = k.sb("W_all", [128, 8, 2, 64], BF16, es)
            BPtm = [k.sb("BPtm%d" % p, [128, 2, 128], BF16, es) for p in range(4)]
            S_f = k.sb("S_f", [128, 4, 64], F32, es)
            S_b = k.sb("S_b", [128, 4, 64], BF16, es)
            ytok = k.sb("ytok", [128, 8, 64], F32, es)
            ysq = k.sb("ysq", [128, 8, 64], F32, es)
            pcl = [k.sb("pcl%d" % i, [128, 8], F32, es) for i in range(2)]
            k.op(POOL, lambda: nc.gpsimd.memset(W_all[:], 0.0), [], [W_all])

            def prefetch(ti):
                b_, i_ = tiles[ti]
                k.dma(xt[ti % 2][:], src[b_, i_ * 128:(i_ + 1) * 128, :], reads=[self.xbuf[b_][i_]], writes=[xt[ti % 2]])
                k.dma(pk[ti % 2][:], self.s_pk[b_, i_], writes=[pk[ti % 2]])
                k.dma(pcl[ti % 2][:], self.s_pc[b_, i_], writes=[pcl[ti % 2]])

            prefetch(0)
            sc = 0
            SCL = 96.0 ** -0.5
            for ti, (b, i) in enumerate(tiles):
                X = xt[ti % 2]
                PK = pk[ti % 2]
                if i == 0:
                    k.dma(kT[0:96, :, :], self.s_kTM[b], writes=[kT])
                    k.dma(vM[:].rearrange("p i h d -> p i (h d)"), self.s_vA[b].rearrange("i p w -> p i w"), writes=[vM])
                if ti + 1 < len(tiles):
                    prefetch(ti + 1)
                qT = PK[:, O_QT:O_QT + 1024].rearrange("p (h t) -> p h t", h=8)
                qmT = PK[:, O_QMT:O_QMT + 256].rearrange("p (a t) -> p a t", a=2)
                sg = PK[:, O_SG:O_SG + 1280]
                AR = PK[:, O_AR:O_AR + 1024].rearrange("p (n w t) -> p n w t", n=4, w=2)
                BKv = PK[:, O_BK:O_BK + 1024].rearrange("p (n c x) -> p n c x", n=4, c=2)
                BPv = PK[:, O_BKP:O_BKP + 1024].rearrange("p (n c x) -> p n c x", n=4, c=2)
                BT = PK[:, O_BT:O_BT + 512].rearrange("p (n t) -> p n t", n=4)
                VT = PK[:, O_VT:O_VT + 512].rearrange("p (n t) -> p n t", n=4)
                BON = PK[:, O_BON:O_BON + 512]
                PCL = pcl[ti % 2]
                pcv = PCL[:]
                first = [True, True]

                def qk_m(j, sc_):
                    for half in range(2):
                        S_ = SBANKS[sc_ % 2][half]
                        for hh in range(4):
                            h = half * 4 + hh
                            k.op(PE, lambda: nc.tensor.matmul(S_[:, hh * 128:(hh + 1) * 128], lhsT=kT[0:96, h, j * 128:(j + 1) * 128],
                                                              rhs=qT[0:96, h, :], start=True, stop=True), [kT, PK], [S_])

                qk_m(0, sc)
                for j in range(i + 1):
                    if j + 1 <= i:
                        qk_m(j + 1, sc + 1)
                    for half in range(2):
                        S = SBANKS[sc % 2][half]
                        pt = pT[sc % 2][half]
                        k.op(ACT, lambda: nc.scalar.activation(out=pt[:].rearrange("p h q -> p (h q)"), in_=S[:, :],
                                                               func=AF.Exp, scale=SCL), [S], [pt])
                        if j == i:
                            k.op(DVE if half == 0 else POOL, lambda: (nc.vector if half == 0 else nc.gpsimd).tensor_tensor(
                                out=pt[:], in0=pt[:], in1=causal[:, :].unsqueeze(1).to_broadcast([128, 4, 128]), op=ALU.mult),
                                [pt, causal], [pt])
                        O = P[5 + half]
                        for hh in range(4):
                            h = half * 4 + hh
                            k.op(PE, lambda: nc.tensor.matmul(O[:, hh * 65:(hh + 1) * 65], lhsT=pt[:, hh, :], rhs=vM[:, j, h, :],
                                                              start=first[half], stop=(j == i), skip_group_check=True),
                                 [pt, vM], [O])
                            first[half] = False
                    sc += 1
                for half in range(2):
                    O = P[5 + half]
                    ov = O[:, 0:260].rearrange("p (h d) -> p h d", h=4)
                    k.op(DVE, lambda: nc.vector.reciprocal(out=rec[:, half * 4:half * 4 + 4], in_=ov[:, :, 64]), [O], [rec])
                    k.op(DVE, lambda: nc.vector.tensor_tensor(
                        out=y[:, half * 256:(half + 1) * 256].rearrange("p (h d) -> p h d", h=4), in0=ov[:, :, 0:64],
                        in1=rec[:, half * 4:half * 4 + 4].unsqueeze(2).to_broadcast([128, 4, 64]), op=ALU.mult), [O, rec], [y])
                k.op(POOL, lambda: nc.gpsimd.memset(W_all[0:64, :, 0, :], 0.0), [], [W_all])
                k.op(POOL, lambda: nc.gpsimd.memset(W_all[64:128, :, 1, :], 0.0), [], [W_all])
                for p_ in range(4):
                    k.op(PE, lambda: nc.tensor.transpose(out=pb2[:, 0:128], in_=AR[:, p_, 0, :], identity=self.ident[:]),
                         [PK, self.ident], [P[2]])
                    k.op(PE, lambda: nc.tensor.transpose(out=pb2[64:128, 128:256], in_=VT[:, p_, 0:64], identity=self.ident[:]),
                         [PK, self.ident], [P[2]])
                    k.op(PE, lambda: nc.tensor.transpose(out=pb2[0:64, 128:256], in_=VT[:, p_, 64:128], identity=self.ident[:]),
                         [PK, self.ident], [P[2]])
                    for c in range(2):
                        k.op(PE, lambda: nc.tensor.transpose(out=pb2[:, 256 + c * 128:384 + c * 128], in_=BPv[:, p_, c, :],
                                                             identity=self.ident[:]), [PK, self.ident], [P[2]])
                    for s_ in range(2):
                        k.op(ACT, lambda: nc.scalar.copy(out=Xa[:, 2 * p_ + s_, 64 * s_:64 * s_ + 64],
                                                         in_=pb2[:, 64 * s_:64 * s_ + 64]), [P[2]], [Xa])
                    k.op(DVE, lambda: nc.vector.tensor_copy(out=W_all[64:128, 2 * p_:2 * p_ + 2, 0, :],
                                                            in_=pb2[64:128, 128:256].rearrange("p (s d) -> p s d", s=2)),
                         [P[2]], [W_all])
                    k.op(DVE, lambda: nc.vector.tensor_copy(out=W_all[0:64, 2 * p_:2 * p_ + 2, 1, :],
                                                            in_=pb2[0:64, 128:256].rearrange("p (s d) -> p s d", s=2)),
                         [P[2]], [W_all])
                    k.op(ACT, lambda: nc.scalar.copy(out=BPtm[p_][:].rearrange("p c x -> p (c x)"), in_=pb2[:, 256:512]),
                         [P[2]], [BPtm[p_]])
                for h in range(8):
                    s_, p_ = h % 2, h // 2
                    rs = slice(64 * s_, 64 * s_ + 64)
                    A = P[3 + s_]
                    for c in range(2):
                        k.op(PE, lambda: nc.tensor.matmul(A[:, c * 128:(c + 1) * 128], lhsT=BKv[rs, p_, c, :],
                                                          rhs=AR[rs, p_, :, 64 * c:64 * c + 64], start=True, stop=True), [PK], [A])
                    k.op(PE, lambda: nc.tensor.matmul(A[:, 256:384], lhsT=AR[rs, p_, 0, :], rhs=BT[rs, p_, :],
                                                      start=True, stop=True), [PK], [A])
                    k.op(PE, lambda: nc.tensor.matmul(A[:, 384:512], lhsT=BT[rs, p_, :], rhs=AR[rs, p_, 0, :],
                                                      start=True, stop=True), [PK], [A])
                    k.op(DVE, lambda: nc.vector.tensor_tensor(out=Am[:, h, :], in0=A[:, :], in1=maskRW[:], op=ALU.mult),
                         [A, maskRW], [Am])
                GB = [(P[0], P[3], P[4]), (P[1], P[5], P[6])]
                for g in range(2):
                    XB = GB[g][0]
                    for q in range(4):
                        h = 4 * g + q
                        for c in range(2):
                            k.op(PE, lambda: nc.tensor.matmul(XB[64 * c:64 * c + 64, q * 64:(q + 1) * 64],
                                                              lhsT=Am[:, h, c * 128:c * 128 + 64], rhs=W_all[:, h, c, :],
                                                              start=True, stop=True), [Am, W_all], [XB])
                    for s_ in range(2):
                        uc = slice(64 * (1 - s_), 64 * (1 - s_) + 64)
                        k.op(ACT, lambda: nc.scalar.copy(
                            out=Xa[:, 4 * g + s_:4 * g + 4:2, uc],
                            in_=XB[:, 0:256].rearrange("p (q d) -> p q d", q=4)[:, s_:4:2, :]), [XB], [Xa])
                Xc, Xn = Xa, Xb
                cur = None
                for lvl in range(6):
                    for g in range(2):
                        XB = GB[g][0]
                        for q in range(4):
                            h = 4 * g + q
                            Mh = Am[:, h, 384:512] if cur is None else cur[:, h, 128:256]
                            k.op(PE, lambda: nc.tensor.matmul(XB[:, q * 128:(q + 1) * 128], lhsT=Mh, rhs=Xc[:, h, :],
                                                              start=True, stop=True), [Am if cur is None else cur, Xc], [XB])
                    for g in range(2):
                        XB = GB[g][0]
                        k.op(DVE, lambda: nc.vector.tensor_tensor(
                            out=Xn[:, 4 * g:4 * g + 4, :].rearrange("p q t -> p (q t)"), in0=XB[:, :],
                            in1=Xc[:, 4 * g:4 * g + 4, :].rearrange("p q t -> p (q t)"), op=ALU.add), [XB, Xc], [Xn])
                    Xc, Xn = Xn, Xc
                    if lvl < 5:
                        nxt = NM[lvl % 2]
                        for g in range(2):
                            Qn, Qm = GB[g][1], GB[g][2]
                            for q in range(4):
                                h = 4 * g + q
                                Nh = Am[:, h, 256:384] if cur is None else cur[:, h, 0:128]
                                Mh = Am[:, h, 384:512] if cur is None else cur[:, h, 128:256]
                                srcb = Am if cur is None else cur
                                if lvl < 4:
                                    k.op(PE, lambda: nc.tensor.matmul(Qn[:, q * 128:(q + 1) * 128], lhsT=Mh, rhs=Nh,
                                                                      start=True, stop=True), [srcb], [Qn])
                                k.op(PE, lambda: nc.tensor.matmul(Qm[:, q * 128:(q + 1) * 128], lhsT=Nh, rhs=Mh,
                                                                  start=True, stop=True), [srcb], [Qm])
                        for g in range(2):
                            Qn, Qm = GB[g][1], GB[g][2]
                            if lvl < 4:
                                k.op(ACT, lambda: nc.scalar.copy(out=nxt[:, 4 * g:4 * g + 4, 0:128],
                                                                 in_=Qn[:, :].rearrange("p (q t) -> p q t", q=4)), [Qn], [nxt])
                            k.op(DVE if g == 0 else ACT, lambda: (nc.vector.tensor_copy if g == 0 else nc.scalar.copy)(
                                out=nxt[:, 4 * g:4 * g + 4, 128:256], in_=Qm[:, :].rearrange("p (q t) -> p q t", q=4)),
                                [Qm], [nxt])
                        cur = nxt
                for h in range(8):
                    k.op(PE, lambda: nc.tensor.transpose(out=pb2[:, h * 128:(h + 1) * 128], in_=Xa[:, h, :],
                                                         identity=self.ident[:]), [Xa, self.ident], [P[2]])
                k.op(ACT, lambda: nc.scalar.copy(out=XT[:].rearrange("p h t -> p (h t)"), in_=pb2[:, 0:1024]), [P[2]], [XT])
                pcv3 = pcv.rearrange("p (n c) -> p n c", n=4)
                for c in range(2):
                    cs = slice(64 * c, 64 * c + 64)
                    fresh = (i == 0 and c == 0)
                    for s_ in range(2):
                        rs = slice(64 * s_, 64 * s_ + 64)
                        uc = slice(64 * (1 - s_), 64 * (1 - s_) + 64)
                        Ba = P[s_]
                        if fresh:
                            k.op(DVE, lambda: nc.vector.tensor_copy(out=W_all[cs, s_:8:2, c, :], in_=Xa[cs, s_:8:2, uc]),
                                 [Xa], [W_all])
                        else:
                            for p_ in range(4):
                                h = 2 * p_ + s_
                                k.op(PE, lambda: nc.tensor.matmul(Ba[cs, p_ * 64:(p_ + 1) * 64], lhsT=XT[rs, h, cs],
                                                                  rhs=S_b[rs, p_, :], start=True, stop=True), [XT, S_b], [Ba])
                            k.op(DVE, lambda: nc.vector.tensor_tensor(
                                out=W_all[cs, s_:8:2, c, :], in0=Ba[cs, 0:256].rearrange("p (q d) -> p q d", q=4),
                                in1=Xa[cs, s_:8:2, uc], op=ALU.add), [Ba, Xa], [W_all])
                    for s_ in range(2):
                        rs = slice(64 * s_, 64 * s_ + 64)
                        Ba, Bb = P[s_], P[3 + s_]
                        for p_ in range(4):
                            h = 2 * p_ + s_
                            yo = slice(256 + p_ * 64, 256 + (p_ + 1) * 64)
                            if not fresh:
                                k.op(PE, lambda: nc.tensor.matmul(Ba[cs, yo], lhsT=AR[rs, p_, 1, cs], rhs=S_b[rs, p_, :],
                                                                  start=True, stop=False), [PK, S_b], [Ba])
                            k.op(PE, lambda: nc.tensor.matmul(Ba[cs, yo], lhsT=Am[:, h, c * 128 + 64:(c + 1) * 128],
                                                              rhs=W_all[:, h, c, :], start=fresh, stop=True), [Am, W_all], [Ba])
                        for p_ in range(4):
                            h = 2 * p_ + s_
                            k.op(PE, lambda: nc.tensor.matmul(Bb[rs, p_ * 64:(p_ + 1) * 64], lhsT=BPtm[p_][:, c, rs],
                                                              rhs=W_all[:, h, c, :], start=True, stop=True),
                                 [BPtm[p_], W_all], [Bb])
                    for s_ in range(2):
                        rs = slice(64 * s_, 64 * s_ + 64)
                        Ba, Bb = P[s_], P[3 + s_]
                        sfv = S_f[rs, :, :]
                        if fresh:
                            k.op(DVE, lambda: nc.vector.tensor_copy(out=sfv, in_=Bb[rs, 0:256].rearrange("p (q d) -> p q d", q=4)),
                                 [Bb], [S_f])
                        else:
                            k.op(POOL, lambda: nc.gpsimd.tensor_tensor(
                                out=sfv, in0=sfv, in1=pcv3[rs, :, c].unsqueeze(2).to_broadcast([64, 4, 64]), op=ALU.mult),
                                [S_f, PCL], [S_f])
                            k.op(DVE, lambda: nc.vector.tensor_tensor(out=sfv, in0=Bb[rs, 0:256].rearrange("p (q d) -> p q d", q=4),
                                                                      in1=sfv, op=ALU.add), [Bb, S_f], [S_f])
                        k.op(POOL, lambda: nc.gpsimd.tensor_copy(out=S_b[rs, :, :], in_=sfv), [S_f], [S_b])
                        k.op(ACT, lambda: nc.scalar.copy(out=ytok[cs, s_:8:2, :],
                                                         in_=Ba[cs, 256:512].rearrange("p (q d) -> p q d", q=4)), [Ba], [ytok])
                k.op(DVE, lambda: nc.vector.tensor_reduce(out=st[:, 8:16], in_=ytok[:], axis=AX.X, op=ALU.add), [ytok], [st])
                k.op(POOL, lambda: nc.gpsimd.tensor_tensor(out=ysq[:], in0=ytok[:], in1=ytok[:], op=ALU.mult), [ytok], [ysq])
                k.op(DVE, lambda: nc.vector.tensor_reduce(out=st[:, 16:24], in_=ysq[:], axis=AX.X, op=ALU.add), [ysq], [st])
                k.op(DVE, lambda: nc.vector.tensor_scalar(out=st[:, 8:16], in0=st[:, 8:16], scalar1=1.0 / 64, scalar2=None,
                                                          op0=ALU.mult), [st], [st])
                k.op(DVE, lambda: nc.vector.tensor_tensor(out=st[:, 24:32], in0=st[:, 8:16], in1=st[:, 8:16], op=ALU.mult),
                     [st], [st])
                k.op(DVE, lambda: nc.vector.scalar_tensor_tensor(out=st[:, 16:24], in0=st[:, 16:24], scalar=1.0 / 64,
                                                                 in1=st[:, 24:32], op0=ALU.mult, op1=ALU.subtract),
                     [st], [st])
                k.op(ACT, lambda: nc.scalar.activation(out=st[:, 24:32], in_=st[:, 16:24], func=AF.Sqrt, bias=LN_EPS), [st], [st])
                k.op(DVE, lambda: nc.vector.reciprocal(out=st[:, 16:24], in_=st[:, 24:32]), [st], [st])
                k.op(DVE, lambda: nc.vector.tensor_tensor(out=ysq[:], in0=ytok[:],
                                                          in1=st[:, 8:16].unsqueeze(2).to_broadcast([128, 8, 64]),
                                                          op=ALU.subtract), [ytok, st], [ysq])
                k.op(DVE, lambda: nc.vector.tensor_tensor(out=ysq[:], in0=ysq[:],
                                                          in1=st[:, 16:24].unsqueeze(2).to_broadcast([128, 8, 64]),
                                                          op=ALU.mult), [ysq, st], [ysq])
                ysf = ysq[:].rearrange("p h d -> p (h d)")
                k.op(POOL, lambda: nc.gpsimd.tensor_tensor(out=ysf, in0=ysf, in1=lnw[:], op=ALU.mult), [ysq, lnw], [ysq])
                k.op(POOL, lambda: nc.gpsimd.tensor_tensor(out=ysf, in0=ysf, in1=lnb[:], op=ALU.add), [ysq, lnb], [ysq])
                for c in range(4):
                    k.op(PE, lambda: nc.tensor.transpose(out=pb2[:, c * 128:(c + 1) * 128], in_=BON[:, c * 128:(c + 1) * 128],
                                                         identity=self.ident[:]), [PK, self.ident], [P[2]])
                k.op(DVE, lambda: nc.vector.tensor_tensor(out=y[:, 512:1024], in0=pb2[:, 0:512], in1=ysf, op=ALU.add),
                     [P[2], ysq], [y])
                self._mem_attn(b, qmT, PK, kmT, vm, pT, rec, y, sc, SBANKS)
                sc += 2
                self._tail(PK, sg, y, yg, ygT, wout, X, xo, junk, st, b, i, last, gfin)
            k.barrier()

    def _mem_attn(self, b, qmT, PK, kmT, vm, pT, rec, y, sc, SBANKS):
        k, nc = self.k, self.nc
        P = self.P
        O = P[7]
        first = True
        for mb in range(2):
            Sb = SBANKS[sc % 2]
            ptp = pT[sc % 2]
            sc += 1
            for h in range(4):
                s_, p_ = h % 2, h // 2
                k.op(PE, lambda: nc.tensor.matmul(Sb[s_][:, p_ * 128:(p_ + 1) * 128],
                                                  lhsT=kmT[64 * s_:64 * s_ + 64, b, p_, mb * 128:(mb + 1) * 128],
                                                  rhs=qmT[64 * s_:64 * s_ + 64, p_, :], start=True, stop=True),
                     [kmT, PK], [Sb[s_]])
            for s_ in range(2):
                pt = ptp[s_]
                k.op(ACT, lambda: nc.scalar.activation(out=pt[:, 0:2, :].rearrange("p h q -> p (h q)"), in_=Sb[s_][:, 0:256],
                                                       func=AF.Exp, scale=0.125), [Sb[s_]], [pt])
                for p_ in range(2):
                    h = 2 * p_ + s_
                    k.op(PE, lambda: nc.tensor.matmul(O[:, h * 65:(h + 1) * 65], lhsT=pt[:, p_, :], rhs=vm[:, b, mb, h, :],
                                                      start=first, stop=(mb == 1), skip_group_check=True), [pt, vm], [O])
                    first = False
        ov = O[:, 0:260].rearrange("p (h d) -> p h d", h=4)
        k.op(DVE, lambda: nc.vector.reciprocal(out=rec[:, 8:12], in_=ov[:, :, 64]), [O], [rec])
        k.op(DVE, lambda: nc.vector.tensor_tensor(
            out=y[:, 1024:1280].rearrange("p (h d) -> p h d", h=4), in0=ov[:, :, 0:64],
            in1=rec[:, 8:12].unsqueeze(2).to_broadcast([128, 4, 64]), op=ALU.mult), [O, rec], [y])

    def _tail(self, PK, sg, y, yg, ygT, wout, X, xo, junk, st, b, i, last, gfin):
        k, nc = self.k, self.nc
        P = self.P
        pb2 = P[2][:].bitcast(BF16)
        k.op(POOL, lambda: nc.gpsimd.tensor_tensor(out=yg[:], in0=y[:], in1=sg, op=ALU.mult), [y, PK], [yg])
        for c0, c1 in ((0, 8), (8, 10)):
            for c in range(c0, c1):
                k.op(PE, lambda: nc.tensor.transpose(out=pb2[:, (c - c0) * 128:(c - c0 + 1) * 128],
                                                     in_=yg[:, c * 128:(c + 1) * 128], identity=self.ident[:]),
                     [yg, self.ident], [P[2]])
            k.op(ACT, lambda: nc.scalar.copy(out=ygT[:, c0:c1, :].rearrange("p c t -> p (c t)"),
                                             in_=pb2[:, 0:(c1 - c0) * 128]), [P[2]], [ygT])
        for n in range(2):
            for c in range(10):
                k.op(PE, lambda: nc.tensor.matmul(P[n][:, :], lhsT=ygT[:, c, :], rhs=wout[:, c, n * 512:(n + 1) * 512],
                                                  start=(c == 0), stop=(c == 9)), [ygT, wout], [P[n]])
            k.op(DVE, lambda: nc.vector.tensor_tensor(out=xo[:, n * 512:(n + 1) * 512], in0=P[n][:, :],
                                                      in1=X[:, n * 512:(n + 1) * 512], op=ALU.add), [P[n], X], [xo])
        if gfin is not None:
            k.op(ACT, lambda: nc.scalar.activation(out=junk[:], in_=xo[:], func=AF.Square, accum_out=st[:, 0:1]),
                 [xo], [junk, st])
            k.op(ACT, lambda: nc.scalar.activation(out=st[:, 1:2], in_=st[:, 0:1], func=AF.Sqrt, scale=1.0 / DM, bias=EPS),
                 [st], [st])
            k.op(DVE, lambda: nc.vector.reciprocal(out=st[:, 2:3], in_=st[:, 1:2]), [st], [st])
            k.op(DVE, lambda: nc.vector.scalar_tensor_tensor(out=xo[:], in0=xo[:], scalar=st[:, 2:3], in1=gfin[:],
                                                             op0=ALU.mult, op1=ALU.mult), [xo, st, gfin], [xo])
        k.dma(self.out[b, i * 128:(i + 1) * 128, :], xo[:], reads=[xo], writes=[self.xbuf[b][i]])


    def _tail_gen(self, PK, sg, y, yg, ygT, wout, X, xo, junk, st, b, i, last, gfin, ob=7):
        k, nc = self.k, self.nc
        P = self.P
        pb2 = P[2][:].bitcast(BF16)
        k.op(POOL, lambda: nc.gpsimd.tensor_tensor(out=yg[:], in0=y[:], in1=sg, op=ALU.mult), [y, PK], [yg])
        yield
        for c0, c1 in ((0, 8), (8, 10)):
            for c in range(c0, c1):
                k.op(PE, lambda: nc.tensor.transpose(out=pb2[:, (c - c0) * 128:(c - c0 + 1) * 128],
                                                     in_=yg[:, c * 128:(c + 1) * 128], identity=self.ident[:]),
                     [yg, self.ident], [P[2]])
            k.op(ACT, lambda: nc.scalar.copy(out=ygT[:, c0:c1, :].rearrange("p c t -> p (c t)"),
                                             in_=pb2[:, 0:(c1 - c0) * 128]), [P[2]], [ygT])
            yield
        for n in range(2):
            for c in range(10):
                k.op(PE, lambda: nc.tensor.matmul(P[ob][:, :], lhsT=ygT[:, c, :], rhs=wout[:, c, n * 512:(n + 1) * 512],
                                                  start=(c == 0), stop=(c == 9)), [ygT, wout], [P[ob]])
                if c == 4:
                    yield
            k.op(DVE, lambda: nc.vector.tensor_tensor(out=xo[:, n * 512:(n + 1) * 512], in0=P[ob][:, :],
                                                      in1=X[:, n * 512:(n + 1) * 512], op=ALU.add), [P[ob], X], [xo])
            yield
        if gfin is not None:
            k.op(ACT, lambda: nc.scalar.activation(out=junk[:], in_=xo[:], func=AF.Square, accum_out=st[:, 0:1]),
                 [xo], [junk, st])
            k.op(ACT, lambda: nc.scalar.activation(out=st[:, 1:2], in_=st[:, 0:1], func=AF.Sqrt, scale=1.0 / DM, bias=EPS),
                 [st], [st])
            k.op(DVE, lambda: nc.vector.reciprocal(out=st[:, 2:3], in_=st[:, 1:2]), [st], [st])
            k.op(DVE, lambda: nc.vector.scalar_tensor_tensor(out=xo[:], in0=xo[:], scalar=st[:, 2:3], in1=gfin[:],
                                                             op0=ALU.mult, op1=ALU.mult), [xo, st, gfin], [xo])
        k.dma(self.out[b, i * 128:(i + 1) * 128, :], xo[:], reads=[xo], writes=[self.xbuf[b][i]])


def _col(v, n):
    return np.ascontiguousarray(np.asarray(v, np.float32).reshape(n, 128).T)


_NET_CACHE = {}


def _get_net(n_layers=4, final_norm=True):
    key = (n_layers, final_norm)
    if key not in _NET_CACHE:
        _NET_CACHE[key] = Net(n_layers, final_norm)
    return _NET_CACHE[key]


def _in_maps(inputs):
    cst = _consts()
    shared = {"c_" + n: v for n, v in cst.items()}
    shared["mem_norm_col"] = _col(inputs["mem_norm"], 8)
    shared["final_norm"] = np.asarray(inputs["final_norm"], np.float32).reshape(1, DM)
    for l in range(4):
        p = "l%d_" % l
        shared[p + "norm_col"] = _col(inputs[p + "norm"], 8)
        shared[p + "w_mem_kv"] = np.asarray(inputs[p + "w_mem_kv"], np.float32)
        shared[p + "w_out"] = np.asarray(inputs[p + "w_out"], np.float32)
        shared[p + "w_in"] = np.asarray(inputs[p + "w_in"], np.float32)
        if l % 2 == 1:
            dc = 1664 if l == 1 else 1696
            shared[p + "qn_col"] = _col(inputs[p + "q_norm"], 2)
            shared[p + "kvn_col"] = _col(inputs[p + "kv_norm"], 1)
            shared[p + "w_qb"] = np.asarray(inputs[p + "w_qb"], np.float32)
            shared[p + "w_kvb"] = np.asarray(inputs[p + "w_kvb"], np.float32)
            mu = np.zeros(14 * 128, np.float32)
            mu[:dc] = np.asarray(inputs[p + "mu_shift"], np.float32)
            v0 = np.asarray(inputs[p + "v0"], np.float32) if l == 3 else np.zeros(512, np.float32)
            cols = [_col(mu, 14)] + [_col(np.asarray(inputs[p + n], np.float32).reshape(-1), 4) if n else _col(v0, 4)
                                     for n in ("w0", "a0", None, "k_k", "k_a", "r_k")]
            shared[p + "cols"] = np.ascontiguousarray(np.concatenate(cols, 1))
            shared[p + "w2"] = np.asarray(inputs[p + "w2"], np.float32)
            shared[p + "a2"] = np.asarray(inputs[p + "a2"], np.float32)
            if l == 3:
                shared[p + "v2"] = np.asarray(inputs[p + "v2"], np.float32)
            shared[p + "lnx_w"] = np.asarray(inputs[p + "lnx_w"], np.float32).reshape(1, 512)
            shared[p + "lnx_b"] = np.asarray(inputs[p + "lnx_b"], np.float32).reshape(1, 512)
    maps = []
    x = np.asarray(inputs["x"], np.float32)
    mem = np.asarray(inputs["mem"], np.float32)
    pos = np.asarray(inputs["positions"], np.int32)
    for c in range(NCORES):
        m = dict(shared)
        m["x"] = np.ascontiguousarray(x[2 * c:2 * c + 2])
        m["mem"] = np.ascontiguousarray(mem[2 * c:2 * c + 2])
        m["pos_col"] = np.ascontiguousarray(pos[2 * c:2 * c + 2].reshape(2, NT, 128).transpose(2, 0, 1).reshape(128, 2 * NT))
        maps.append(m)
    return maps


ALL_INPUTS = (
    "x", "mem", "positions", "mem_norm", "final_norm",
    "l0_norm", "l0_w_in", "l0_w_mem_kv", "l0_w_out",
    "l1_norm", "l1_w_in", "l1_q_norm", "l1_w_qb", "l1_kv_norm", "l1_w_kvb", "l1_mu_shift", "l1_w0", "l1_w2", "l1_a0",
    "l1_a2", "l1_k_k", "l1_k_a", "l1_r_k", "l1_lnx_w", "l1_lnx_b", "l1_w_mem_kv", "l1_w_out",
    "l2_norm", "l2_w_in", "l2_w_mem_kv", "l2_w_out",
    "l3_norm", "l3_w_in", "l3_q_norm", "l3_w_qb", "l3_kv_norm", "l3_w_kvb", "l3_mu_shift", "l3_w0", "l3_w2", "l3_a0",
    "l3_a2", "l3_v0", "l3_v2", "l3_k_k", "l3_k_a", "l3_r_k", "l3_lnx_w", "l3_lnx_b", "l3_w_mem_kv", "l3_w_out",
)


def kernel(**inputs):
    assert all(n in inputs for n in ALL_INPUTS)
    net = _get_net()
    maps = _in_maps(inputs)
    res = run_bass_kernel_spmd(net.nc, maps, core_ids=list(range(NCORES)))
    return np.concatenate([np.asarray(r["out"], np.float32) for r in res.results], axis=0)
```

```python
import contextlib
import math
import numpy as np
import ml_dtypes
import concourse.bass as bass
import concourse.mybir as mybir
from concourse.bass_utils import run_bass_kernel_spmd

F32 = mybir.dt.float32
BF16 = mybir.dt.bfloat16
I32 = mybir.dt.int32
ALU = mybir.AluOpType
AF = mybir.ActivationFunctionType
AX = mybir.AxisListType
NPBF = ml_dtypes.bfloat16

PE, ACT, DVE, POOL, SP = 0, 1, 2, 3, 4
SEM_ROT = 30000
NCORES = 8
SEQ = 2048
NT = 16
DM = 1024
EPS = 1e-6


class Buf:
    __slots__ = ("w", "r")

    def __init__(self):
        self.w = None
        self.r = []


class T:
    def __init__(self, t, b=None, excl=False):
        self.t = t
        self.b = b if b is not None else Buf()
        self.excl = excl

    def __getitem__(self, idx):
        return self.t[idx]


def _b(x):
    return x.b if isinstance(x, T) else x


class KB:
    def __init__(self, n_dma_slots=14):
        self.nc = bass.Bass("TRN2", target_bir_lowering=False)
        nc = self.nc
        self.es = contextlib.ExitStack()
        self.eng = [nc.tensor, nc.scalar, nc.vector, nc.gpsimd, nc.sync]
        self.esem = []
        self.ecnt = [0] * 5
        self.eepoch = [0] * 5
        for e in range(5):
            self.esem.append([self.es.enter_context(nc.semaphore("e%d_0" % e))])
        self.waited = [dict() for _ in range(5)]
        self.dsem = [self.es.enter_context(nc.semaphore("d%d" % i)) for i in range(n_dma_slots)]
        self.dcnt = [0] * n_dma_slots
        self.dnext = 0
        self.ninst = 0
        self.nwait = 0

    def sb(self, name, shape, dt, es=None):
        self.nsb = getattr(self, "nsb", 0) + 1
        return T((es or self.es).enter_context(self.nc.sbuf_tensor("%s_%d" % (name, self.nsb), list(shape), dt)))

    def ps(self, name, shape, dt=F32):
        return T(self.es.enter_context(self.nc.psum_tensor(name, list(shape), dt)), excl=True)

    def dram(self, name, shape, dt, kind="Internal"):
        return self.nc.dram_tensor(name, list(shape), dt, kind=kind).ap()

    def _wait(self, e, dep):
        kind, idx, val = dep
        if kind == "e" and idx[0] == e and e == PE:
            return
        key = (kind, idx)
        if self.waited[e].get(key, 0) >= val:
            return
        self.waited[e][key] = val
        sem = self.esem[idx[0]][idx[1]] if kind == "e" else self.dsem[idx]
        self.eng[e].wait_ge(sem, val)
        self.nwait += 1

    def _deps(self, e, reads, writes):
        for b in reads:
            if b.w is not None:
                self._wait(e, b.w)
        for b in writes:
            if b.w is not None:
                self._wait(e, b.w)
            for d in b.r:
                self._wait(e, d)

    def _mark(self, tag, reads, writes):
        for b in writes:
            b.w = tag
            b.r = []
        for b in reads:
            if b.w is tag:
                continue
            b.r = [d for d in b.r if not (d[0] == tag[0] and d[1] == tag[1])]
            b.r.append(tag)

    def op(self, e, fn, reads=(), writes=()):
        writes = [_b(x) for x in writes] + [x.b for x in reads if isinstance(x, T) and x.excl]
        reads = [_b(x) for x in reads]
        self._deps(e, reads, writes)
        inst = fn()
        if self.ecnt[e] >= SEM_ROT:
            self.eepoch[e] += 1
            self.ecnt[e] = 0
            self.esem[e].append(self.es.enter_context(
                self.nc.semaphore("e%d_%d" % (e, self.eepoch[e]))))
        self.ecnt[e] += 1
        inst.then_inc(self.esem[e][self.eepoch[e]], 1)
        tag = ("e", (e, self.eepoch[e]), self.ecnt[e])
        self._mark(tag, reads, writes)
        self.ninst += 1
        return inst

    def dma(self, out, in_, reads=(), writes=(), q=SP, **kw):
        reads = [_b(x) for x in reads]
        writes = [_b(x) for x in writes]
        e = q
        self._deps(e, reads, writes)
        s = self.dnext
        self.dnext = (self.dnext + 1) % len(self.dsem)
        if self.dcnt[s] > 0:
            self._wait(e, ("d", s, self.dcnt[s]))
        inst = self.eng[e].dma_start(out=out, in_=in_, **kw)
        self.dcnt[s] += 16
        inst.then_inc(self.dsem[s], 16)
        tag = ("d", s, self.dcnt[s])
        self._mark(tag, reads, writes)
        self.ninst += 1
        return inst

    def barrier(self):
        for e in range(5):
            for o in range(5):
                if o != e and (self.ecnt[o] > 0 or self.eepoch[o] > 0):
                    if self.ecnt[o] > 0:
                        self._wait(e, ("e", (o, self.eepoch[o]), self.ecnt[o]))
                    else:
                        self._wait(e, ("e", (o, self.eepoch[o] - 1), SEM_ROT))
            for s in range(len(self.dsem)):
                if self.dcnt[s] > 0:
                    self._wait(e, ("d", s, self.dcnt[s]))

    def finish(self):
        self.barrier()
        self.es.close()


def _consts():
    c = {}
    c["ident"] = np.eye(128, dtype=np.float32).astype(NPBF)
    kk = np.arange(128)[:, None, None]
    dl = np.arange(16)[None, :, None]
    qq = np.arange(128)[None, None, :]
    d = dl * 128 + qq - kk
    m = ((d >= 0) & (d <= 128)).astype(np.float32)
    m += ((d >= 0) & (d % 4 == 0) & (d <= 512)).astype(np.float32)
    m += ((d >= 0) & (d % 16 == 0) & (d <= 2047 * 16)).astype(np.float32)
    c["maskA"] = m.astype(NPBF)
    c["causal"] = (np.arange(128)[None, :] >= np.arange(128)[:, None]).astype(np.float32).astype(NPBF)
    g = 1.0 - np.exp2(-5.0 - np.arange(4, dtype=np.float64))
    lg = np.log(g)
    kq = (np.arange(128)[None, :] - np.arange(128)[:, None]).astype(np.float64)
    DT = np.where(kq[:, None, :] >= 0, np.exp(np.maximum(kq, 0)[:, None, :] * lg[None, :, None]), 0.0) / 8.0
    c["retDT"] = DT.astype(np.float32)
    FS = np.zeros((128, 2, 128), np.float64)
    G128 = np.zeros((128, 2), np.float64)
    for p in range(2):
        for s in range(2):
            h = 2 * p + s
            FS[64 * s:64 * s + 64, p, :] = np.exp((np.arange(128) + 1.0) * lg[h])[None, :] / 8.0
            G128[64 * s:64 * s + 64, p] = np.exp(128.0 * lg[h])
    c["retFS"] = FS.astype(np.float32)
    c["retG"] = G128.astype(np.float32)
    c["retTE"] = np.exp((127.0 - np.arange(128))[:, None] * lg[None, :]).astype(np.float32)
    def inv(dim, theta):
        return np.exp(-math.log(theta) * np.arange(0, dim, 2, dtype=np.float32) / dim).astype(np.float32)
    iv = np.concatenate([inv(16, 500000.0), inv(64, 10000.0), inv(32, 500000.0)])
    c["ropeinv"] = np.tile(iv[None, :], (128, 1)).astype(np.float32)
    j = np.arange(64)
    t = np.arange(64)
    strict = (t[None, :] > j[:, None]).astype(np.float32)
    incl = (t[None, :] >= j[:, None]).astype(np.float32)
    mS = np.zeros((128, 128), np.float32)
    for seg in range(2):
        mS[seg * 64:(seg + 1) * 64, 0:64] = strict
        mS[seg * 64:(seg + 1) * 64, 64:128] = incl
    mN = np.zeros((128, 128), np.float32)
    mM = np.zeros((128, 128), np.float32)
    for cc in range(2):
        mN[cc * 64:(cc + 1) * 64, cc * 64:(cc + 1) * 64] = strict.T
        mM[cc * 64:(cc + 1) * 64, cc * 64:(cc + 1) * 64] = strict
    c["maskRW"] = np.concatenate([mS, mS, mN, mM], 1).astype(NPBF)
    ob = np.zeros((128, 128), np.float32)
    ob[:64, :64] = 1.0
    ob[64:, 64:] = 1.0
    c["onesblk"] = ob.astype(NPBF)
    rst = np.ones((128, 128), np.float32)
    rst[:, 0] = 0.0
    rst[:, 64] = 0.0
    c["scanrst"] = rst
    return c


CONST_SHAPES = {
    "ident": ([128, 128], BF16), "maskA": ([128, 16, 128], BF16), "causal": ([128, 128], BF16),
    "retDT": ([128, 4, 128], F32), "retFS": ([128, 2, 128], F32), "retG": ([128, 2], F32),
    "retTE": ([128, 4], F32), "ropeinv": ([128, 56], F32),
    "maskRW": ([128, 512], BF16), "onesblk": ([128, 128], BF16), "scanrst": ([128, 128], F32),
}

ODD_W = 7184
O_QT, O_QMT, O_SG, O_AR, O_BK, O_BKP, O_VT, O_BON, O_PC, O_BT = 0, 1024, 1280, 2560, 3584, 4608, 5632, 6144, 6656, 6672
NCOLS = 38
C_MU, C_W0, C_A0, C_V0, C_KK, C_KA, C_RK = 0, 14, 18, 22, 26, 30, 34
LN_EPS = 64e-5

EVEN_W = 3328
E_QAT, E_QRT, E_KRT, E_KD, E_VR, E_QMT, E_SG = 0, 512, 768, 1024, 1280, 1792, 2048


class Net:
    def __init__(self, n_layers=4, final_norm=True, stop=None):
        self.stop = stop
        self.n_layers = n_layers
        self.final_norm = final_norm
        self.k = KB()
        k = self.k
        nc = k.nc
        self.nc = nc
        D = lambda name, shape, dt=F32: k.dram(name, shape, dt, "ExternalInput")
        self.x_in = D("x", [2, SEQ, DM])
        self.mem_in = D("mem", [2, 256, DM])
        self.pos_in = D("pos_col", [128, 32], I32)
        self.out = k.dram("out", [2, SEQ, DM], F32, "ExternalOutput")
        self.cst = {n: D("c_" + n, s, dt) for n, (s, dt) in CONST_SHAPES.items()}
        self.w = {}
        self.w["mem_norm_col"] = D("mem_norm_col", [128, 8])
        self.w["final_norm"] = D("final_norm", [1, DM])
        for l in range(4):
            p = "l%d_" % l
            self.w[p + "norm_col"] = D(p + "norm_col", [128, 8])
            self.w[p + "w_mem_kv"] = D(p + "w_mem_kv", [DM, 512])
            self.w[p + "w_out"] = D(p + "w_out", [1280, DM])
            if l % 2 == 0:
                self.w[p + "w_in"] = D(p + "w_in", [DM, 4096])
            else:
                dc = 1664 if l == 1 else 1696
                self.w[p + "w_in"] = D(p + "w_in", [DM, 416 + dc + 256 + 1280])
                self.w[p + "qn_col"] = D(p + "qn_col", [128, 2])
                self.w[p + "kvn_col"] = D(p + "kvn_col", [128, 1])
                self.w[p + "w_qb"] = D(p + "w_qb", [256, 768])
                self.w[p + "w_kvb"] = D(p + "w_kvb", [128, 1024])
                self.w[p + "cols"] = D(p + "cols", [128, NCOLS])
                self.w[p + "w2"] = D(p + "w2", [64, 512])
                self.w[p + "a2"] = D(p + "a2", [64, 512])
                if l == 3:
                    self.w[p + "v2"] = D(p + "v2", [32, 512])
                self.w[p + "lnx_w"] = D(p + "lnx_w", [1, 512])
                self.w[p + "lnx_b"] = D(p + "lnx_b", [1, 512])
        self.s_kTM = k.dram("s_kTM", [2, 96, 8, SEQ], BF16)
        self.s_vf = k.dram("s_vf", [2, NT, 128, 512], F32)
        self.s_pc = k.dram("s_pc", [2, NT, 128, 8], F32)
        self.s_pk = k.dram("s_pk", [2, NT, 128, ODD_W], BF16)
        self.s_kT = k.dram("s_kT", [2, 128, 4, SEQ], BF16)
        self.s_vA = k.dram("s_vA", [2, NT, 128, 8 * 65], BF16)
        self.s_memT = k.dram("s_memT", [2, 128, 8, 256], BF16)
        self.xbuf = [[Buf() for _ in range(NT)] for _ in range(2)]
        self.P = [k.ps("bank%d" % i, [128, 512], F32) for i in range(8)]
        self.ident = k.sb("ident", [128, 128], BF16)
        self.rot = k.sb("rot", [128, 2 * NT, 2, 56], F32)
        k.dma(self.ident[:], self.cst["ident"][:, :], writes=[self.ident])
        self._rope_tables()
        if stop == "rope":
            k.finish(); return
        self._mem_prep()
        if stop == "mem":
            k.finish(); return
        for l in range(n_layers):
            if l % 2 == 0:
                self._even_layer(l)
            else:
                self._odd_layer(l)
        k.finish()

    def _rope_tables(self):
        k, nc = self.k, self.nc
        with contextlib.ExitStack() as es:
            pi = k.sb("pos_i", [128, 32], I32, es)
            pf = k.sb("pos_f", [128, 32], F32, es)
            inv = k.sb("ropeinv", [128, 56], F32, es)
            ang = k.sb("ang", [128, 32, 56], F32, es)
            nf = k.sb("nf", [128, 32, 56], F32, es)
            ni = k.sb("ni", [128, 32, 56], I32, es)
            msk = k.sb("msk", [128, 32, 56], F32, es)
            k.dma(pi[:], self.pos_in[:, :], writes=[pi])
            k.dma(inv[:], self.cst["ropeinv"][:, :], writes=[inv])
            k.op(DVE, lambda: nc.vector.tensor_copy(out=pf[:], in_=pi[:]), [pi], [pf])
            k.op(DVE, lambda: nc.vector.tensor_tensor(
                out=ang[:], in0=pf[:].unsqueeze(2).to_broadcast([128, 32, 56]),
                in1=inv[:].unsqueeze(1).to_broadcast([128, 32, 56]), op=ALU.mult), [pf, inv], [ang])
            TWO_PI = 2.0 * math.pi
            C1 = 6.28125
            C2 = TWO_PI - C1

            def reduce_and_sin(shift, dst):
                k.op(DVE, lambda: nc.vector.tensor_scalar(out=nf[:], in0=ang[:], scalar1=1.0 / TWO_PI,
                                                          scalar2=shift / TWO_PI, op0=ALU.mult, op1=ALU.add),
                     [ang], [nf])
                k.op(DVE, lambda: nc.vector.tensor_copy(out=ni[:], in_=nf[:]), [nf], [ni])
                k.op(DVE, lambda: nc.vector.tensor_copy(out=nf[:], in_=ni[:]), [ni], [nf])
                k.op(DVE, lambda: nc.vector.scalar_tensor_tensor(out=msk[:], in0=nf[:], scalar=-C1, in1=ang[:],
                                                                 op0=ALU.mult, op1=ALU.add), [nf, ang], [msk])
                k.op(DVE, lambda: nc.vector.scalar_tensor_tensor(out=msk[:], in0=nf[:], scalar=-C2, in1=msk[:],
                                                                 op0=ALU.mult, op1=ALU.add), [nf, msk], [msk])
                if shift != 0.0:
                    k.op(DVE, lambda: nc.vector.tensor_scalar(out=msk[:], in0=msk[:], scalar1=shift, scalar2=None,
                                                              op0=ALU.add), [msk], [msk])
                k.op(DVE, lambda: nc.vector.tensor_scalar(out=nf[:], in0=msk[:], scalar1=math.pi, scalar2=-TWO_PI,
                                                          op0=ALU.is_gt, op1=ALU.mult), [msk], [nf])
                k.op(DVE, lambda: nc.vector.tensor_tensor(out=msk[:], in0=msk[:], in1=nf[:], op=ALU.add),
                     [msk, nf], [msk])
                k.op(DVE, lambda: nc.vector.tensor_scalar(out=nf[:], in0=msk[:], scalar1=-math.pi, scalar2=TWO_PI,
                                                          op0=ALU.is_lt, op1=ALU.mult), [msk], [nf])
                k.op(DVE, lambda: nc.vector.tensor_tensor(out=msk[:], in0=msk[:], in1=nf[:], op=ALU.add),
                     [msk, nf], [msk])
                k.op(DVE, lambda: nc.vector.tensor_scalar(out=msk[:], in0=msk[:], scalar1=3.1415925, scalar2=-3.1415925,
                                                          op0=ALU.min, op1=ALU.max), [msk], [msk])
                k.op(ACT, lambda: nc.scalar.activation(out=dst, in_=msk[:], func=AF.Sin), [msk], [self.rot])

            reduce_and_sin(math.pi / 2.0, self.rot[:, :, 0, :])
            reduce_and_sin(0.0, self.rot[:, :, 1, :])
            k.barrier()

    def _mem_prep(self):
        k, nc = self.k, self.nc
        P = self.P
        with contextlib.ExitStack() as es:
            gcol = k.sb("memg", [128, 8], F32, es)
            k.dma(gcol[:], self.w["mem_norm_col"][:, :], writes=[gcol])
            mt = k.sb("mem_t", [128, DM], F32, es)
            junk = k.sb("mem_junk", [128, DM], BF16, es)
            st = k.sb("mem_st", [128, 4], F32, es)
            hb = k.sb("mem_h", [128, DM], BF16, es)
            mT = k.sb("mem_T", [128, 8, 256], BF16, es)
            pb = P[2][:].bitcast(BF16)
            for b in range(2):
                for mb in range(2):
                    k.dma(mt[:], self.mem_in[b, mb * 128:(mb + 1) * 128, :], writes=[mt])
                    self._rms_to_bf16(mt, junk, st, hb, DM)
                    for c in range(8):
                        k.op(PE, lambda: nc.tensor.transpose(out=pb[:, c * 128:(c + 1) * 128],
                                                             in_=hb[:, c * 128:(c + 1) * 128], identity=self.ident[:]),
                             [hb, self.ident], [P[2]])
                    for c in range(8):
                        k.op(DVE, lambda: nc.vector.tensor_scalar(out=mT[:, c, mb * 128:(mb + 1) * 128],
                                                                  in0=pb[:, c * 128:(c + 1) * 128],
                                                                  scalar1=gcol[:, c:c + 1], scalar2=None, op0=ALU.mult),
                             [P[2], gcol], [mT])
                k.dma(self.s_memT[b], mT[:], reads=[mT], writes=[])
            k.barrier()

    def _rms_to_bf16(self, xt, junk, st, hb, n, eps=EPS):
        k, nc = self.k, self.nc
        k.op(ACT, lambda: nc.scalar.activation(out=junk[:, 0:n], in_=xt[:, 0:n], func=AF.Square, accum_out=st[:, 0:1]),
             [xt], [junk, st])
        k.op(ACT, lambda: nc.scalar.activation(out=st[:, 1:2], in_=st[:, 0:1], func=AF.Sqrt, scale=1.0 / n, bias=eps),
             [st], [st])
        k.op(DVE, lambda: nc.vector.reciprocal(out=st[:, 2:3], in_=st[:, 1:2]), [st], [st])
        k.op(DVE, lambda: nc.vector.tensor_scalar(out=hb[:, 0:n], in0=xt[:, 0:n], scalar1=st[:, 2:3], scalar2=None,
                                                  op0=ALU.mult), [xt, st], [hb])

    def _load_w(self, es, name, dram, rows, cols, gcol=None, wb=None):
        k, nc = self.k, self.nc
        nch = rows // 128
        if wb is None:
            wb = k.sb(name, [128, nch, cols], BF16, es)
        CW = 2048
        i = 0
        for c in range(nch):
            for c0 in range(0, cols, CW):
                cw = min(CW, cols - c0)
                stg = self.stage[i % 2]
                k.dma(stg[:, 0:cw], dram[c * 128:(c + 1) * 128, c0:c0 + cw], writes=[stg])
                eng = POOL if i % 2 == 0 else ACT
                if gcol is None:
                    if eng == POOL:
                        k.op(POOL, lambda: nc.gpsimd.tensor_copy(out=wb[:, c, c0:c0 + cw], in_=stg[:, 0:cw]), [stg], [wb])
                    else:
                        k.op(ACT, lambda: nc.scalar.copy(out=wb[:, c, c0:c0 + cw], in_=stg[:, 0:cw]), [stg], [wb])
                else:
                    if eng == POOL:
                        k.op(POOL, lambda: nc.gpsimd.tensor_scalar(out=wb[:, c, c0:c0 + cw], in0=stg[:, 0:cw],
                                                                   scalar1=gcol[:, c:c + 1], scalar2=1.0,
                                                                   op0=ALU.mult, op1=ALU.mult), [stg, gcol], [wb])
                    else:
                        k.op(ACT, lambda: nc.scalar.activation(out=wb[:, c, c0:c0 + cw], in_=stg[:, 0:cw],
                                                               func=AF.Copy, scale=gcol[:, c:c + 1]), [stg, gcol], [wb])
                i += 1
        return wb

    def _rotary(self, xv, tile_idx, f0, nf, tA, tB, out1, out2, reads, writes, nh):
        k, nc = self.k, self.nc
        cs = self.rot[:, tile_idx, 0, f0:f0 + nf].unsqueeze(1).unsqueeze(1).to_broadcast([128, nh, 2, nf])
        sn = self.rot[:, tile_idx, 1, f0:f0 + nf].unsqueeze(1).unsqueeze(1).to_broadcast([128, nh, 2, nf])
        a = tA[:, 0:nh * 2 * nf].rearrange("p (h t f) -> p h t f", h=nh, t=2)
        bq = tB[:, 0:nh * 2 * nf].rearrange("p (h t f) -> p h t f", h=nh, t=2)
        k.op(DVE, lambda: nc.vector.tensor_tensor(out=a, in0=xv, in1=cs, op=ALU.mult), reads + [self.rot], [tA])
        k.op(DVE, lambda: nc.vector.tensor_tensor(out=bq, in0=xv, in1=sn, op=ALU.mult), reads + [self.rot], [tB])
        k.op(POOL, lambda: nc.gpsimd.tensor_tensor(out=out1, in0=a[:, :, 0, :], in1=bq[:, :, 1, :], op=ALU.subtract),
             [tA, tB], writes)
        k.op(POOL, lambda: nc.gpsimd.tensor_tensor(out=out2, in0=a[:, :, 1, :], in1=bq[:, :, 0, :], op=ALU.add),
             [tA, tB], writes)

    def _mem_kv(self, es, wmem, tagp):
        k, nc = self.k, self.nc
        P = self.P
        kmT = k.sb(tagp + "kmT", [128, 2, 2, 256], BF16, es)
        vm = k.sb(tagp + "vm", [128, 2, 2, 4, 65], BF16, es)
        k.op(POOL, lambda: nc.gpsimd.memset(vm[:], 1.0), [], [vm])
        with contextlib.ExitStack() as es2:
            mT = k.sb(tagp + "memT", [128, 8, 256], BF16, es2)
            for b in range(2):
                k.dma(mT[:], self.s_memT[b], writes=[mT])
                for p in range(2):
                    for c in range(8):
                        k.op(PE, lambda: nc.tensor.matmul(P[0][:, p * 256:(p + 1) * 256],
                                                          lhsT=wmem[:, c, p * 128:(p + 1) * 128], rhs=mT[:, c, :],
                                                          start=(c == 0), stop=(c == 7)), [wmem, mT], [P[0]])
                k.op(ACT, lambda: nc.scalar.copy(out=kmT[:, b, :, :].rearrange("p a m -> p (a m)"), in_=P[0][:, 0:512]),
                     [P[0]], [kmT])
                for mb in range(2):
                    for c in range(8):
                        k.op(PE, lambda: nc.tensor.matmul(P[1][:, mb * 256:(mb + 1) * 256],
                                                          lhsT=mT[:, c, mb * 128:(mb + 1) * 128], rhs=wmem[:, c, 256:512],
                                                          start=(c == 0), stop=(c == 7)), [wmem, mT], [P[1]])
                for mb in range(2):
                    k.op(DVE, lambda: nc.vector.tensor_copy(
                        out=vm[:, b, mb, :, 0:64],
                        in_=P[1][:, mb * 256:(mb + 1) * 256].rearrange("p (h d) -> p h d", h=4)), [P[1]], [vm])
            k.barrier()
        return kmT, vm

    def _even_layer(self, l):
        k, nc = self.k, self.nc
        P = self.P
        pre = "l%d_" % l
        last = (l == self.n_layers - 1)
        src = self.x_in if l == 0 else self.out
        pb2 = P[2][:].bitcast(BF16)
        with contextlib.ExitStack() as es:
            self.stage = [k.sb("stgA", [128, 2048], F32, es), k.sb("stgB", [128, 2048], F32, es)]
            gcol = k.sb("gcol", [128, 8], F32, es)
            k.dma(gcol[:], self.w[pre + "norm_col"][:, :], writes=[gcol])
            wb = self._load_w(es, "wb", self.w[pre + "w_in"], DM, 4096, gcol)
            retTE = k.sb("retTE", [128, 4], F32, es)
            k.dma(retTE[:], self.cst["retTE"][:, :], writes=[retTE])
            xt = [k.sb("xt%d" % i, [128, DM], F32, es) for i in range(2)]
            junk = k.sb("junk", [128, DM], BF16, es)
            st = k.sb("st", [128, 4], F32, es)
            hb = k.sb("hb", [128, DM], BF16, es)
            hT = k.sb("hT", [128, 8, 128], BF16, es)
            pk = [k.sb("pk%d" % i, [128, EVEN_W], BF16, es) for i in range(2)]
            qk_b = k.sb("qk_b", [128, 2, 8, 64], BF16, es)
            qkr_b = k.sb("qkr_b", [128, 8, 64], BF16, es)
            vA_t = [k.sb("vA_t%d" % i, [128, 8, 65], BF16, es) for i in range(2)]
            kT_t = [k.sb("kT_t%d" % i, [128, 4, 128], BF16, es) for i in range(2)]
            tA = k.sb("rotA", [128, 512], F32, es)
            tB = k.sb("rotB", [128, 512], F32, es)
            for i in range(2):
                k.op(POOL, lambda: nc.gpsimd.memset(vA_t[i][:], 1.0), [], [vA_t[i]])
            tiles = [(b, i) for b in range(2) for i in range(NT)]
            k.dma(xt[0][:], src[0, 0:128, :], reads=[self.xbuf[0][0]], writes=[xt[0]])
            for ti, (b, i) in enumerate(tiles):
                X = xt[ti % 2]
                PK = pk[ti % 2]
                VA = vA_t[ti % 2]
                KT = kT_t[ti % 2]
                if ti + 1 < len(tiles):
                    nb, ni_ = tiles[ti + 1]
                    k.dma(xt[(ti + 1) % 2][:], src[nb, ni_ * 128:(ni_ + 1) * 128, :],
                          reads=[self.xbuf[nb][ni_]], writes=[xt[(ti + 1) % 2]])
                tix = b * NT + i
                self._rms_to_bf16(X, junk, st, hb, DM)
                for c in range(8):
                    k.op(PE, lambda: nc.tensor.transpose(out=pb2[:, c * 128:(c + 1) * 128],
                                                         in_=hb[:, c * 128:(c + 1) * 128], identity=self.ident[:]),
                         [hb, self.ident], [P[2]])
                k.op(ACT, lambda: nc.scalar.copy(out=hT[:].rearrange("p c t -> p (c t)"), in_=pb2[:, 0:1024]),
                     [P[2]], [hT])
                for n in range(8):
                    bk = P[n % 2]
                    for c in range(8):
                        k.op(PE, lambda: nc.tensor.matmul(bk[:, :], lhsT=hT[:, c, :], rhs=wb[:, c, n * 512:(n + 1) * 512],
                                                          start=(c == 0), stop=(c == 7)), [hT, wb], [bk])
                    if n in (0, 1):
                        dst = qk_b[:, n, :, :]
                        k.op(ACT, lambda: nc.scalar.copy(out=dst, in_=bk[:, :].rearrange("p (h d) -> p h d", h=8)),
                             [bk], [qk_b])
                        xv = bk[:, :].rearrange("p (h d) -> p h d", h=8)[:, :, 0:16].rearrange(
                            "p h (t f) -> p h t f", t=2)
                        self._rotary(xv, tix, 0, 8, tA, tB, qk_b[:, n, :, 0:8], qk_b[:, n, :, 8:16], [bk], [qk_b], 8)
                    elif n == 2:
                        k.op(ACT, lambda: nc.scalar.copy(out=VA[:, :, 0:64],
                                                         in_=bk[:, :].rearrange("p (h d) -> p h d", h=8)), [bk], [VA])
                    elif n == 3:
                        xv = bk[:, :].rearrange("p (h t f) -> p h t f", h=8, t=2)
                        self._rotary(xv, tix, 8, 32, tA, tB, qkr_b[:, :, 0:32], qkr_b[:, :, 32:64], [bk], [qkr_b], 8)
                    elif n == 4:
                        k.op(ACT, lambda: nc.scalar.copy(out=PK[:, E_VR:E_VR + 512], in_=bk[:, :]), [bk], [PK])
                    elif n == 5:
                        k.op(DVE, lambda: nc.vector.tensor_copy(out=junk[:, 0:256], in_=bk[:, 0:256]), [bk], [junk])
                        k.op(ACT, lambda: nc.scalar.activation(out=PK[:, E_SG:E_SG + 256], in_=bk[:, 256:512],
                                                               func=AF.Silu), [bk], [PK])
                    else:
                        o = E_SG + 256 + (n - 6) * 512
                        k.op(ACT, lambda: nc.scalar.activation(out=PK[:, o:o + 512], in_=bk[:, :], func=AF.Silu),
                             [bk], [PK])
                qkf = qk_b[:].rearrange("p a h d -> p (a h d)")
                for c in range(8):
                    k.op(PE, lambda: nc.tensor.transpose(out=pb2[:, c * 128:(c + 1) * 128],
                                                         in_=qkf[:, c * 128:(c + 1) * 128], identity=self.ident[:]),
                         [qk_b, self.ident], [P[2]])
                k.op(ACT, lambda: nc.scalar.copy(out=PK[:, E_QAT:E_QAT + 512], in_=pb2[:, 0:512]), [P[2]], [PK])
                k.op(DVE, lambda: nc.vector.tensor_copy(out=KT[:].rearrange("p a t -> p (a t)"), in_=pb2[:, 512:1024]),
                     [P[2]], [KT])
                k.op(DVE, lambda: nc.vector.tensor_tensor(
                    out=PK[:, E_KD:E_KD + 256].rearrange("p (h d) -> p h d", h=4), in0=qkr_b[:, 4:8, :],
                    in1=retTE[:].unsqueeze(2).to_broadcast([128, 4, 64]), op=ALU.mult), [qkr_b, retTE], [PK])
                qrf = qkr_b[:].rearrange("p h d -> p (h d)")
                for c in range(4):
                    k.op(PE, lambda: nc.tensor.transpose(out=pb2[:, c * 128:(c + 1) * 128],
                                                         in_=qrf[:, c * 128:(c + 1) * 128], identity=self.ident[:]),
                         [qkr_b, self.ident], [P[2]])
                for c in range(2):
                    k.op(PE, lambda: nc.tensor.transpose(out=pb2[:, (4 + c) * 128:(5 + c) * 128],
                                                         in_=junk[:, c * 128:(c + 1) * 128], identity=self.ident[:]),
                         [junk, self.ident], [P[2]])
                k.op(ACT, lambda: nc.scalar.copy(out=PK[:, E_QRT:E_QRT + 512], in_=pb2[:, 0:512]), [P[2]], [PK])
                k.op(DVE, lambda: nc.vector.tensor_copy(out=PK[:, E_QMT:E_QMT + 256], in_=pb2[:, 512:768]), [P[2]], [PK])
                k.dma(self.s_pk[b, i, :, 0:EVEN_W], PK[:], reads=[PK])
                k.dma(self.s_kT[b, :, :, i * 128:(i + 1) * 128], KT[:], reads=[KT])
                k.dma(self.s_vA[b, i], VA[:].rearrange("p h d -> p (h d)"), reads=[VA])
            k.barrier()
        if self.stop == "p1":
            return
        with contextlib.ExitStack() as es:
            self.stage = [k.sb("stgA2", [128, 2048], F32, es), k.sb("stgB2", [128, 2048], F32, es)]
            wout = self._load_w(es, "wout", self.w[pre + "w_out"], 1280, DM)
            wmem = self._load_w(es, "wmem", self.w[pre + "w_mem_kv"], DM, 512)
            kmT, vm = self._mem_kv(es, wmem, "e")
            maskA = k.sb("maskA", [128, 16, 128], BF16, es)
            retDT = k.sb("retDT", [128, 4, 128], F32, es)
            retFS = k.sb("retFS", [128, 2, 128], F32, es)
            retG = k.sb("retG", [128, 2], F32, es)
            k.dma(maskA[:], self.cst["maskA"], writes=[maskA])
            k.dma(retDT[:], self.cst["retDT"], writes=[retDT])
            k.dma(retFS[:], self.cst["retFS"], writes=[retFS])
            k.dma(retG[:], self.cst["retG"], writes=[retG])
            if last and self.final_norm:
                gfin = k.sb("gfin", [128, DM], F32, es)
                k.dma(gfin[:], self.w["final_norm"][0:1, :].broadcast_to([128, DM]), writes=[gfin])
            kT = k.sb("kTc", [128, 4, SEQ], BF16, es)
            vA = k.sb("vAc", [128, NT, 8, 65], BF16, es)
            xt = [k.sb("xt2_%d" % i, [128, DM], F32, es) for i in range(3)]
            pk = [k.sb("pk2_%d" % i, [128, EVEN_W], BF16, es) for i in range(3)]
            pT = [[k.sb("pT%d_%d" % (i, j), [128, 4, 128], BF16, es) for j in range(2)] for i in range(2)]
            SBANKS = [(P[3], P[4]), (P[0], P[1])]
            sTb = k.sb("sTb", [128, 4, 128], BF16, es)
            qs = k.sb("qs", [128, 2, 128], BF16, es)
            st_f = k.sb("st_f", [128, 2, 128], F32, es)
            st_b = k.sb("st_b", [128, 2, 128], BF16, es)
            rec = k.sb("rec", [128, 16], F32, es)
            y2 = [k.sb("y%d" % i, [128, 1280], F32, es) for i in range(2)]
            ob = k.sb("ob", [128, 512], F32, es)
            sq = k.sb("sq", [128, 512], F32, es)
            yg = k.sb("yg", [128, 1280], BF16, es)
            ygT = k.sb("ygT", [128, 10, 128], BF16, es)
            xo = k.sb("xo", [128, DM], F32, es)
            junk = k.sb("junk2", [128, DM], BF16, es)
            st = k.sb("st2", [128, 8], F32, es)
            tiles = [(b, i) for b in range(2) for i in range(NT)]

            def prefetch(ti):
                b_, i_ = tiles[ti]
                k.dma(xt[ti % 3][:], src[b_, i_ * 128:(i_ + 1) * 128, :], reads=[self.xbuf[b_][i_]], writes=[xt[ti % 3]])
                k.dma(pk[ti % 3][:], self.s_pk[b_, i_, :, 0:EVEN_W], writes=[pk[ti % 3]])

            NTL = len(tiles)
            prefetch(0)
            if NTL > 1:
                prefetch(1)
            scl = [0]

            def T1(ti, b, i):
                PK = pk[ti % 3]
                y = y2[ti % 2]
                sc = scl[0]
                if i == 0:
                    k.dma(kT[:], self.s_kT[b], writes=[kT])
                    k.dma(vA[:].rearrange("p i h d -> p i (h d)"), self.s_vA[b].rearrange("i p w -> p i w"), writes=[vA])
                qaT = PK[:, E_QAT:E_QAT + 512].rearrange("p (a t) -> p a t", a=4)
                qrT = PK[:, E_QRT:E_QRT + 256].rearrange("p (a t) -> p a t", a=2)
                krT = PK[:, E_KRT:E_KRT + 256].rearrange("p (a t) -> p a t", a=2)
                kd = PK[:, E_KD:E_KD + 256].rearrange("p (h d) -> p h d", h=4)
                vr = PK[:, E_VR:E_VR + 512].rearrange("p (h e) -> p h e", h=4)
                qmT = PK[:, E_QMT:E_QMT + 256].rearrange("p (a t) -> p a t", a=2)
                sg = PK[:, E_SG:E_SG + 1280]
                first = [True, True]

                def qk_a(j, sc_):
                    Sb_ = SBANKS[sc_ % 2]
                    for p_ in range(4):
                        for s_ in range(2):
                            k.op(PE, lambda: nc.tensor.matmul(Sb_[s_][:, p_ * 128:(p_ + 1) * 128],
                                                              lhsT=kT[64 * s_:64 * s_ + 64, p_, j * 128:(j + 1) * 128],
                                                              rhs=qaT[64 * s_:64 * s_ + 64, p_, :], start=True, stop=True),
                                 [kT, PK], [Sb_[s_]])

                qk_a(0, sc)
                for j in range(i + 1):
                    Sb = SBANKS[sc % 2]
                    ptp = pT[sc % 2]
                    sc += 1
                    scl[0] = sc
                    if j + 1 <= i:
                        qk_a(j + 1, sc)
                    for s_ in range(2):
                        pt = ptp[s_]
                        k.op(ACT, lambda: nc.scalar.activation(out=pt[:].rearrange("p h q -> p (h q)"), in_=Sb[s_][:, :],
                                                               func=AF.Exp, scale=0.125), [Sb[s_]], [pt])
                        k.op(DVE if s_ == 0 else POOL, lambda: (nc.vector if s_ == 0 else nc.gpsimd).tensor_tensor(
                            out=pt[:], in0=pt[:], in1=maskA[:, i - j, :].unsqueeze(1).to_broadcast([128, 4, 128]),
                            op=ALU.mult), [pt, maskA], [pt])
                        O = P[5 + s_]
                        for p_ in range(4):
                            k.op(PE, lambda: nc.tensor.matmul(O[:, p_ * 65:(p_ + 1) * 65], lhsT=pt[:, p_, :],
                                                              rhs=vA[:, j, 2 * p_ + s_, :], start=first[s_], stop=(j == i),
                                                              skip_group_check=True), [pt, vA], [O])
                            first[s_] = False
                    yield
                for s_ in range(2):
                    O = P[5 + s_]
                    ov = O[:, 0:260].rearrange("p (h d) -> p h d", h=4)
                    k.op(DVE, lambda: nc.vector.reciprocal(out=rec[:, s_ * 4:s_ * 4 + 4], in_=ov[:, :, 64]), [O], [rec])
                    k.op(DVE, lambda: nc.vector.tensor_tensor(
                        out=y[:, 0:512].rearrange("p (a s d) -> p a s d", a=4, s=2)[:, :, s_, :], in0=ov[:, :, 0:64],
                        in1=rec[:, s_ * 4:s_ * 4 + 4].unsqueeze(2).to_broadcast([128, 4, 64]), op=ALU.mult),
                        [O, rec], [y])
                scl[0] = sc
                yield

            def T2(ti, b, i):
                PK = pk[ti % 3]
                y = y2[ti % 2]
                sc = scl[0]
                qaT = PK[:, E_QAT:E_QAT + 512].rearrange("p (a t) -> p a t", a=4)
                qrT = PK[:, E_QRT:E_QRT + 256].rearrange("p (a t) -> p a t", a=2)
                krT = PK[:, E_KRT:E_KRT + 256].rearrange("p (a t) -> p a t", a=2)
                kd = PK[:, E_KD:E_KD + 256].rearrange("p (h d) -> p h d", h=4)
                vr = PK[:, E_VR:E_VR + 512].rearrange("p (h e) -> p h e", h=4)
                qmT = PK[:, E_QMT:E_QMT + 256].rearrange("p (a t) -> p a t", a=2)
                sg = PK[:, E_SG:E_SG + 1280]
                Sb = SBANKS[sc % 2]
                sc += 1
                for h in range(4):
                    s_, p_ = h % 2, h // 2
                    k.op(PE, lambda: nc.tensor.matmul(Sb[s_][:, p_ * 128:(p_ + 1) * 128], lhsT=krT[64 * s_:64 * s_ + 64, p_, :],
                                                      rhs=qrT[64 * s_:64 * s_ + 64, p_, :], start=True, stop=True),
                         [PK], [Sb[s_]])
                for s_ in range(2):
                    k.op(DVE, lambda: nc.vector.tensor_tensor(
                        out=sTb[:].rearrange("p (a s) q -> p a s q", s=2)[:, :, s_, :],
                        in0=Sb[s_][:, 0:256].rearrange("p (a q) -> p a q", a=2),
                        in1=retDT[:].rearrange("p (a s) q -> p a s q", s=2)[:, :, s_, :], op=ALU.mult),
                        [Sb[s_], retDT], [sTb])
                if i > 0:
                    k.op(POOL, lambda: nc.gpsimd.tensor_tensor(out=qs[:], in0=qrT, in1=retFS[:], op=ALU.mult),
                         [PK, retFS], [qs])
                OB = P[0]
                for h in range(4):
                    s_, p_ = h % 2, h // 2
                    k.op(PE, lambda: nc.tensor.matmul(OB[:, h * 128:(h + 1) * 128], lhsT=sTb[:, h, :], rhs=vr[:, h, :],
                                                      start=True, stop=(i == 0)), [sTb, PK], [OB])
                    if i > 0:
                        k.op(PE, lambda: nc.tensor.matmul(OB[:, h * 128:(h + 1) * 128], lhsT=qs[64 * s_:64 * s_ + 64, p_, :],
                                                          rhs=st_b[64 * s_:64 * s_ + 64, p_, :], start=False, stop=True),
                             [qs, st_b], [OB])
                SU = P[1]
                for h in range(4):
                    s_, p_ = h % 2, h // 2
                    k.op(PE, lambda: nc.tensor.matmul(SU[64 * s_:64 * s_ + 64, p_ * 128:(p_ + 1) * 128], lhsT=kd[:, h, :],
                                                      rhs=vr[:, h, :], start=True, stop=True), [PK], [SU])
                if i == 0:
                    k.op(DVE, lambda: nc.vector.tensor_copy(out=st_f[:].rearrange("p a e -> p (a e)"), in_=SU[:, 0:256]),
                         [SU], [st_f])
                else:
                    for p_ in range(2):
                        k.op(DVE, lambda: nc.vector.scalar_tensor_tensor(
                            out=st_f[:, p_, :], in0=st_f[:, p_, :], scalar=retG[:, p_:p_ + 1],
                            in1=SU[:, p_ * 128:(p_ + 1) * 128], op0=ALU.mult, op1=ALU.add), [st_f, retG, SU], [st_f])
                k.op(ACT, lambda: nc.scalar.copy(out=ob[:], in_=OB[:, :]), [OB], [ob])
                k.op(POOL, lambda: nc.gpsimd.tensor_copy(out=st_b[:], in_=st_f[:]), [st_f], [st_b])
                k.op(POOL, lambda: nc.gpsimd.tensor_tensor(out=sq[:], in0=ob[:], in1=ob[:], op=ALU.mult), [ob], [sq])
                k.op(DVE, lambda: nc.vector.tensor_reduce(out=st[:, 0:4], in_=sq[:].rearrange("p (h e) -> p h e", h=4),
                                                          axis=AX.X, op=ALU.add), [sq], [st])
                k.op(ACT, lambda: nc.scalar.activation(out=st[:, 4:8], in_=st[:, 0:4], func=AF.Sqrt, scale=1.0 / 128,
                                                       bias=EPS), [st], [st])
                k.op(DVE, lambda: nc.vector.reciprocal(out=st[:, 0:4], in_=st[:, 4:8]), [st], [st])
                k.op(DVE, lambda: nc.vector.tensor_tensor(
                    out=y[:, 512:1024].rearrange("p (h e) -> p h e", h=4), in0=ob[:].rearrange("p (h e) -> p h e", h=4),
                    in1=st[:, 0:4].unsqueeze(2).to_broadcast([128, 4, 128]), op=ALU.mult), [ob, st], [y])
                self._mem_attn(b, qmT, PK, kmT, vm, pT, rec, y, sc, SBANKS)
                sc += 2
                scl[0] = sc

            def T3(ti, b, i):
                PK = pk[ti % 3]
                sg = PK[:, E_SG:E_SG + 1280]
                return self._tail_gen(PK, sg, y2[ti % 2], yg, ygT, wout, xt[ti % 3], xo, junk, st, b, i, last,
                                      gfin if (last and self.final_norm) else None, ob=7)

            def drain(*gens):
                alive = [g for g in gens if g is not None]
                while alive:
                    for g in list(alive):
                        try:
                            next(g)
                        except StopIteration:
                            alive.remove(g)

            drain(T1(0, *tiles[0]))
            T2(0, *tiles[0])
            for ti in range(NTL):
                if ti + 2 < NTL:
                    prefetch(ti + 2)
                drain(T3(ti, *tiles[ti]), T1(ti + 1, *tiles[ti + 1]) if ti + 1 < NTL else None)
                if ti + 1 < NTL:
                    T2(ti + 1, *tiles[ti + 1])
            k.barrier()

    def _odd_layer(self, l):
        k, nc = self.k, self.nc
        P = self.P
        pre = "l%d_" % l
        last = (l == self.n_layers - 1)
        src = self.out
        dc = 1664 if l == 1 else 1696
        ntd = (dc + 127) // 128
        c_qm = 416 + dc
        ncols = c_qm + 1536
        vres = (l == 3)
        pb2 = P[2][:].bitcast(BF16)
        tiles = [(b, i) for b in range(2) for i in range(NT)]
        NEG_E = -math.exp(-0.5)
        with contextlib.ExitStack() as es:
            self.stage = [k.sb("stgA", [128, 2048], F32, es), k.sb("stgB", [128, 2048], F32, es)]
            gcol = k.sb("gcol", [128, 8], F32, es)
            qn = k.sb("qn", [128, 2], F32, es)
            kvn = k.sb("kvn", [128, 1], F32, es)
            cols = k.sb("cols", [128, NCOLS], F32, es)
            k.dma(gcol[:], self.w[pre + "norm_col"][:, :], writes=[gcol])
            k.dma(qn[:], self.w[pre + "qn_col"][:, :], writes=[qn])
            k.dma(kvn[:], self.w[pre + "kvn_col"][:, :], writes=[kvn])
            k.dma(cols[:], self.w[pre + "cols"][:, :], writes=[cols])
            wb = self._load_w(es, "wbo", self.w[pre + "w_in"], DM, ncols, gcol)
            wqb = self._load_w(es, "wqb", self.w[pre + "w_qb"], 256, 768, qn)
            wkvb = self._load_w(es, "wkvb", self.w[pre + "w_kvb"], 128, 1024, kvn)
            w2sb = k.sb("w2sb", [128, 512], F32, es)
            a2sb = k.sb("a2sb", [128, 512], F32, es)
            k.dma(w2sb[0:64, :], self.w[pre + "w2"][:, :], writes=[w2sb])
            k.dma(a2sb[64:128, :], self.w[pre + "a2"][:, :], writes=[a2sb])
            if vres:
                v2sb = k.sb("v2sb", [128, 512], F32, es)
                k.dma(v2sb[0:32, :], self.w[pre + "v2"][:, :], writes=[v2sb])
                vft = [k.sb("vft%d" % i, [128, 4, 128], F32, es) for i in range(2)]
            onesblk = k.sb("onesblk", [128, 128], BF16, es)
            sqb = k.sb("sqb", [128, 4, 128], BF16, es)
            scanrst = k.sb("scanrst", [128, 128], F32, es)
            k.dma(onesblk[:], self.cst["onesblk"], writes=[onesblk])
            k.dma(scanrst[:], self.cst["scanrst"], writes=[scanrst])
            xt = [k.sb("xt%d" % i, [128, DM], F32, es) for i in range(2)]
            junk = k.sb("junk", [128, DM], BF16, es)
            st = k.sb("st", [128, 8], F32, es)
            hb = k.sb("hb", [128, DM], BF16, es)
            hT = k.sb("hT", [128, 8, 128], BF16, es)
            pk = [k.sb("pk%d" % i, [128, ODD_W], BF16, es) for i in range(2)]
            mq_b = k.sb("mq_b", [128, 512], BF16, es)
            qm_b = k.sb("qm_b", [128, 256], BF16, es)
            cqnT = k.sb("cqnT", [128, 2, 128], BF16, es)
            ckvnT = k.sb("ckvnT", [128, 128], BF16, es)
            kpeT = k.sb("kpeT", [128, 128], BF16, es)
            q_b = k.sb("q_b", [128, 8, 96], BF16, es)
            KTt = [k.sb("KTt%d" % i, [128, 8, 128], BF16, es) for i in range(2)]
            VMt = [k.sb("VMt%d" % i, [128, 8, 65], BF16, es) for i in range(2)]
            tA = k.sb("rotA", [128, 512], F32, es)
            tB = k.sb("rotB", [128, 512], F32, es)
            d_fs = [k.sb("d_f%d" % i, [128, 14, 128], F32, es) for i in range(2)]
            diff = k.sb("diff", [128, 14, 128], F32, es)
            carry = k.sb("carry", [128, 14], F32, es)
            tw = k.sb("tw", [128, 128], F32, es)
            G = [k.sb("g%d" % i, [128, 4, 128], F32, es) for i in range(12)]
            pcs = [k.sb("pcs%d" % i, [128, 8], F32, es) for i in range(2)]
            for i in range(2):
                k.op(POOL, lambda: nc.gpsimd.memset(VMt[i][:], 1.0), [], [VMt[i]])
                k.op(POOL, lambda: nc.gpsimd.memset(pk[i][:], 0.0), [], [pk[i]])
                k.op(POOL, lambda: nc.gpsimd.memset(KTt[i][:], 0.0), [], [KTt[i]])
            for i in range(2):
                k.op(POOL, lambda: nc.gpsimd.memset(d_fs[i][:], 0.0), [], [d_fs[i]])
            k.op(POOL, lambda: nc.gpsimd.memset(mq_b[:], 0.0), [], [mq_b])

            def cb(c0, n=4):
                return cols[:, c0:c0 + n].unsqueeze(2).to_broadcast([128, n, 128])

            def v4(t):
                return t[:].rearrange("p n t -> p (n t)")

            def load(ti):
                b_, i_ = tiles[ti]
                k.dma(xt[ti % 2][:], src[b_, i_ * 128:(i_ + 1) * 128, :], reads=[self.xbuf[b_][i_]], writes=[xt[ti % 2]])

            def load_vf(ti):
                b_, i_ = tiles[ti]
                k.dma(vft[ti % 2][:].rearrange("p n t -> p (n t)"), self.s_vf[b_, i_], writes=[vft[ti % 2]])

            load(0)
            if vres:
                load_vf(0)
            def F_gen(ti, b, i):
                d_f = d_fs[ti % 2]
                X = xt[ti % 2]
                PK = pk[ti % 2]
                KT = KTt[ti % 2]
                VM = VMt[ti % 2]
                if ti + 1 < len(tiles):
                    load(ti + 1)
                tix = b * NT + i
                self._rms_to_bf16(X, junk, st, hb, DM)
                for c in range(8):
                    k.op(PE, lambda: nc.tensor.transpose(out=pb2[:, c * 128:(c + 1) * 128],
                                                         in_=hb[:, c * 128:(c + 1) * 128], identity=self.ident[:]),
                         [hb, self.ident], [P[2]])
                k.op(ACT, lambda: nc.scalar.copy(out=hT[:].rearrange("p c t -> p (c t)"), in_=pb2[:, 0:1024]),
                     [P[2]], [hT])
                yield
                for c in range(8):
                    k.op(PE, lambda: nc.tensor.matmul(P[0][:, 0:416], lhsT=hT[:, c, :], rhs=wb[:, c, 0:416],
                                                      start=(c == 0), stop=(c == 7)), [hT, wb], [P[0]])
                for (c0, c1, so) in ((0, 256, 0), (256, 384, 3)):
                    n_ = c1 - c0
                    k.op(ACT, lambda: nc.scalar.activation(out=junk[:, c0:c1], in_=P[0][:, c0:c1], func=AF.Square,
                                                           accum_out=st[:, so:so + 1]), [P[0]], [junk, st])
                    k.op(ACT, lambda: nc.scalar.activation(out=st[:, so + 1:so + 2], in_=st[:, so:so + 1], func=AF.Sqrt,
                                                           scale=1.0 / n_, bias=EPS), [st], [st])
                    k.op(DVE, lambda: nc.vector.reciprocal(out=st[:, so + 2:so + 3], in_=st[:, so + 1:so + 2]), [st], [st])
                    k.op(DVE, lambda: nc.vector.tensor_scalar(out=mq_b[:, c0:c1], in0=P[0][:, c0:c1],
                                                              scalar1=st[:, so + 2:so + 3], scalar2=None, op0=ALU.mult),
                         [P[0], st], [mq_b])
                xv = P[0][:, 384:416].rearrange("p (h t f) -> p h t f", h=1, t=2)
                self._rotary(xv, tix, 40, 16, tA, tB, mq_b[:, 384:400].unsqueeze(1), mq_b[:, 400:416].unsqueeze(1),
                             [P[0]], [mq_b], 1)
                yield
                for g in range(3):
                    bk = P[(g + 1) % 2]
                    c0 = c_qm + g * 512
                    for c in range(8):
                        k.op(PE, lambda: nc.tensor.matmul(bk[:, :], lhsT=hT[:, c, :], rhs=wb[:, c, c0:c0 + 512],
                                                          start=(c == 0), stop=(c == 7)), [hT, wb], [bk])
                    if g == 0:
                        k.op(DVE, lambda: nc.vector.tensor_copy(out=qm_b[:], in_=bk[:, 0:256]), [bk], [qm_b])
                        k.op(ACT, lambda: nc.scalar.activation(out=PK[:, O_SG:O_SG + 256], in_=bk[:, 256:512],
                                                               func=AF.Silu), [bk], [PK])
                    else:
                        o = O_SG + 256 + (g - 1) * 512
                        k.op(ACT, lambda: nc.scalar.activation(out=PK[:, o:o + 512], in_=bk[:, :], func=AF.Silu),
                             [bk], [PK])
                yield
                for c in range(3):
                    k.op(PE, lambda: nc.tensor.transpose(out=pb2[:, c * 128:(c + 1) * 128],
                                                         in_=mq_b[:, c * 128:(c + 1) * 128], identity=self.ident[:]),
                         [mq_b, self.ident], [P[2]])
                k.op(PE, lambda: nc.tensor.transpose(out=pb2[64:96, 384:512], in_=mq_b[:, 384:416], identity=self.ident[:]),
                     [mq_b, self.ident], [P[2]])
                for c in range(2):
                    k.op(PE, lambda: nc.tensor.transpose(out=pb2[:, (4 + c) * 128:(5 + c) * 128],
                                                         in_=qm_b[:, c * 128:(c + 1) * 128], identity=self.ident[:]),
                         [qm_b, self.ident], [P[2]])
                k.op(ACT, lambda: nc.scalar.copy(out=cqnT[:].rearrange("p c t -> p (c t)"), in_=pb2[:, 0:256]), [P[2]], [cqnT])
                k.op(DVE, lambda: nc.vector.tensor_copy(out=ckvnT[:], in_=pb2[:, 256:384]), [P[2]], [ckvnT])
                k.op(DVE, lambda: nc.vector.tensor_copy(out=kpeT[64:96, :], in_=pb2[64:96, 384:512]), [P[2]], [kpeT])
                k.op(ACT, lambda: nc.scalar.copy(out=PK[:, O_QMT:O_QMT + 256], in_=pb2[:, 512:768]), [P[2]], [PK])
                yield
                for n2 in range(2):
                    bk = P[n2]
                    for c in range(2):
                        k.op(PE, lambda: nc.tensor.matmul(bk[:, 0:384], lhsT=cqnT[:, c, :],
                                                          rhs=wqb[:, c, n2 * 384:(n2 + 1) * 384],
                                                          start=(c == 0), stop=(c == 1)), [cqnT, wqb], [bk])
                    qv = bk[:, 0:384].rearrange("p (h d) -> p h d", h=4)
                    k.op(ACT, lambda: nc.scalar.copy(out=q_b[:, n2 * 4:(n2 + 1) * 4, 0:64], in_=qv[:, :, 0:64]), [bk], [q_b])
                    xv = qv[:, :, 64:96].rearrange("p h (t f) -> p h t f", t=2)
                    self._rotary(xv, tix, 40, 16, tA, tB, q_b[:, n2 * 4:(n2 + 1) * 4, 64:80],
                                 q_b[:, n2 * 4:(n2 + 1) * 4, 80:96], [bk], [q_b], 4)
                for h in range(8):
                    k.op(PE, lambda: nc.tensor.transpose(out=pb2[0:96, h * 128:(h + 1) * 128], in_=q_b[:, h, :],
                                                         identity=self.ident[:]), [q_b, self.ident], [P[2]])
                k.op(ACT, lambda: nc.scalar.copy(out=PK[0:96, O_QT:O_QT + 1024], in_=pb2[0:96, 0:1024]), [P[2]], [PK])
                yield
                for h in range(8):
                    bk = P[h // 4]
                    k.op(PE, lambda: nc.tensor.matmul(bk[0:64, (h % 4) * 128:(h % 4 + 1) * 128],
                                                      lhsT=wkvb[:, 0, h * 128:h * 128 + 64], rhs=ckvnT[:, :],
                                                      start=True, stop=True), [wkvb, ckvnT], [bk])
                k.op(ACT, lambda: nc.scalar.copy(out=KT[0:64, 0:4, :].rearrange("p h t -> p (h t)"), in_=P[0][0:64, :]),
                     [P[0]], [KT])
                k.op(DVE, lambda: nc.vector.tensor_copy(out=KT[0:64, 4:8, :].rearrange("p h t -> p (h t)"), in_=P[1][0:64, :]),
                     [P[1]], [KT])
                k.op(POOL, lambda: nc.gpsimd.tensor_copy(out=KT[64:96, :, :],
                                                         in_=kpeT[64:96, :].unsqueeze(1).to_broadcast([32, 8, 128])),
                     [kpeT], [KT])
                k.dma(self.s_kTM[b, :, :, i * 128:(i + 1) * 128], KT[0:96, :, :], reads=[KT])
                k.op(PE, lambda: nc.tensor.matmul(P[0][:, :], lhsT=ckvnT[:, :],
                                                  rhs=wkvb[:, 0, :].rearrange("p (h x) -> p h x", h=8)[:, :, 64:128],
                                                  start=True, stop=True), [wkvb, ckvnT], [P[0]])
                k.op(ACT, lambda: nc.scalar.copy(out=VM[:, :, 0:64], in_=P[0][:, :].rearrange("p (h d) -> p h d", h=8)),
                     [P[0]], [VM])
                k.dma(self.s_vA[b, i], VM[:].rearrange("p h d -> p (h d)"), reads=[VM])
                yield
                for g0 in range(0, ntd, 4):
                    bk = P[(g0 // 4) % 2]
                    cnt = min(4, ntd - g0)
                    for sl in range(cnt):
                        nt = g0 + sl
                        rows = min(128, dc - nt * 128)
                        for c in range(8):
                            k.op(PE, lambda: nc.tensor.matmul(bk[0:rows, sl * 128:(sl + 1) * 128],
                                                              lhsT=wb[:, c, 416 + nt * 128:416 + nt * 128 + rows],
                                                              rhs=hT[:, c, :], start=(c == 0), stop=(c == 7)), [hT, wb], [bk])
                    full = cnt if (dc - (g0 + cnt - 1) * 128) >= 128 else cnt - 1
                    if full > 0:
                        k.op(ACT, lambda: nc.scalar.copy(out=d_f[:, g0:g0 + full, :].rearrange("p n t -> p (n t)"),
                                                         in_=bk[:, 0:full * 128]), [bk], [d_f])
                    if full < cnt:
                        rows = dc - (g0 + cnt - 1) * 128
                        k.op(ACT, lambda: nc.scalar.copy(out=d_f[0:rows, g0 + cnt - 1, :],
                                                         in_=bk[0:rows, (cnt - 1) * 128:cnt * 128]), [bk], [d_f])
            def R_gen(ti, b, i):
                PK = pk[ti % 2]
                d_f = d_fs[ti % 2]
                if vres and ti + 1 < len(tiles):
                    load_vf(ti + 1)
                k.op(POOL, lambda: nc.gpsimd.tensor_tensor(out=diff[:, :, 1:128], in0=d_f[:, :, 0:127], in1=d_f[:, :, 1:128],
                                                           op=ALU.subtract), [d_f], [diff])
                if i == 0:
                    k.op(POOL, lambda: nc.gpsimd.tensor_scalar(out=diff[:, :, 0], in0=d_f[:, :, 0], scalar1=-1.0, scalar2=1.0,
                                                               op0=ALU.mult, op1=ALU.mult), [d_f], [diff])
                else:
                    k.op(POOL, lambda: nc.gpsimd.tensor_tensor(out=diff[:, :, 0], in0=carry[:, :], in1=d_f[:, :, 0],
                                                               op=ALU.subtract), [carry, d_f], [diff])
                k.op(POOL, lambda: nc.gpsimd.tensor_copy(out=carry[:, :], in_=d_f[:, :, 127]), [d_f], [carry])
                k.op(DVE, lambda: nc.vector.tensor_tensor(out=diff[:], in0=diff[:], in1=cb(C_MU, 14), op=ALU.mult),
                     [diff, cols], [diff])
                sh = diff
                k.op(DVE, lambda: nc.vector.tensor_tensor(out=sh[:], in0=diff[:], in1=d_f[:], op=ALU.add), [diff, d_f], [sh])
                R_ = sh[:, 0:4, :]
                K_ = sh[:, 4:8, :]
                V_ = sh[:, 8:12, :]
                sgw, alr, kx, tmp, kmod, bb, lp, eP, eNP, ePp, eD, tmp2 = G
                yield
                k.op(ACT, lambda: nc.scalar.activation(out=tw[0:64, :], in_=sh[0:64, 12, :], func=AF.Tanh), [sh], [tw])
                for nt in range(4):
                    k.op(PE, lambda: nc.tensor.matmul(P[3][:, nt * 128:(nt + 1) * 128], lhsT=w2sb[0:64, nt * 128:(nt + 1) * 128],
                                                      rhs=tw[0:64, :], start=True, stop=True), [w2sb, tw], [P[3]])
                for nt in range(4):
                    k.op(PE, lambda: nc.tensor.matmul(P[4][:, nt * 128:(nt + 1) * 128], lhsT=a2sb[64:128, nt * 128:(nt + 1) * 128],
                                                      rhs=sh[64:128, 12, :], start=True, stop=True), [a2sb, sh], [P[4]])
                for nt in range(4):
                    k.op(ACT, lambda: nc.scalar.activation(out=sgw[:, nt, :], in_=P[3][:, nt * 128:(nt + 1) * 128],
                                                           func=AF.Sigmoid, bias=cols[:, C_W0 + nt:C_W0 + nt + 1]),
                         [P[3], cols], [sgw])
                for nt in range(4):
                    k.op(ACT, lambda: nc.scalar.activation(out=alr[:, nt, :], in_=P[4][:, nt * 128:(nt + 1) * 128],
                                                           func=AF.Sigmoid, bias=cols[:, C_A0 + nt:C_A0 + nt + 1]),
                         [P[4], cols], [alr])
                if vres:
                    VF = vft[ti % 2]
                    for nt in range(4):
                        k.op(PE, lambda: nc.tensor.matmul(P[5][:, nt * 128:(nt + 1) * 128], lhsT=v2sb[0:32, nt * 128:(nt + 1) * 128],
                                                          rhs=sh[0:32, 13, :], start=True, stop=True), [v2sb, sh], [P[5]])
                    for nt in range(4):
                        k.op(ACT, lambda: nc.scalar.activation(out=tmp[:, nt, :], in_=P[5][:, nt * 128:(nt + 1) * 128],
                                                               func=AF.Sigmoid, bias=cols[:, C_V0 + nt:C_V0 + nt + 1]),
                             [P[5], cols], [tmp])
                    k.op(POOL, lambda: nc.gpsimd.tensor_tensor(out=tmp2[:], in0=VF[:], in1=V_, op=ALU.subtract), [VF, sh], [tmp2])
                    k.op(POOL, lambda: nc.gpsimd.tensor_tensor(out=tmp2[:], in0=tmp2[:], in1=tmp[:], op=ALU.mult), [tmp2, tmp], [tmp2])
                    k.op(POOL, lambda: nc.gpsimd.tensor_tensor(out=V_, in0=V_, in1=tmp2[:], op=ALU.add), [sh, tmp2], [sh])
                else:
                    k.dma(self.s_vf[b, i].rearrange("p (n t) -> p n t", n=4), V_, reads=[sh])
                k.op(DVE, lambda: nc.vector.tensor_scalar(out=v4(sgw), in0=v4(sgw), scalar1=NEG_E, scalar2=None, op0=ALU.mult),
                     [sgw], [sgw])
                lw = sgw
                yield
                k.op(POOL, lambda: nc.gpsimd.tensor_tensor(out=kx[:], in0=K_, in1=cb(C_KK), op=ALU.mult), [sh, cols], [kx])
                k.op(POOL, lambda: nc.gpsimd.tensor_tensor(out=sqb[:], in0=kx[:], in1=kx[:], op=ALU.mult), [kx], [sqb])
                for nt in range(4):
                    k.op(PE, lambda: nc.tensor.matmul(P[6][:, nt * 128:(nt + 1) * 128], lhsT=onesblk[:, :], rhs=sqb[:, nt, :],
                                                      start=True, stop=True), [onesblk, sqb], [P[6]])
                k.op(ACT, lambda: nc.scalar.activation(out=v4(tmp2), in_=P[6][:, :], func=AF.Sqrt), [P[6]], [tmp2])
                k.op(DVE, lambda: nc.vector.tensor_scalar(out=v4(tmp2), in0=v4(tmp2), scalar1=1e-12, scalar2=None, op0=ALU.max),
                     [tmp2], [tmp2])
                k.op(DVE, lambda: nc.vector.reciprocal(out=v4(tmp2), in_=v4(tmp2)), [tmp2], [tmp2])
                yield
                kk = kx
                k.op(POOL, lambda: nc.gpsimd.tensor_tensor(out=kk[:], in0=kx[:], in1=tmp2[:], op=ALU.mult), [kx, tmp2], [kk])
                k.op(DVE, lambda: nc.vector.tensor_tensor(out=tmp[:], in0=alr[:], in1=cb(C_KA), op=ALU.mult), [alr, cols], [tmp])
                k.op(DVE, lambda: nc.vector.tensor_tensor(out=tmp[:], in0=tmp[:], in1=cb(C_KA), op=ALU.subtract), [tmp, cols], [tmp])
                k.op(DVE, lambda: nc.vector.scalar_tensor_tensor(out=v4(kmod), in0=v4(tmp), scalar=1.0,
                                                                 in1=K_.rearrange("p n t -> p (n t)"),
                                                                 op0=ALU.add, op1=ALU.mult), [tmp, sh], [kmod])
                k.op(POOL, lambda: nc.gpsimd.tensor_tensor(out=bb[:], in0=kk[:], in1=alr[:], op=ALU.mult), [kk, alr], [bb])
                yield
                k.op(POOL, lambda: nc.gpsimd.tensor_tensor(out=tmp2[:], in0=R_, in1=cb(C_RK), op=ALU.mult), [sh, cols], [tmp2])
                k.op(POOL, lambda: nc.gpsimd.tensor_tensor(out=sqb[:], in0=tmp2[:], in1=kmod[:], op=ALU.mult), [tmp2, kmod], [sqb])
                for nt in range(4):
                    k.op(PE, lambda: nc.tensor.matmul(P[7][:, nt * 128:(nt + 1) * 128], lhsT=onesblk[:, :], rhs=sqb[:, nt, :],
                                                      start=True, stop=True), [onesblk, sqb], [P[7]])
                k.op(DVE, lambda: nc.vector.tensor_tensor(out=PK[:, O_BON:O_BON + 512], in0=P[7][:, :],
                                                          in1=V_.rearrange("p n t -> p (n t)"), op=ALU.mult), [P[7], sh], [PK])
                yield
                for nt in range(4):
                    k.op(DVE, lambda: nc.vector.tensor_tensor_scan(out=lp[:, nt, :], data0=scanrst[:, :], data1=lw[:, nt, :],
                                                                   initial=0.0, op0=ALU.mult, op1=ALU.add),
                         [scanrst, lw], [lp])
                k.op(ACT, lambda: nc.scalar.activation(out=v4(eP), in_=v4(lp), func=AF.Exp), [lp], [eP])
                k.op(ACT, lambda: nc.scalar.activation(out=v4(eNP), in_=v4(lp), func=AF.Exp, scale=-1.0), [lp], [eNP])
                k.op(POOL, lambda: nc.gpsimd.tensor_tensor(out=ePp[:], in0=lp[:], in1=lw[:], op=ALU.subtract), [lp, lw], [ePp])
                k.op(ACT, lambda: nc.scalar.activation(out=v4(ePp), in_=v4(ePp), func=AF.Exp), [ePp], [ePp])
                yield
                for c in range(2):
                    k.op(POOL, lambda: nc.gpsimd.tensor_tensor(
                        out=eD[:, :, 64 * c:64 * c + 64], in0=lp[:, :, 64 * c + 63:64 * c + 64].to_broadcast([128, 4, 64]),
                        in1=lp[:, :, 64 * c:64 * c + 64], op=ALU.subtract), [lp], [eD])
                k.op(ACT, lambda: nc.scalar.activation(out=v4(eD), in_=v4(eD), func=AF.Exp), [eD], [eD])
                PCS = pcs[ti % 2]
                k.op(DVE, lambda: nc.vector.tensor_copy(out=PCS[:].rearrange("p (n c) -> p n c", n=4),
                                                        in_=eP[:].rearrange("p n (c t) -> p n c t", c=2)[:, :, :, 63]),
                     [eP], [PCS])
                k.dma(self.s_pc[b, i], PCS[:], reads=[PCS])
                yield
                AR = PK[:, O_AR:O_AR + 1024].rearrange("p (n w t) -> p n w t", n=4, w=2)
                BKv = PK[:, O_BK:O_BK + 1024].rearrange("p (n c g t) -> p n c g t", n=4, c=2, g=2)
                BPv = PK[:, O_BKP:O_BKP + 1024].rearrange("p (n c g t) -> p n c g t", n=4, c=2, g=2)
                BT = PK[:, O_BT:O_BT + 512].rearrange("p (n t) -> p n t", n=4)
                k.op(DVE, lambda: nc.vector.scalar_tensor_tensor(out=AR[:, :, 0, :], in0=kk[:], scalar=-1.0, in1=ePp[:],
                                                                 op0=ALU.mult, op1=ALU.mult), [kk, ePp], [PK])
                k.op(POOL, lambda: nc.gpsimd.tensor_tensor(out=AR[:, :, 1, :], in0=R_, in1=eP[:], op=ALU.mult), [sh, eP], [PK])
                k.op(POOL, lambda: nc.gpsimd.tensor_tensor(out=BT, in0=bb[:], in1=eNP[:], op=ALU.mult), [bb, eNP], [PK])
                yield
                n_ = 0
                for (dst, ee) in ((BKv, eNP), (BPv, eD)):
                    for c in range(2):
                        sl = slice(64 * c, 64 * c + 64)
                        for (srcv, g_) in ((bb, c), (kmod, 1 - c)):
                            eng = DVE if n_ % 2 == 0 else POOL
                            mod = nc.vector if eng == DVE else nc.gpsimd
                            k.op(eng, lambda: mod.tensor_tensor(out=dst[:, :, c, g_, :], in0=srcv[:, :, sl], in1=ee[:, :, sl],
                                                                op=ALU.mult), [srcv, ee], [PK])
                            n_ += 1
                k.op(ACT, lambda: nc.scalar.copy(out=PK[:, O_VT:O_VT + 512], in_=V_.rearrange("p n t -> p (n t)")), [sh], [PK])
                k.dma(self.s_pk[b, i], PK[:], reads=[PK])

            def drain(*gens):
                alive = [g for g in gens if g is not None]
                while alive:
                    for g in list(alive):
                        try:
                            next(g)
                        except StopIteration:
                            alive.remove(g)

            drain(F_gen(0, *tiles[0]))
            for ti in range(len(tiles)):
                nf = F_gen(ti + 1, *tiles[ti + 1]) if ti + 1 < len(tiles) else None
                drain(R_gen(ti, *tiles[ti]), nf)
            k.barrier()
        if self.stop == "p1":
            return
        with contextlib.ExitStack() as es:
            wout = k.sb("wout", [128, 10, DM], BF16, es)
            wmem = k.sb("wmem", [128, 8, 512], BF16, es)
            with contextlib.ExitStack() as es0:
                self.stage = [k.sb("stgA2", [128, 2048], F32, es0), k.sb("stgB2", [128, 2048], F32, es0)]
                self._load_w(es, "wout", self.w[pre + "w_out"], 1280, DM, wb=wout)
                self._load_w(es, "wmem", self.w[pre + "w_mem_kv"], DM, 512, wb=wmem)
                k.barrier()
            kmT, vm = self._mem_kv(es, wmem, "o")
            causal = k.sb("causal", [128, 128], BF16, es)
            maskRW = k.sb("maskRW", [128, 512], BF16, es)
            lnw = k.sb("lnw", [128, 512], F32, es)
            lnb = k.sb("lnb", [128, 512], F32, es)
            k.dma(causal[:], self.cst["causal"], writes=[causal])
            k.dma(maskRW[:], self.cst["maskRW"], writes=[maskRW])
            k.dma(lnw[:], self.w[pre + "lnx_w"][0:1, :].broadcast_to([128, 512]), writes=[lnw])
            k.dma(lnb[:], self.w[pre + "lnx_b"][0:1, :].broadcast_to([128, 512]), writes=[lnb])
            gfin = None
            if last and self.final_norm:
                gfin = k.sb("gfin", [128, DM], F32, es)
                k.dma(gfin[:], self.w["final_norm"][0:1, :].broadcast_to([128, DM]), writes=[gfin])
            kT = k.sb("kTM", [128, 8, SEQ], BF16, es)
            vM = k.sb("vMc", [128, NT, 8, 65], BF16, es)
            xt = [k.sb("xt2_%d" % i, [128, DM], F32, es) for i in range(2)]
            pk = [k.sb("pk2_%d" % i, [128, ODD_W], BF16, es) for i in range(2)]
            pT = [[k.sb("pT%d_%d" % (i, j), [128, 4, 128], BF16, es) for j in range(2)] for i in range(2)]
            SBANKS = [(P[3], P[4]), (P[0], P[1])]
            rec = k.sb("rec", [128, 16], F32, es)
            y = k.sb("y", [128, 1280], F32, es)
            yg = k.sb("yg", [128, 1280], BF16, es)
            ygT = k.sb("ygT", [128, 10, 128], BF16, es)
            xo = k.sb("xo", [128, DM], F32, es)
            junk = k.sb("junk2", [128, DM], BF16, es)
            st = k.sb("st2", [128, 48], F32, es)
            Am = k.sb("Am", [128, 8, 512], BF16, es)
            Xa = k.sb("Xa", [128, 8, 128], BF16, es)
            Xb = k.sb("Xb", [128, 8, 128], BF16, es)
            XT = k.sb("XT", [128, 8, 128], BF16, es)
            NM = [k.sb("NM%d" % i, [128, 8, 256], BF16, es) for i in range(2)]
            W_all = k.sb("W_all", [128, 8, 2, 64], BF16, es)
            BPtm = [k.sb("BPtm%d" % p, [128, 2, 128], BF16, es) for p in range(4)]
            S_f = k.sb("S_f", [128, 4, 64], F32, es)
            S_b = k.sb("S_b", [128, 4, 64], BF16, es)
            ytok = k.sb("ytok", [128, 8, 64], F32, es)
            ysq = k.sb("ysq", [128, 8, 64], F32, es)
            pcl = [k.sb("pcl%d" % i, [128, 8], F32, es) for i in range(2)]
            k.op(POOL, lambda: nc.gpsimd.memset(W_all[:], 0.0), [], [W_all])

            def prefetch(ti):
                b_, i_ = tiles[ti]
                k.dma(xt[ti % 2][:], src[b_, i_ * 128:(i_ + 1) * 128, :], reads=[self.xbuf[b_][i_]], writes=[xt[ti % 2]])
                k.dma(pk[ti % 2][:], self.s_pk[b_, i_], writes=[pk[ti % 2]])
                k.dma(pcl[ti % 2][:], self.s_pc[b_, i_], writes=[pcl[ti % 2]])

            prefetch(0)
            sc = 0
            SCL = 96.0 ** -0.5
            for ti, (b, i) in enumerate(tiles):
                X = xt[ti % 2]
                PK = pk[ti % 2]
                if i == 0:
                    k.dma(kT[0:96, :, :], self.s_kTM[b], writes=[kT])
                    k.dma(vM[:].rearrange("p i h d -> p i (h d)"), self.s_vA[b].rearrange("i p w -> p i w"), writes=[vM])
                if ti + 1 < len(tiles):
                    prefetch(ti + 1)
                qT = PK[:, O_QT:O_QT + 1024].rearrange("p (h t) -> p h t", h=8)
                qmT = PK[:, O_QMT:O_QMT + 256].rearrange("p (a t) -> p a t", a=2)
                sg = PK[:, O_SG:O_SG + 1280]
                AR = PK[:, O_AR:O_AR + 1024].rearrange("p (n w t) -> p n w t", n=4, w=2)
                BKv = PK[:, O_BK:O_BK + 1024].rearrange("p (n c x) -> p n c x", n=4, c=2)
                BPv = PK[:, O_BKP:O_BKP + 1024].rearrange("p (n c x) -> p n c x", n=4, c=2)
                BT = PK[:, O_BT:O_BT + 512].rearrange("p (n t) -> p n t", n=4)
                VT = PK[:, O_VT:O_VT + 512].rearrange("p (n t) -> p n t", n=4)
                BON = PK[:, O_BON:O_BON + 512]
                PCL = pcl[ti % 2]
                pcv = PCL[:]
                first = [True, True]

                def qk_m(j, sc_):
                    for half in range(2):
                        S_ = SBANKS[sc_ % 2][half]
                        for hh in range(4):
                            h = half * 4 + hh
                            k.op(PE, lambda: nc.tensor.matmul(S_[:, hh * 128:(hh + 1) * 128], lhsT=kT[0:96, h, j * 128:(j + 1) * 128],
                                                              rhs=qT[0:96, h, :], start=True, stop=True), [kT, PK], [S_])

                qk_m(0, sc)
                for j in range(i + 1):
                    if j + 1 <= i:
                        qk_m(j + 1, sc + 1)
                    for half in range(2):
                        S = SBANKS[sc % 2][half]
                        pt = pT[sc % 2][half]
                        k.op(ACT, lambda: nc.scalar.activation(out=pt[:].rearrange("p h q -> p (h q)"), in_=S[:, :],
                                                               func=AF.Exp, scale=SCL), [S], [pt])
                        if j == i:
                            k.op(DVE if half == 0 else POOL, lambda: (nc.vector if half == 0 else nc.gpsimd).tensor_tensor(
                                out=pt[:], in0=pt[:], in1=causal[:, :].unsqueeze(1).to_broadcast([128, 4, 128]), op=ALU.mult),
                                [pt, causal], [pt])
                        O = P[5 + half]
                        for hh in range(4):
                            h = half * 4 + hh
                            k.op(PE, lambda: nc.tensor.matmul(O[:, hh * 65:(hh + 1) * 65], lhsT=pt[:, hh, :], rhs=vM[:, j, h, :],
                                                              start=first[half], stop=(j == i), skip_group_check=True),
                                 [pt, vM], [O])
                            first[half] = False
                    sc += 1
                for half in range(2):
                    O = P[5 + half]
                    ov = O[:, 0:260].rearrange("p (h d) -> p h d", h=4)
                    k.op(DVE, lambda: nc.vector.reciprocal(out=rec[:, half * 4:half * 4 + 4], in_=ov[:, :, 64]), [O], [rec])
                    k.op(DVE, lambda: nc.vector.tensor_tensor(
                        out=y[:, half * 256:(half + 1) * 256].rearrange("p (h d) -> p h d", h=4), in0=ov[:, :, 0:64],
                        in1=rec[:, half * 4:half * 4 + 4].unsqueeze(2).to_broadcast([128, 4, 64]), op=ALU.mult), [O, rec], [y])
                k.op(POOL, lambda: nc.gpsimd.memset(W_all[0:64, :, 0, :], 0.0), [], [W_all])
                k.op(POOL, lambda: nc.gpsimd.memset(W_all[64:128, :, 1, :], 0.0), [], [W_all])
                for p_ in range(4):
                    k.op(PE, lambda: nc.tensor.transpose(out=pb2[:, 0:128], in_=AR[:, p_, 0, :], identity=self.ident[:]),
                         [PK, self.ident], [P[2]])
                    k.op(PE, lambda: nc.tensor.transpose(out=pb2[64:128, 128:256], in_=VT[:, p_, 0:64], identity=self.ident[:]),
                         [PK, self.ident], [P[2]])
                    k.op(PE, lambda: nc.tensor.transpose(out=pb2[0:64, 128:256], in_=VT[:, p_, 64:128], identity=self.ident[:]),
                         [PK, self.ident], [P[2]])
                    for c in range(2):
                        k.op(PE, lambda: nc.tensor.transpose(out=pb2[:, 256 + c * 128:384 + c * 128], in_=BPv[:, p_, c, :],
                                                             identity=self.ident[:]), [PK, self.ident], [P[2]])
                    for s_ in range(2):
                        k.op(ACT, lambda: nc.scalar.copy(out=Xa[:, 2 * p_ + s_, 64 * s_:64 * s_ + 64],
                                                         in_=pb2[:, 64 * s_:64 * s_ + 64]), [P[2]], [Xa])
                    k.op(DVE, lambda: nc.vector.tensor_copy(out=W_all[64:128, 2 * p_:2 * p_ + 2, 0, :],
                                                            in_=pb2[64:128, 128:256].rearrange("p (s d) -> p s d", s=2)),
                         [P[2]], [W_all])
                    k.op(DVE, lambda: nc.vector.tensor_copy(out=W_all[0:64, 2 * p_:2 * p_ + 2, 1, :],
                                                            in_=pb2[0:64, 128:256].rearrange("p (s d) -> p s d", s=2)),
                         [P[2]], [W_all])
                    k.op(ACT, lambda: nc.scalar.copy(out=BPtm[p_][:].rearrange("p c x -> p (c x)"), in_=pb2[:, 256:512]),
                         [P[2]], [BPtm[p_]])
                for h in range(8):
                    s_, p_ = h % 2, h // 2
                    rs = slice(64 * s_, 64 * s_ + 64)
                    A = P[3 + s_]
                    for c in range(2):
                        k.op(PE, lambda: nc.tensor.matmul(A[:, c * 128:(c + 1) * 128], lhsT=BKv[rs, p_, c, :],
                                                          rhs=AR[rs, p_, :, 64 * c:64 * c + 64], start=True, stop=True), [PK], [A])
                    k.op(PE, lambda: nc.tensor.matmul(A[:, 256:384], lhsT=AR[rs, p_, 0, :], rhs=BT[rs, p_, :],
                                                      start=True, stop=True), [PK], [A])
                    k.op(PE, lambda: nc.tensor.matmul(A[:, 384:512], lhsT=BT[rs, p_, :], rhs=AR[rs, p_, 0, :],
                                                      start=True, stop=True), [PK], [A])
                    k.op(DVE, lambda: nc.vector.tensor_tensor(out=Am[:, h, :], in0=A[:, :], in1=maskRW[:], op=ALU.mult),
                         [A, maskRW], [Am])
                GB = [(P[0], P[3], P[4]), (P[1], P[5], P[6])]
                for g in range(2):
                    XB = GB[g][0]
                    for q in range(4):
                        h = 4 * g + q
                        for c in range(2):
                            k.op(PE, lambda: nc.tensor.matmul(XB[64 * c:64 * c + 64, q * 64:(q + 1) * 64],
                                                              lhsT=Am[:, h, c * 128:c * 128 + 64], rhs=W_all[:, h, c, :],
                                                              start=True, stop=True), [Am, W_all], [XB])
                    for s_ in range(2):
                        uc = slice(64 * (1 - s_), 64 * (1 - s_) + 64)
                        k.op(ACT, lambda: nc.scalar.copy(
                            out=Xa[:, 4 * g + s_:4 * g + 4:2, uc],
                            in_=XB[:, 0:256].rearrange("p (q d) -> p q d", q=4)[:, s_:4:2, :]), [XB], [Xa])
                Xc, Xn = Xa, Xb
                cur = None
                for lvl in range(6):
                    for g in range(2):
                        XB = GB[g][0]
                        for q in range(4):
                            h = 4 * g + q
                            Mh = Am[:, h, 384:512] if cur is None else cur[:, h, 128:256]
                            k.op(PE, lambda: nc.tensor.matmul(XB[:, q * 128:(q + 1) * 128], lhsT=Mh, rhs=Xc[:, h, :],
                                                              start=True, stop=True), [Am if cur is None else cur, Xc], [XB])
                    for g in range(2):
                        XB = GB[g][0]
                        k.op(DVE, lambda: nc.vector.tensor_tensor(
                            out=Xn[:, 4 * g:4 * g + 4, :].rearrange("p q t -> p (q t)"), in0=XB[:, :],
                            in1=Xc[:, 4 * g:4 * g + 4, :].rearrange("p q t -> p (q t)"), op=ALU.add), [XB, Xc], [Xn])
                    Xc, Xn = Xn, Xc
                    if lvl < 5:
                        nxt = NM[lvl % 2]
                        for g in range(2):
                            Qn, Qm = GB[g][1], GB[g][2]
                            for q in range(4):
                                h = 4 * g + q
                                Nh = Am[:, h, 256:384] if cur is None else cur[:, h, 0:128]
                                Mh = Am[:, h, 384:512] if cur is None else cur[:, h, 128:256]
                                srcb = Am if cur is None else cur
                                if lvl < 4:
                                    k.op(PE, lambda: nc.tensor.matmul(Qn[:, q * 128:(q + 1) * 128], lhsT=Mh, rhs=Nh,
                                                                      start=True, stop=True), [srcb], [Qn])
                                k.op(PE, lambda: nc.tensor.matmul(Qm[:, q * 128:(q + 1) * 128], lhsT=Nh, rhs=Mh,
                                                                  start=True, stop=True), [srcb], [Qm])
                        for g in range(2):
                            Qn, Qm = GB[g][1], GB[g][2]
                            if lvl < 4:
                                k.op(ACT, lambda: nc.scalar.copy(out=nxt[:, 4 * g:4 * g + 4, 0:128],
                                                                 in_=Qn[:, :].rearrange("p (q t) -> p q t", q=4)), [Qn], [nxt])
                            k.op(DVE if g == 0 else ACT, lambda: (nc.vector.tensor_copy if g == 0 else nc.scalar.copy)(
                                out=nxt[:, 4 * g:4 * g + 4, 128:256], in_=Qm[:, :].rearrange("p (q t) -> p q t", q=4)),
                                [Qm], [nxt])
                        cur = nxt
                for h in range(8):
                    k.op(PE, lambda: nc.tensor.transpose(out=pb2[:, h * 128:(h + 1) * 128], in_=Xa[:, h, :],
                                                         identity=self.ident[:]), [Xa, self.ident], [P[2]])
                k.op(ACT, lambda: nc.scalar.copy(out=XT[:].rearrange("p h t -> p (h t)"), in_=pb2[:, 0:1024]), [P[2]], [XT])
                pcv3 = pcv.rearrange("p (n c) -> p n c", n=4)
                for c in range(2):
                    cs = slice(64 * c, 64 * c + 64)
                    fresh = (i == 0 and c == 0)
                    for s_ in range(2):
                        rs = slice(64 * s_, 64 * s_ + 64)
                        uc = slice(64 * (1 - s_), 64 * (1 - s_) + 64)
                        Ba = P[s_]
                        if fresh:
                            k.op(DVE, lambda: nc.vector.tensor_copy(out=W_all[cs, s_:8:2, c, :], in_=Xa[cs, s_:8:2, uc]),
                                 [Xa], [W_all])
                        else:
                            for p_ in range(4):
                                h = 2 * p_ + s_
                                k.op(PE, lambda: nc.tensor.matmul(Ba[cs, p_ * 64:(p_ + 1) * 64], lhsT=XT[rs, h, cs],
                                                                  rhs=S_b[rs, p_, :], start=True, stop=True), [XT, S_b], [Ba])
                            k.op(DVE, lambda: nc.vector.tensor_tensor(
                                out=W_all[cs, s_:8:2, c, :], in0=Ba[cs, 0:256].rearrange("p (q d) -> p q d", q=4),
                                in1=Xa[cs, s_:8:2, uc], op=ALU.add), [Ba, Xa], [W_all])
                    for s_ in range(2):
                        rs = slice(64 * s_, 64 * s_ + 64)
                        Ba, Bb = P[s_], P[3 + s_]
                        for p_ in range(4):
                            h = 2 * p_ + s_
                            yo = slice(256 + p_ * 64, 256 + (p_ + 1) * 64)
                            if not fresh:
                                k.op(PE, lambda: nc.tensor.matmul(Ba[cs, yo], lhsT=AR[rs, p_, 1, cs], rhs=S_b[rs, p_, :],
                                                                  start=True, stop=False), [PK, S_b], [Ba])
                            k.op(PE, lambda: nc.tensor.matmul(Ba[cs, yo], lhsT=Am[:, h, c * 128 + 64:(c + 1) * 128],
                                                              rhs=W_all[:, h, c, :], start=fresh, stop=True), [Am, W_all], [Ba])
                        for p_ in range(4):
                            h = 2 * p_ + s_
                            k.op(PE, lambda: nc.tensor.matmul(Bb[rs, p_ * 64:(p_ + 1) * 64], lhsT=BPtm[p_][:, c, rs],
                                                              rhs=W_all[:, h, c, :], start=True, stop=True),
                                 [BPtm[p_], W_all], [Bb])
                    for s_ in range(2):
                        rs = slice(64 * s_, 64 * s_ + 64)
                        Ba, Bb = P[s_], P[3 + s_]
                        sfv = S_f[rs, :, :]
                        if fresh:
                            k.op(DVE, lambda: nc.vector.tensor_copy(out=sfv, in_=Bb[rs, 0:256].rearrange("p (q d) -> p q d", q=4)),
                                 [Bb], [S_f])
                        else:
                            k.op(POOL, lambda: nc.gpsimd.tensor_tensor(
                                out=sfv, in0=sfv, in1=pcv3[rs, :, c].unsqueeze(2).to_broadcast([64, 4, 64]), op=ALU.mult),
                                [S_f, PCL], [S_f])
                            k.op(DVE, lambda: nc.vector.tensor_tensor(out=sfv, in0=Bb[rs, 0:256].rearrange("p (q d) -> p q d", q=4),
                                                                      in1=sfv, op=ALU.add), [Bb, S_f], [S_f])
                        k.op(POOL, lambda: nc.gpsimd.tensor_copy(out=S_b[rs, :, :], in_=sfv), [S_f], [S_b])
                        k.op(ACT, lambda: nc.scalar.copy(out=ytok[cs, s_:8:2, :],
                                                         in_=Ba[cs, 256:512].rearrange("p (q d) -> p q d", q=4)), [Ba], [ytok])
                k.op(DVE, lambda: nc.vector.tensor_reduce(out=st[:, 8:16], in_=ytok[:], axis=AX.X, op=ALU.add), [ytok], [st])
                k.op(POOL, lambda: nc.gpsimd.tensor_tensor(out=ysq[:], in0=ytok[:], in1=ytok[:], op=ALU.mult), [ytok], [ysq])
                k.op(DVE, lambda: nc.vector.tensor_reduce(out=st[:, 16:24], in_=ysq[:], axis=AX.X, op=ALU.add), [ysq], [st])
                k.op(DVE, lambda: nc.vector.tensor_scalar(out=st[:, 8:16], in0=st[:, 8:16], scalar1=1.0 / 64, scalar2=None,
                                                          op0=ALU.mult), [st], [st])
                k.op(DVE, lambda: nc.vector.tensor_tensor(out=st[:, 24:32], in0=st[:, 8:16], in1=st[:, 8:16], op=ALU.mult),
                     [st], [st])
                k.op(DVE, lambda: nc.vector.scalar_tensor_tensor(out=st[:, 16:24], in0=st[:, 16:24], scalar=1.0 / 64,
                                                                 in1=st[:, 24:32], op0=ALU.mult, op1=ALU.subtract),
                     [st], [st])
                k.op(ACT, lambda: nc.scalar.activation(out=st[:, 24:32], in_=st[:, 16:24], func=AF.Sqrt, bias=LN_EPS), [st], [st])
                k.op(DVE, lambda: nc.vector.reciprocal(out=st[:, 16:24], in_=st[:, 24:32]), [st], [st])
                k.op(DVE, lambda: nc.vector.tensor_tensor(out=ysq[:], in0=ytok[:],
                                                          in1=st[:, 8:16].unsqueeze(2).to_broadcast([128, 8, 64]),
                                                          op=ALU.subtract), [ytok, st], [ysq])
                k.op(DVE, lambda: nc.vector.tensor_tensor(out=ysq[:], in0=ysq[:],
                                                          in1=st[:, 16:24].unsqueeze(2).to_broadcast([128, 8, 64]),
                                                          op=ALU.mult), [ysq, st], [ysq])
                ysf = ysq[:].rearrange("p h d -> p (h d)")
                k.op(POOL, lambda: nc.gpsimd.tensor_tensor(out=ysf, in0=ysf, in1=lnw[:], op=ALU.mult), [ysq, lnw], [ysq])
                k.op(POOL, lambda: nc.gpsimd.tensor_tensor(out=ysf, in0=ysf, in1=lnb[:], op=ALU.add), [ysq, lnb], [ysq])
                for c in range(4):
                    k.op(PE, lambda: nc.tensor.transpose(out=pb2[:, c * 128:(c + 1) * 128], in_=BON[:, c * 128:(c + 1) * 128],
                                                         identity=self.ident[:]), [PK, self.ident], [P[2]])
                k.op(DVE, lambda: nc.vector.tensor_tensor(out=y[:, 512:1024], in0=pb2[:, 0:512], in1=ysf, op=ALU.add),
                     [P[2], ysq], [y])
                self._mem_attn(b, qmT, PK, kmT, vm, pT, rec, y, sc, SBANKS)
                sc += 2
                self._tail(PK, sg, y, yg, ygT, wout, X, xo, junk, st, b, i, last, gfin)
            k.barrier()

    def _mem_attn(self, b, qmT, PK, kmT, vm, pT, rec, y, sc, SBANKS):
        k, nc = self.k, self.nc
        P = self.P
        O = P[7]
        first = True
        for mb in range(2):
            Sb = SBANKS[sc % 2]
            ptp = pT[sc % 2]
            sc += 1
            for h in range(4):
                s_, p_ = h % 2, h // 2
                k.op(PE, lambda: nc.tensor.matmul(Sb[s_][:, p_ * 128:(p_ + 1) * 128],
                                                  lhsT=kmT[64 * s_:64 * s_ + 64, b, p_, mb * 128:(mb + 1) * 128],
                                                  rhs=qmT[64 * s_:64 * s_ + 64, p_, :], start=True, stop=True),
                     [kmT, PK], [Sb[s_]])
            for s_ in range(2):
                pt = ptp[s_]
                k.op(ACT, lambda: nc.scalar.activation(out=pt[:, 0:2, :].rearrange("p h q -> p (h q)"), in_=Sb[s_][:, 0:256],
                                                       func=AF.Exp, scale=0.125), [Sb[s_]], [pt])
                for p_ in range(2):
                    h = 2 * p_ + s_
                    k.op(PE, lambda: nc.tensor.matmul(O[:, h * 65:(h + 1) * 65], lhsT=pt[:, p_, :], rhs=vm[:, b, mb, h, :],
                                                      start=first, stop=(mb == 1), skip_group_check=True), [pt, vm], [O])
                    first = False
        ov = O[:, 0:260].rearrange("p (h d) -> p h d", h=4)
        k.op(DVE, lambda: nc.vector.reciprocal(out=rec[:, 8:12], in_=ov[:, :, 64]), [O], [rec])
        k.op(DVE, lambda: nc.vector.tensor_tensor(
            out=y[:, 1024:1280].rearrange("p (h d) -> p h d", h=4), in0=ov[:, :, 0:64],
            in1=rec[:, 8:12].unsqueeze(2).to_broadcast([128, 4, 64]), op=ALU.mult), [O, rec], [y])

    def _tail(self, PK, sg, y, yg, ygT, wout, X, xo, junk, st, b, i, last, gfin):
        k, nc = self.k, self.nc
        P = self.P
        pb2 = P[2][:].bitcast(BF16)
        k.op(POOL, lambda: nc.gpsimd.tensor_tensor(out=yg[:], in0=y[:], in1=sg, op=ALU.mult), [y, PK], [yg])
        for c0, c1 in ((0, 8), (8, 10)):
            for c in range(c0, c1):
                k.op(PE, lambda: nc.tensor.transpose(out=pb2[:, (c - c0) * 128:(c - c0 + 1) * 128],
                                                     in_=yg[:, c * 128:(c + 1) * 128], identity=self.ident[:]),
                     [yg, self.ident], [P[2]])
            k.op(ACT, lambda: nc.scalar.copy(out=ygT[:, c0:c1, :].rearrange("p c t -> p (c t)"),
                                             in_=pb2[:, 0:(c1 - c0) * 128]), [P[2]], [ygT])
        for n in range(2):
            for c in range(10):
                k.op(PE, lambda: nc.tensor.matmul(P[n][:, :], lhsT=ygT[:, c, :], rhs=wout[:, c, n * 512:(n + 1) * 512],
                                                  start=(c == 0), stop=(c == 9)), [ygT, wout], [P[n]])
            k.op(DVE, lambda: nc.vector.tensor_tensor(out=xo[:, n * 512:(n + 1) * 512], in0=P[n][:, :],
                                                      in1=X[:, n * 512:(n + 1) * 512], op=ALU.add), [P[n], X], [xo])
        if gfin is not None:
            k.op(ACT, lambda: nc.scalar.activation(out=junk[:], in_=xo[:], func=AF.Square, accum_out=st[:, 0:1]),
                 [xo], [junk, st])
            k.op(ACT, lambda: nc.scalar.activation(out=st[:, 1:2], in_=st[:, 0:1], func=AF.Sqrt, scale=1.0 / DM, bias=EPS),
                 [st], [st])
            k.op(DVE, lambda: nc.vector.reciprocal(out=st[:, 2:3], in_=st[:, 1:2]), [st], [st])
            k.op(DVE, lambda: nc.vector.scalar_tensor_tensor(out=xo[:], in0=xo[:], scalar=st[:, 2:3], in1=gfin[:],
                                                             op0=ALU.mult, op1=ALU.mult), [xo, st, gfin], [xo])
        k.dma(self.out[b, i * 128:(i + 1) * 128, :], xo[:], reads=[xo], writes=[self.xbuf[b][i]])


    def _tail_gen(self, PK, sg, y, yg, ygT, wout, X, xo, junk, st, b, i, last, gfin, ob=7):
        k, nc = self.k, self.nc
        P = self.P
        pb2 = P[2][:].bitcast(BF16)
        k.op(POOL, lambda: nc.gpsimd.tensor_tensor(out=yg[:], in0=y[:], in1=sg, op=ALU.mult), [y, PK], [yg])
        yield
        for c0, c1 in ((0, 8), (8, 10)):
            for c in range(c0, c1):
                k.op(PE, lambda: nc.tensor.transpose(out=pb2[:, (c - c0) * 128:(c - c0 + 1) * 128],
                                                     in_=yg[:, c * 128:(c + 1) * 128], identity=self.ident[:]),
                     [yg, self.ident], [P[2]])
            k.op(ACT, lambda: nc.scalar.copy(out=ygT[:, c0:c1, :].rearrange("p c t -> p (c t)"),
                                             in_=pb2[:, 0:(c1 - c0) * 128]), [P[2]], [ygT])
            yield
        for n in range(2):
            for c in range(10):
                k.op(PE, lambda: nc.tensor.matmul(P[ob][:, :], lhsT=ygT[:, c, :], rhs=wout[:, c, n * 512:(n + 1) * 512],
                                                  start=(c == 0), stop=(c == 9)), [ygT, wout], [P[ob]])
                if c == 4:
                    yield
            k.op(DVE, lambda: nc.vector.tensor_tensor(out=xo[:, n * 512:(n + 1) * 512], in0=P[ob][:, :],
                                                      in1=X[:, n * 512:(n + 1) * 512], op=ALU.add), [P[ob], X], [xo])
            yield
        if gfin is not None:
            k.op(ACT, lambda: nc.scalar.activation(out=junk[:], in_=xo[:], func=AF.Square, accum_out=st[:, 0:1]),
                 [xo], [junk, st])
            k.op(ACT, lambda: nc.scalar.activation(out=st[:, 1:2], in_=st[:, 0:1], func=AF.Sqrt, scale=1.0 / DM, bias=EPS),
                 [st], [st])
            k.op(DVE, lambda: nc.vector.reciprocal(out=st[:, 2:3], in_=st[:, 1:2]), [st], [st])
            k.op(DVE, lambda: nc.vector.scalar_tensor_tensor(out=xo[:], in0=xo[:], scalar=st[:, 2:3], in1=gfin[:],
                                                             op0=ALU.mult, op1=ALU.mult), [xo, st, gfin], [xo])
        k.dma(self.out[b, i * 128:(i + 1) * 128, :], xo[:], reads=[xo], writes=[self.xbuf[b][i]])


def _col(v, n):
    return np.ascontiguousarray(np.asarray(v, np.float32).reshape(n, 128).T)


_NET_CACHE = {}


def _get_net(n_layers=4, final_norm=True):
    key = (n_layers, final_norm)
    if key not in _NET_CACHE:
        _NET_CACHE[key] = Net(n_layers, final_norm)
    return _NET_CACHE[key]


def _in_maps(inputs):
    cst = _consts()
    shared = {"c_" + n: v for n, v in cst.items()}
    shared["mem_norm_col"] = _col(inputs["mem_norm"], 8)
    shared["final_norm"] = np.asarray(inputs["final_norm"], np.float32).reshape(1, DM)
    for l in range(4):
        p = "l%d_" % l
        shared[p + "norm_col"] = _col(inputs[p + "norm"], 8)
        shared[p + "w_mem_kv"] = np.asarray(inputs[p + "w_mem_kv"], np.float32)
        shared[p + "w_out"] = np.asarray(inputs[p + "w_out"], np.float32)
        shared[p + "w_in"] = np.asarray(inputs[p + "w_in"], np.float32)
        if l % 2 == 1:
            dc = 1664 if l == 1 else 1696
            shared[p + "qn_col"] = _col(inputs[p + "q_norm"], 2)
            shared[p + "kvn_col"] = _col(inputs[p + "kv_norm"], 1)
            shared[p + "w_qb"] = np.asarray(inputs[p + "w_qb"], np.float32)
            shared[p + "w_kvb"] = np.asarray(inputs[p + "w_kvb"], np.float32)
            mu = np.zeros(14 * 128, np.float32)
            mu[:dc] = np.asarray(inputs[p + "mu_shift"], np.float32)
            v0 = np.asarray(inputs[p + "v0"], np.float32) if l == 3 else np.zeros(512, np.float32)
            cols = [_col(mu, 14)] + [_col(np.asarray(inputs[p + n], np.float32).reshape(-1), 4) if n else _col(v0, 4)
                                     for n in ("w0", "a0", None, "k_k", "k_a", "r_k")]
            shared[p + "cols"] = np.ascontiguousarray(np.concatenate(cols, 1))
            shared[p + "w2"] = np.asarray(inputs[p + "w2"], np.float32)
            shared[p + "a2"] = np.asarray(inputs[p + "a2"], np.float32)
            if l == 3:
                shared[p + "v2"] = np.asarray(inputs[p + "v2"], np.float32)
            shared[p + "lnx_w"] = np.asarray(inputs[p + "lnx_w"], np.float32).reshape(1, 512)
            shared[p + "lnx_b"] = np.asarray(inputs[p + "lnx_b"], np.float32).reshape(1, 512)
    maps = []
    x = np.asarray(inputs["x"], np.float32)
    mem = np.asarray(inputs["mem"], np.float32)
    pos = np.asarray(inputs["positions"], np.int32)
    for c in range(NCORES):
        m = dict(shared)
        m["x"] = np.ascontiguousarray(x[2 * c:2 * c + 2])
        m["mem"] = np.ascontiguousarray(mem[2 * c:2 * c + 2])
        m["pos_col"] = np.ascontiguousarray(pos[2 * c:2 * c + 2].reshape(2, NT, 128).transpose(2, 0, 1).reshape(128, 2 * NT))
        maps.append(m)
    return maps


ALL_INPUTS = (
    "x", "mem", "positions", "mem_norm", "final_norm",
    "l0_norm", "l0_w_in", "l0_w_mem_kv", "l0_w_out",
    "l1_norm", "l1_w_in", "l1_q_norm", "l1_w_qb", "l1_kv_norm", "l1_w_kvb", "l1_mu_shift", "l1_w0", "l1_w2", "l1_a0",
    "l1_a2", "l1_k_k", "l1_k_a", "l1_r_k", "l1_lnx_w", "l1_lnx_b", "l1_w_mem_kv", "l1_w_out",
    "l2_norm", "l2_w_in", "l2_w_mem_kv", "l2_w_out",
    "l3_norm", "l3_w_in", "l3_q_norm", "l3_w_qb", "l3_kv_norm", "l3_w_kvb", "l3_mu_shift", "l3_w0", "l3_w2", "l3_a0",
    "l3_a2", "l3_v0", "l3_v2", "l3_k_k", "l3_k_a", "l3_r_k", "l3_lnx_w", "l3_lnx_b", "l3_w_mem_kv", "l3_w_out",
)


def kernel(**inputs):
    assert all(n in inputs for n in ALL_INPUTS)
    net = _get_net()
    maps = _in_maps(inputs)
    res = run_bass_kernel_spmd(net.nc, maps, core_ids=list(range(NCORES)))
    return np.concatenate([np.asarray(r["out"], np.float32) for r in res.results], axis=0)
```
